# Optimizing a Trainium2 kernel written in Bass

```python
import jax, jax.numpy as jnp
from jax import lax
import numpy as np

D_MODEL = 2048
BATCH = 16
SEQ = 256
DEPTH = 1
DEC_BATCH = 4
DEC_SEQ = 4096
PAST_LEN = 512

GRID_W = 64
MLSTM_HEADS = 4
MLSTM_DK = 128
MLSTM_DV = 256
MLSTM_WIDTH = MLSTM_HEADS * MLSTM_DV
MLSTM_QK = MLSTM_HEADS * MLSTM_DK
MLSTM_CHUNK = 64
RGLRU_WIDTH = 1024
RGLRU_BLOCKS = 8
RGLRU_BLOCK_DIM = RGLRU_WIDTH // RGLRU_BLOCKS
RGLRU_C = 8.0
CONV_WIDTH = 4
CONV_LEFT = 2
MIX_WIDTH = MLSTM_WIDTH + RGLRU_WIDTH
IN_COLS = 2 * MLSTM_QK + 2 * MLSTM_WIDTH + 4 * MLSTM_HEADS + 2 * RGLRU_WIDTH
N_GROUPS = 4
EXPERTS_PER_GROUP = 8
N_EXPERTS = N_GROUPS * EXPERTS_PER_GROUP
TOP_K = 2
EXPERT_FF = 1024
MOE_BLOCK = 128
EPS = 1e-6

kernel_name = "hybrid_mlstm_rglru_hmoe_diffusion_step"


def _rms_norm(x, w):
    xf = x.astype(jnp.float32)
    y = xf * lax.rsqrt(jnp.mean(xf * xf, axis=-1, keepdims=True) + EPS)
    return (y * w.astype(jnp.float32)).astype(x.dtype)


def _modulate(x, w, shift, scale):
    return _rms_norm(x, w) * (1.0 + scale[:, None, :]) + shift[:, None, :]


def _mlstm_chunkwise(q, k, v, i_pre, f_pre, c0, n0, m0):
    f32 = jnp.float32
    B, H, T, _ = q.shape
    L = MLSTM_CHUNK
    nc = T // L
    log_i = i_pre.astype(f32)
    log_f = jax.nn.log_sigmoid(f_pre.astype(f32))

    def chunks(a):
        return jnp.moveaxis(a.astype(f32).reshape(B, H, nc, L, *a.shape[3:]), 2, 0)

    xs = (chunks(q), chunks(k), chunks(v), chunks(log_i), chunks(log_f))
    lower = jnp.tril(jnp.ones((L, L), dtype=bool))

    def step(carry, xc):
        c, n, m = carry
        qc, kc, vc, li, lf = xc
        b = jnp.cumsum(lf, axis=-1)
        d = jnp.where(lower, b[..., :, None] - b[..., None, :] + li[..., None, :], -jnp.inf)
        inter = b + m[..., None]
        m_t = jnp.maximum(inter, jnp.max(d, axis=-1))
        s = jnp.einsum('bhtd,bhsd->bhts', qc, kc) * jnp.exp(d - m_t[..., None])
        w_inter = jnp.exp(inter - m_t)
        num = jnp.einsum('bhts,bhsv->bhtv', s, vc) + w_inter[..., None] * jnp.einsum('bhtd,bhdv->bhtv', qc, c)
        den = jnp.sum(s, axis=-1) + w_inter * jnp.einsum('bhtd,bhd->bht', qc, n)
        h = num / jnp.maximum(jnp.abs(den), jnp.exp(-m_t))[..., None]
        b_end = b[..., -1]
        g = b_end[..., None] - b + li
        m_new = jnp.maximum(b_end + m, jnp.max(g, axis=-1))
        wk = jnp.exp(g - m_new[..., None])
        decay = jnp.exp(b_end + m - m_new)
        c_new = decay[..., None, None] * c + jnp.einsum('bhsd,bhsv->bhdv', kc * wk[..., None], vc)
        n_new = decay[..., None] * n + jnp.einsum('bhs,bhsd->bhd', wk, kc)
        return (c_new, n_new, m_new), h

    carry0 = (c0.astype(f32), n0.astype(f32), m0.astype(f32))
    (c, n, m), hs = lax.scan(step, carry0, xs)
    h = jnp.moveaxis(hs, 0, 2).reshape(B, H, T, v.shape[-1])
    return h, c, n, m


def _linear_combine(e1, e2):
    a1, b1 = e1
    a2, b2 = e2
    return a1 * a2, a2 * b1 + b2


def _rglru(x, wa, ba, wx, bx, lam, h0):
    B, T, W = x.shape
    xf = x.astype(jnp.float32)
    xb = xf.reshape(B, T, RGLRU_BLOCKS, RGLRU_BLOCK_DIM)
    r = jax.nn.sigmoid(jnp.einsum('btgi,gij->btgj', xb, wa).reshape(B, T, W) + ba)
    ig = jax.nn.sigmoid(jnp.einsum('btgi,gij->btgj', xb, wx).reshape(B, T, W) + bx)
    log_a = -RGLRU_C * r * jax.nn.softplus(-lam.astype(jnp.float32))
    a = jnp.exp(log_a)
    u = jnp.sqrt(-jnp.expm1(2.0 * log_a)) * (ig * xf)
    a_cum, h = lax.associative_scan(_linear_combine, (a, u), axis=1)
    h = h + a_cum * h0.astype(jnp.float32)[:, None, :]
    return h, h[:, -1]


def _dwconv(x, w, b):
    T = x.shape[1]
    xp = jnp.pad(x, ((0, 0), (CONV_LEFT, CONV_WIDTH - 1 - CONV_LEFT), (0, 0)))
    out = b + xp[:, 0:T] * w[0]
    for j in range(1, CONV_WIDTH):
        out = out + xp[:, j:j + T] * w[j]
    return out


def _mixers(h, st, latent, w_in, b_gates, conv_w, conv_b, rg_wa, rg_ba, rg_wx, rg_bx, rg_lambda, mlstm_norm_w, w_out):
    st_c, st_n, st_m, st_h = st
    B, T, _ = h.shape
    z = jnp.einsum('btd,de->bte', h, w_in)
    sizes = (MLSTM_QK, MLSTM_QK, MLSTM_WIDTH, MLSTM_WIDTH, 4 * MLSTM_HEADS, RGLRU_WIDTH, RGLRU_WIDTH)
    q, k, v, o, gates, xr, xg = jnp.split(z, np.cumsum(sizes)[:-1].tolist(), axis=-1)

    def heads(a, dh):
        return a.reshape(B, T, MLSTM_HEADS, dh).transpose(0, 2, 1, 3)

    q = heads(q, MLSTM_DK) * (MLSTM_DK ** -0.5)
    k = heads(k, MLSTM_DK)
    v = heads(v, MLSTM_DV)
    gates = (gates + b_gates).reshape(B, T, 4, MLSTM_HEADS).transpose(2, 0, 3, 1)

    def flip_t(a):
        return jnp.flip(a, axis=2)

    hf, cf, nf, mf = _mlstm_chunkwise(q, k, v, gates[0], gates[1], st_c[:, 0], st_n[:, 0], st_m[:, 0])
    hb, cb, nb, mb = _mlstm_chunkwise(flip_t(q), flip_t(k), flip_t(v), flip_t(gates[2]), flip_t(gates[3]),
                                      st_c[:, 1], st_n[:, 1], st_m[:, 1])
    h_a = (hf + flip_t(hb)).transpose(0, 2, 1, 3)
    h_a = _rms_norm(h_a, mlstm_norm_w.reshape(MLSTM_HEADS, MLSTM_DV)).reshape(B, T, MLSTM_WIDTH)
    y_a = jax.nn.sigmoid(o) * h_a

    if latent:
        rows = T // GRID_W
        xc = _dwconv(xr.reshape(B * rows, GRID_W, RGLRU_WIDTH), conv_w, conv_b).reshape(B, T, RGLRU_WIDTH)
    else:
        xc = _dwconv(xr, conv_w, conv_b)
    hr_f, last_f = _rglru(xc, rg_wa[0], rg_ba[0], rg_wx[0], rg_bx[0], rg_lambda[0], st_h[:, 0])
    hr_b, last_b = _rglru(jnp.flip(xc, axis=1), rg_wa[1], rg_ba[1], rg_wx[1], rg_bx[1], rg_lambda[1], st_h[:, 1])
    y_b = (hr_f + jnp.flip(hr_b, axis=1)) * jax.nn.gelu(xg)

    y = jnp.einsum('btm,md->btd', jnp.concatenate([y_a, y_b.astype(y_a.dtype)], axis=-1), w_out)
    new_st = (jnp.stack([cf, cb], axis=1), jnp.stack([nf, nb], axis=1),
              jnp.stack([mf, mb], axis=1), jnp.stack([last_f, last_b], axis=1))
    return y.astype(h.dtype), new_st


def _hier_moe(h, rg_w, rg_b, re_w, re_b, wg, wu, wd):
    B, T, D = h.shape
    n_tok = B * T
    xt = h.reshape(n_tok, D)
    glog = jnp.einsum('nd,dg->ng', xt, rg_w).astype(jnp.float32) + rg_b
    grp = jnp.argmax(glog, axis=-1)
    p_grp = jnp.take_along_axis(jax.nn.softmax(glog, axis=-1), grp[:, None], axis=-1)
    elog = jnp.einsum('nd,gde->nge', xt, re_w).astype(jnp.float32) + re_b
    elog = jnp.take_along_axis(elog, grp[:, None, None], axis=1)[:, 0]
    top_v, top_i = lax.top_k(elog, TOP_K)
    w_assign = jax.nn.softmax(top_v, axis=-1) * p_grp
    e_assign = grp[:, None].astype(jnp.int32) * EXPERTS_PER_GROUP + top_i.astype(jnp.int32)

    n_assign = n_tok * TOP_K
    flat_e = e_assign.reshape(-1)
    order = jnp.argsort(flat_e)
    sorted_e = flat_e[order]
    counts = jnp.bincount(flat_e, length=N_EXPERTS)
    padded = (counts + MOE_BLOCK - 1) // MOE_BLOCK * MOE_BLOCK
    pad_end = jnp.cumsum(padded)
    pad_start = pad_end - padded
    start = jnp.cumsum(counts) - counts
    dest = pad_start[sorted_e] + jnp.arange(n_assign, dtype=jnp.int32) - start[sorted_e]
    n_blocks = -(-n_assign // MOE_BLOCK) + N_EXPERTS
    n_rows = n_blocks * MOE_BLOCK
    src_tok = (order // TOP_K).astype(jnp.int32)
    row_tok = jnp.full((n_rows,), n_tok, dtype=jnp.int32).at[dest].set(src_tok)
    x_pad = jnp.concatenate([xt, jnp.zeros((1, D), xt.dtype)], axis=0)
    rows = x_pad[row_tok].reshape(n_blocks, MOE_BLOCK, D)
    blk_e = jnp.minimum(jnp.searchsorted(pad_end, jnp.arange(n_blocks, dtype=jnp.int32) * MOE_BLOCK, side='right'),
                        N_EXPERTS - 1)

    def run_block(args):
        xb, e = args
        return (jax.nn.silu(xb @ wg[e]) * (xb @ wu[e])) @ wd[e]

    y_rows = lax.map(run_block, (rows, blk_e)).reshape(n_rows, D)
    contrib = y_rows[dest] * w_assign.reshape(-1)[order][:, None]
    out = jax.ops.segment_sum(contrib, src_tok, num_segments=n_tok)
    return out.reshape(B, T, D).astype(h.dtype)


def _layer(x, mod, st, latent, w_in, b_gates, conv_w, conv_b, rg_wa, rg_ba, rg_wx, rg_bx, rg_lambda,
           mlstm_norm_w, w_out, norm1_w, norm2_w, rgw, rgb, rew, reb, ewg, ewu, ewd):
    shift1, scale1, gate1, shift2, scale2, gate2 = jnp.split(mod.astype(x.dtype), 6, axis=-1)
    h = _modulate(x, norm1_w, shift1, scale1)
    y, new_st = _mixers(h, st, latent, w_in, b_gates, conv_w, conv_b, rg_wa, rg_ba, rg_wx, rg_bx, rg_lambda,
                        mlstm_norm_w, w_out)
    x = x + gate1[:, None, :] * y
    h = _modulate(x, norm2_w, shift2, scale2)
    x = x + gate2[:, None, :] * _hier_moe(h, rgw, rgb, rew, reb, ewg, ewu, ewd)
    return x, new_st


def setup_inputs(seed: int = 0) -> dict:
    key = jax.random.key(seed)
    keys = list(jax.random.split(key, 40))

    def nrm(shape, s):
        return jax.random.normal(keys.pop(), shape, jnp.float32) * s

    D = D_MODEL
    x_prompt = nrm((BATCH, SEQ, D), 1.0)
    x_sample = nrm((DEC_BATCH, DEC_SEQ, D), 1.0)
    state_mlstm_c = nrm((DEC_BATCH, DEPTH, 2, MLSTM_HEADS, MLSTM_DK, MLSTM_DV), 0.5)
    state_mlstm_n = nrm((DEC_BATCH, DEPTH, 2, MLSTM_HEADS, MLSTM_DK), 0.5)
    state_mlstm_m = jax.random.uniform(keys.pop(), (DEC_BATCH, DEPTH, 2, MLSTM_HEADS), jnp.float32, 0.0, 4.0)
    state_rglru_h = nrm((DEC_BATCH, DEPTH, 2, RGLRU_WIDTH), 0.5)
    c = nrm((DEC_BATCH, D), 1.0)
    c_ctx = nrm((D,), 1.0)
    w_ada = nrm((DEPTH, D, 6 * D), 0.5 * D ** -0.5)
    b_ada = nrm((DEPTH, 6 * D), 0.01)
    norm1_w = 1.0 + nrm((DEPTH, D), 0.02)
    w_in = nrm((DEPTH, D, IN_COLS), D ** -0.5)
    gate_i = nrm((DEPTH, 2, 1, MLSTM_HEADS), 0.1)
    gate_f = jnp.linspace(3.0, 6.0, MLSTM_HEADS, dtype=jnp.float32) + nrm((DEPTH, 2, 1, MLSTM_HEADS), 0.1)
    b_gates = jnp.concatenate([gate_i, gate_f], axis=2).reshape(DEPTH, 4 * MLSTM_HEADS)
    conv_w = nrm((DEPTH, CONV_WIDTH, RGLRU_WIDTH), CONV_WIDTH ** -0.5)
    conv_b = nrm((DEPTH, RGLRU_WIDTH), 0.01)
    rg_wa = nrm((DEPTH, 2, RGLRU_BLOCKS, RGLRU_BLOCK_DIM, RGLRU_BLOCK_DIM), RGLRU_BLOCK_DIM ** -0.5)
    rg_ba = nrm((DEPTH, 2, RGLRU_WIDTH), 0.01)
    rg_wx = nrm((DEPTH, 2, RGLRU_BLOCKS, RGLRU_BLOCK_DIM, RGLRU_BLOCK_DIM), RGLRU_BLOCK_DIM ** -0.5)
    rg_bx = nrm((DEPTH, 2, RGLRU_WIDTH), 0.01)
    a0 = jax.random.uniform(keys.pop(), (DEPTH, 2, RGLRU_WIDTH), jnp.float32, 0.9, 0.999)
    s0 = a0 ** (1.0 / RGLRU_C)
    rg_lambda = jnp.log(s0) - jnp.log1p(-s0)
    mlstm_norm_w = 1.0 + nrm((DEPTH, MLSTM_WIDTH), 0.02)
    w_out = nrm((DEPTH, MIX_WIDTH, D), MIX_WIDTH ** -0.5)
    norm2_w = 1.0 + nrm((DEPTH, D), 0.02)
    router_group_w = nrm((DEPTH, D, N_GROUPS), D ** -0.5)
    router_group_b = nrm((DEPTH, N_GROUPS), 0.01)
    router_expert_w = nrm((DEPTH, N_GROUPS, D, EXPERTS_PER_GROUP), D ** -0.5)
    router_expert_b = nrm((DEPTH, N_GROUPS, EXPERTS_PER_GROUP), 0.01)
    expert_w_gate = nrm((DEPTH, N_EXPERTS, D, EXPERT_FF), D ** -0.5)
    expert_w_up = nrm((DEPTH, N_EXPERTS, D, EXPERT_FF), D ** -0.5)
    expert_w_down = nrm((DEPTH, N_EXPERTS, EXPERT_FF, D), EXPERT_FF ** -0.5)
    final_norm_w = 1.0 + nrm((D,), 0.02)
    return {"x_prompt": x_prompt, "x_sample": x_sample, "state_mlstm_c": state_mlstm_c,
            "state_mlstm_n": state_mlstm_n, "state_mlstm_m": state_mlstm_m, "state_rglru_h": state_rglru_h,
            "c": c, "c_ctx": c_ctx, "w_ada": w_ada, "b_ada": b_ada, "norm1_w": norm1_w, "w_in": w_in,
            "b_gates": b_gates, "conv_w": conv_w, "conv_b": conv_b, "rg_wa": rg_wa, "rg_ba": rg_ba,
            "rg_wx": rg_wx, "rg_bx": rg_bx, "rg_lambda": rg_lambda, "mlstm_norm_w": mlstm_norm_w,
            "w_out": w_out, "norm2_w": norm2_w, "router_group_w": router_group_w,
            "router_group_b": router_group_b, "router_expert_w": router_expert_w,
            "router_expert_b": router_expert_b, "expert_w_gate": expert_w_gate, "expert_w_up": expert_w_up,
            "expert_w_down": expert_w_down, "final_norm_w": final_norm_w}


def reference(x_prompt, x_sample, state_mlstm_c, state_mlstm_n, state_mlstm_m, state_rglru_h, c, c_ctx,
              w_ada, b_ada, norm1_w, w_in, b_gates, conv_w, conv_b, rg_wa, rg_ba, rg_wx, rg_bx, rg_lambda,
              mlstm_norm_w, w_out, norm2_w, router_group_w, router_group_b, router_expert_w, router_expert_b,
              expert_w_gate, expert_w_up, expert_w_down, final_norm_w):
    n_req = x_prompt.shape[0]
    zero_st = (jnp.zeros((n_req, 2, MLSTM_HEADS, MLSTM_DK, MLSTM_DV), jnp.float32),
               jnp.zeros((n_req, 2, MLSTM_HEADS, MLSTM_DK), jnp.float32),
               jnp.zeros((n_req, 2, MLSTM_HEADS), jnp.float32),
               jnp.zeros((n_req, 2, RGLRU_WIDTH), jnp.float32))
    yp, ys = x_prompt, x_sample
    new_c, new_n, new_m, new_h = [], [], [], []
    for l in range(DEPTH):
        lp = (w_in[l], b_gates[l], conv_w[l], conv_b[l], rg_wa[l], rg_ba[l], rg_wx[l], rg_bx[l], rg_lambda[l],
              mlstm_norm_w[l], w_out[l], norm1_w[l], norm2_w[l], router_group_w[l], router_group_b[l],
              router_expert_w[l], router_expert_b[l], expert_w_gate[l], expert_w_up[l], expert_w_down[l])
        mod_ctx = jax.nn.silu(c_ctx) @ w_ada[l] + b_ada[l]
        mod_ctx = jnp.broadcast_to(mod_ctx[None, :], (n_req, 6 * D_MODEL))
        mod_lat = jax.nn.silu(c) @ w_ada[l] + b_ada[l]
        yp, (sc, sn, sm, sh) = _layer(yp, mod_ctx, zero_st, False, *lp)
        lat_st = (state_mlstm_c[:, l], state_mlstm_n[:, l], state_mlstm_m[:, l], state_rglru_h[:, l])
        ys, _ = _layer(ys, mod_lat, lat_st, True, *lp)
        new_c.append(sc)
        new_n.append(sn)
        new_m.append(sm)
        new_h.append(sh)
    y_prompt = _rms_norm(yp, final_norm_w)
    y_sample = _rms_norm(ys, final_norm_w)
    return (y_prompt, y_sample, jnp.stack(new_c, axis=1), jnp.stack(new_n, axis=1),
            jnp.stack(new_m, axis=1), jnp.stack(new_h, axis=1))
```

```python
import contextlib
import numpy as np
import concourse.bass as bass
import concourse.mybir as mybir
from concourse.bass_utils import run_bass_kernel_spmd

F32 = mybir.dt.float32
BF16 = mybir.dt.bfloat16
I32 = mybir.dt.int32
U32 = mybir.dt.uint32
AF = mybir.ActivationFunctionType
ALU = mybir.AluOpType
AX = mybir.AxisListType

D = 2048
KD = 16
H = 4
DK = 128
DV = 256
RW = 1024
NG = 8
NE = 32
FF = 1024
EPS = 1e-6
NEG = -30000.0


class Track:
    __slots__ = ("w", "r", "multi", "ds")

    def __init__(self, multi=False):
        self.w = {}
        self.r = {}
        self.multi = multi
        self.ds = {}


class Eng:
    def __init__(self, name, obj, sem, same_wait=True):
        self.name = name
        self.obj = obj
        self.sem = sem
        self.cnt = 0
        self.known = {}
        self.same_wait = same_wait


class DSem:
    def __init__(self, sem):
        self.sem = sem
        self.cnt = 0


class KB:
    def __init__(self, nc, es):
        self.nc = nc
        self.es = es
        self.pe = Eng("pe", nc.tensor, es.enter_context(nc.semaphore("sem_pe")), same_wait=False)
        self.act = Eng("act", nc.scalar, es.enter_context(nc.semaphore("sem_act")))
        self.dve = Eng("dve", nc.vector, es.enter_context(nc.semaphore("sem_dve")))
        self.pool = Eng("pool", nc.gpsimd, es.enter_context(nc.semaphore("sem_pool")))
        self.sp = Eng("sp", nc.sync, None)
        self.engs = [self.pe, self.act, self.dve, self.pool, self.sp]
        self.all_ds = []
        self.bc_regs = {}
        self.free_ds = {"sp": [], "pool": []}
        self.tracks = []
        self.psum = []
        self.pst = []
        for i in range(8):
            t = es.enter_context(nc.psum_tensor(f"psb{i}", [128, 512], F32))
            self.psum.append(t)
            self.pst.append(self.track())
        self.pi = 0

    def track(self, multi=False):
        t = Track(multi)
        self.tracks.append(t)
        return t

    def _ds(self, t, qn):
        if qn not in t.ds:
            if self.free_ds[qn]:
                t.ds[qn] = self.free_ds[qn].pop()
            else:
                d = DSem(self.es.enter_context(self.nc.semaphore(f"dsem_{qn}{len(self.all_ds)}")))
                self.all_ds.append(d)
                t.ds[qn] = d
        return t.ds[qn]

    def bank(self):
        i = self.pi
        self.pi = (self.pi + 1) % 8
        return self.psum[i], self.pst[i]

    def _wait(self, eng, reads, writes):
        deps = {}

        def add(s, v):
            if deps.get(s, 0) < v:
                deps[s] = v

        for t in reads:
            for s, v in t.w.items():
                add(s, v)
        for t in writes:
            if t.multi:
                continue
            for s, v in t.w.items():
                add(s, v)
            for s, v in t.r.items():
                add(s, v)
        for s, v in deps.items():
            if (not eng.same_wait) and eng.sem is not None and s == eng.sem:
                continue
            if eng.known.get(s, 0) < v:
                eng.obj.wait_ge(s, v)
                eng.known[s] = v

    def _mark(self, tok, reads, writes):
        s, v = tok
        for t in writes:
            if t.multi:
                if t.w.get(s, 0) < v:
                    t.w[s] = v
            else:
                t.w = {s: v}
                t.r = {}
        for t in reads:
            if t.r.get(s, 0) < v:
                t.r[s] = v

    def op(self, eng, fn, reads=(), writes=()):
        self._wait(eng, reads, writes)
        ins = fn()
        eng.cnt += 1
        ins.then_inc(eng.sem, 1)
        self._mark((eng.sem, eng.cnt), reads, writes)

    def dma(self, q, out, in_, sem_track, reads=(), writes=(), **kw):
        ds = self._ds(sem_track, q.name)
        self._wait(q, reads, writes)
        ins = q.obj.dma_start(out=out, in_=in_, **kw)
        ds.cnt += 16
        ins.then_inc(ds.sem, 16)
        self._mark((ds.sem, ds.cnt), reads, writes)

    def idma(self, out, out_off, in_, in_off, sem_track, reads=(), writes=(), **kw):
        q = self.pool
        ds = self._ds(sem_track, q.name)
        self._wait(q, reads, writes)
        if "bounds_check" in kw and not hasattr(kw["bounds_check"], "regnum"):
            key = int(kw["bounds_check"])
            if key not in self.bc_regs:
                self.bc_regs[key] = self.nc.gpsimd.to_reg(key)
            kw["bounds_check"] = self.bc_regs[key]
        ins = q.obj.indirect_dma_start(out=out, out_offset=out_off, in_=in_, in_offset=in_off, **kw)
        ds.cnt += 16
        ins.then_inc(ds.sem, 16)
        self._mark((ds.sem, ds.cnt), reads, writes)

    def load(self, q, out, in_, t_dst, reads=(), **kw):
        self.dma(q, out, in_, t_dst, reads=reads, writes=[t_dst], **kw)

    def store(self, q, out, in_, t_src, t_dst, **kw):
        self.dma(q, out, in_, t_src, reads=[t_src], writes=[t_dst], **kw)

    def barrier(self):
        for e in self.engs:
            for e2 in self.engs:
                if e2.sem is not None and e2.cnt > 0 and (e2 is not e or e.same_wait):
                    if e.known.get(e2.sem, 0) < e2.cnt:
                        e.obj.wait_ge(e2.sem, e2.cnt)
                        e.known[e2.sem] = e2.cnt
            for d in self.all_ds:
                if d.cnt > 0 and e.known.get(d.sem, 0) < d.cnt:
                    e.obj.wait_ge(d.sem, d.cnt)
                    e.known[d.sem] = d.cnt
        for t in self.tracks:
            t.w = {}
            t.r = {}
            for qn, d in t.ds.items():
                self.free_ds[qn].append(d)
            t.ds = {}

    def final_wait(self):
        for d in self.all_ds:
            if d.cnt > 0 and self.sp.known.get(d.sem, 0) < d.cnt:
                self.sp.obj.wait_ge(d.sem, d.cnt)
                self.sp.known[d.sem] = d.cnt


class Cfg:
    def __init__(self, NP=2, TP=256, TS=2048, TO=2048, CAP=512, debug=False):
        self.NP, self.TP, self.TS, self.TO, self.CAP, self.debug = NP, TP, TS, TO, CAP, debug
        self.NPT = NP * TP
        self.NOWN = self.NPT + TS
        self.NALL = self.NOWN + TO
        assert self.NPT % 512 == 0 or self.NPT in (256,), self.NPT
        self.blocks = []
        t = 0
        while t < self.NPT:
            n = min(512, self.NPT - t)
            self.blocks.append((t, n, 0))
            t += n
        while t < self.NOWN:
            n = min(512, self.NOWN - t)
            self.blocks.append((t, n, 1))
            t += n
        self.oblocks = []
        while t < self.NALL:
            n = min(512, self.NALL - t)
            self.oblocks.append((t, n, 1))
            t += n


WF = 3072
WT = 2560
WCOLS = WF + WT


def phase01(k, cfg, io, scr):
    nc = k.nc
    T = k.track
    with contextlib.ExitStack() as es:
        def sb(name, shape, dt):
            return es.enter_context(nc.sbuf_tensor("s_" + name, list(shape), dt))

        es0 = contextlib.ExitStack()
        _sb_main = sb

        def sb(name, shape, dt):
            return es0.enter_context(nc.sbuf_tensor("s_" + name, list(shape), dt))
        c2T = sb("c2T", [128, 32], F32)
        c2s = sb("c2s", [128, 32], F32)
        c2b = sb("c2b", [128, 32], BF16)
        t_c2T, t_c2s, t_c2b = T(), T(), T()
        k.load(k.sp, c2T[:], io["c2T"], t_c2T)
        k.op(k.act, lambda: nc.scalar.activation(out=c2s[:], in_=c2T[:], func=AF.Tanh, scale=0.5),
             reads=[t_c2T], writes=[t_c2s])
        k.op(k.dve, lambda: nc.vector.tensor_scalar(out=c2s[:], in0=c2s[:], scalar1=0.5, scalar2=0.5,
                                                     op0=ALU.mult, op1=ALU.add), reads=[t_c2s], writes=[t_c2s])
        k.op(k.dve, lambda: nc.vector.tensor_tensor(out=c2b[:], in0=c2s[:], in1=c2T[:], op=ALU.mult),
             reads=[t_c2s, t_c2T], writes=[t_c2b])
        wa = [sb(f"wa{i}", [128, KD, 512], BF16) for i in range(2)]
        t_wa = [T(), T()]
        brow = [sb(f"brow{i}", [2, 512], F32) for i in range(2)]
        t_brow = [T(), T()]
        mrow = [sb(f"mrow{i}", [2, 512], F32) for i in range(2)]
        t_mrow = [T(), T()]
        t_modrow = scr["t_modrow"]
        w_ada_v = io["w_ada"].rearrange("(k p) c -> p k c", p=128)
        NCT = 6 * D // 512

        def load_wa(ct):
            s = ct % 2
            for hh in range(2):
                k.load(k.pool, wa[s][:, hh * 8:(hh + 1) * 8, :], w_ada_v[:, hh * 8:(hh + 1) * 8, ct * 512:(ct + 1) * 512], t_wa[s])
            k.load(k.sp, brow[s][:], io["b_ada2"][:, ct * 512:(ct + 1) * 512], t_brow[s])

        load_wa(0)
        for ct in range(NCT):
            if ct + 1 < NCT:
                load_wa(ct + 1)
            s = ct % 2
            ps, pt = k.bank()

            def mm(ps=ps, s=s):
                for kk in range(KD):
                    ins = nc.tensor.matmul(ps[0:2, :], lhsT=c2b[:, kk * 2:kk * 2 + 2], rhs=wa[s][:, kk, :],
                                           start=(kk == 0), stop=(kk == KD - 1))
                return ins
            k.op(k.pe, mm, reads=[t_c2b, t_wa[s]], writes=[pt])
            k.op(k.dve, lambda ps=ps, s=s: nc.vector.tensor_tensor(out=mrow[s][:], in0=ps[0:2, :], in1=brow[s][:], op=ALU.add),
                 reads=[pt, t_brow[s]], writes=[t_mrow[s]])
            k.store(k.sp, scr["modrow"][:, ct * 512:(ct + 1) * 512], mrow[s][:], t_mrow[s], t_modrow)

        k.barrier()
        es0.close()
        sb = _sb_main

        NTOK = max(cfg.NOWN, cfg.TO)
        hT = sb("hT", [128, KD, NTOK], BF16)
        t_hT = T()
        n1bc = sb("n1bc", [128, D], F32)
        t_n1 = T()
        k.load(k.sp, n1bc[:], io["norm1_w"].partition_broadcast(128), t_n1)
        g1bc = sb("g1bc", [128, D], F32)
        s1bc = sb("s1bc", [128, D], F32)
        t_g1, t_s1 = T(), T()
        ident = sb("ident_b", [128, 128], BF16)
        t_id = T()
        k.load(k.pool, ident[:], io["ident"], t_id)
        xs = [sb(f"xs{i}", [128, D], F32) for i in range(2)]
        t_xs = [T(), T()]
        junk = sb("junk", [128, D], BF16)
        t_junk = T()
        tmpf = sb("tmpf", [128, D], F32)
        t_tmpf = T()
        hb = [sb(f"hb{i}", [128, D], BF16) for i in range(2)]
        t_hb = [T(), T()]
        st = sb("st1", [128, 8], F32)
        t_st = T()
        cur_mod = [-1]

        def load_mod(m):
            if cur_mod[0] == m:
                return
            cur_mod[0] = m
            k.load(k.sp, g1bc[:], scr["modrow"][m:m + 1, D:2 * D].partition_broadcast(128), t_g1, reads=[t_modrow])
            k.load(k.sp, s1bc[:], scr["modrow"][m:m + 1, 0:D].partition_broadcast(128), t_s1, reads=[t_modrow])
            k.op(k.dve, lambda: nc.vector.scalar_tensor_tensor(out=g1bc[:], in0=g1bc[:], scalar=1.0, in1=n1bc[:],
                                                                op0=ALU.add, op1=ALU.mult), reads=[t_g1, t_n1], writes=[t_g1])

        ntile = [0]

        def norm_tiles(tok0, ntok, m, hoff):
            load_mod(m)
            for i in range(ntok // 128):
                s = ntile[0] % 2
                ntile[0] += 1
                k.load(k.sp, xs[s][:], io["x"][tok0 + i * 128: tok0 + (i + 1) * 128, :], t_xs[s])
                k.op(k.act, lambda s=s: nc.scalar.activation(out=junk[:], in_=xs[s][:], func=AF.Square, accum_out=st[:, 0:1]),
                     reads=[t_xs[s]], writes=[t_junk, t_st])
                k.op(k.dve, lambda: nc.vector.tensor_scalar(out=st[:, 1:2], in0=st[:, 0:1], scalar1=1.0 / D, scalar2=EPS,
                                                             op0=ALU.mult, op1=ALU.add), reads=[t_st], writes=[t_st])
                k.op(k.act, lambda: nc.scalar.activation(out=st[:, 2:3], in_=st[:, 1:2], func=AF.Ln), reads=[t_st], writes=[t_st])
                k.op(k.act, lambda: nc.scalar.activation(out=st[:, 3:4], in_=st[:, 2:3], func=AF.Exp, scale=-0.5), reads=[t_st], writes=[t_st])
                k.op(k.dve, lambda s=s: nc.vector.scalar_tensor_tensor(out=tmpf[:], in0=xs[s][:], scalar=st[:, 3:4], in1=g1bc[:],
                                                                        op0=ALU.mult, op1=ALU.mult),
                     reads=[t_xs[s], t_st, t_g1], writes=[t_tmpf])
                k.op(k.dve, lambda s=s: nc.vector.tensor_tensor(out=hb[s][:], in0=tmpf[:], in1=s1bc[:], op=ALU.add),
                     reads=[t_tmpf, t_s1], writes=[t_hb[s]])
                for half in range(2):
                    ps, pt = k.bank()
                    psb = ps[:].bitcast(BF16)

                    def tr(psb=psb, s=s, half=half):
                        for j in range(8):
                            kk = half * 8 + j
                            ins = nc.tensor.transpose(psb[:, j * 128:(j + 1) * 128], hb[s][:, kk * 128:(kk + 1) * 128], ident[:])
                        return ins
                    k.op(k.pe, tr, reads=[t_hb[s], t_id], writes=[pt])
                    dst = hT[:, half * 8:(half + 1) * 8, hoff + i * 128: hoff + (i + 1) * 128]
                    src = psb.rearrange("p (j t) -> p j t", j=8)
                    if half == 0:
                        k.op(k.act, lambda dst=dst, src=src: nc.scalar.copy(out=dst, in_=src), reads=[pt], writes=[t_hT])
                    else:
                        k.op(k.dve, lambda dst=dst, src=src: nc.vector.tensor_copy(out=dst, in_=src), reads=[pt], writes=[t_hT])

        wt = [sb(f"wt{i}", [128, KD, 512], BF16) for i in range(2)]
        t_wt = [T(), T()]
        wg = sb("wgate", [128, KD, 16], BF16)
        t_wg = T()
        bg = sb("bgate", [8, 2], F32)
        t_bg = T()
        k.load(k.pool, wg[:], io["w_gates"].rearrange("(k p) c -> p k c", p=128), t_wg)
        k.load(k.sp, bg[:], io["b_gates"], t_bg)
        ev = [sb(f"ev{i}", [128, 512], BF16) for i in range(4)]
        t_ev = [T() for _ in range(4)]
        evg = [sb(f"evg{i}", [8, 512], F32) for i in range(2)]
        t_evg = [T(), T()]
        w_in_v = io["w_in"].rearrange("(k p) c -> p k c", p=128)
        wcnt = [0]
        evc = [0]

        def load_w(c0):
            s = wcnt[0] % 2
            wcnt[0] += 1
            for hh in range(2):
                k.load(k.pool, wt[s][:, hh * 8:(hh + 1) * 8, :], w_in_v[:, hh * 8:(hh + 1) * 8, c0:c0 + 512], t_wt[s])
            return s

        def evac_n(ps, pt, dram_dst, t_dst, scale, n):
            i = evc[0] % 4
            evc[0] += 1
            if i % 2 == 0:
                if scale is None:
                    k.op(k.act, lambda: nc.scalar.copy(out=ev[i][:, 0:n], in_=ps[:, 0:n]), reads=[pt], writes=[t_ev[i]])
                else:
                    k.op(k.act, lambda: nc.scalar.mul(out=ev[i][:, 0:n], in_=ps[:, 0:n], mul=scale), reads=[pt], writes=[t_ev[i]])
            else:
                if scale is None:
                    k.op(k.dve, lambda: nc.vector.tensor_copy(out=ev[i][:, 0:n], in_=ps[:, 0:n]), reads=[pt], writes=[t_ev[i]])
                else:
                    k.op(k.dve, lambda: nc.vector.tensor_scalar(out=ev[i][:, 0:n], in0=ps[:, 0:n], scalar1=scale, scalar2=None,
                                                                 op0=ALU.mult), reads=[pt], writes=[t_ev[i]])
            k.store(k.sp, dram_dst, ev[i][:, 0:n], t_ev[i], t_dst)

        def project(blocks, hoff0, ftiles, ttiles, zF, zT, gLI, gFP, t_z, qscale):
            for (tok0, n, m) in blocks:
                ho = tok0 - hoff0
                for gi, gdst in enumerate((gLI, gFP)):
                    ps, pt = k.bank()

                    def mm(ps=ps, gi=gi, ho=ho, n=n):
                        for kk in range(KD):
                            ins = nc.tensor.matmul(ps[0:8, 0:n], lhsT=wg[:, kk, gi * 8:(gi + 1) * 8], rhs=hT[:, kk, ho:ho + n],
                                                   start=(kk == 0), stop=(kk == KD - 1))
                        return ins
                    k.op(k.pe, mm, reads=[t_wg, t_hT], writes=[pt])
                    k.op(k.act, lambda ps=ps, gi=gi, n=n: nc.scalar.activation(out=evg[gi][:, 0:n], in_=ps[0:8, 0:n], func=AF.Identity,
                                                                               bias=bg[:, gi:gi + 1]),
                         reads=[pt, t_bg], writes=[t_evg[gi]])
                    k.store(k.sp, gdst[:, ho:ho + n], evg[gi][:, 0:n], t_evg[gi], t_z)
            tiles = [("F", c) for c in ftiles] + [("T", c) for c in ttiles]
            nxt = load_w(tiles[0][1][0])
            for ti, (kind, (c0, zc0)) in enumerate(tiles):
                s = nxt
                if ti + 1 < len(tiles):
                    nxt = load_w(tiles[ti + 1][1][0])
                if kind == "F":
                    for (tok0, n, m) in blocks:
                        ho = tok0 - hoff0
                        for sub in range(4):
                            ps, pt = k.bank()

                            def mm(ps=ps, s=s, sub=sub, ho=ho, n=n):
                                for kk in range(KD):
                                    ins = nc.tensor.matmul(ps[:, 0:n], lhsT=wt[s][:, kk, sub * 128:(sub + 1) * 128], rhs=hT[:, kk, ho:ho + n],
                                                           start=(kk == 0), stop=(kk == KD - 1))
                                return ins
                            k.op(k.pe, mm, reads=[t_wt[s], t_hT], writes=[pt])
                            row0 = zc0 + sub * 128
                            scale = DK ** -0.5 if (qscale and row0 < 512) else None
                            evac_n(ps, pt, zF[row0:row0 + 128, ho:ho + n], t_z, scale, n)
                else:
                    for (tok0, n, m) in blocks:
                        ho = tok0 - hoff0
                        for tt in range(n // 128):
                            ps, pt = k.bank()

                            def mm(ps=ps, s=s, tt=tt, ho=ho):
                                for kk in range(KD):
                                    ins = nc.tensor.matmul(ps[:, :], lhsT=hT[:, kk, ho + tt * 128: ho + (tt + 1) * 128], rhs=wt[s][:, kk, :],
                                                           start=(kk == 0), stop=(kk == KD - 1))
                                return ins
                            k.op(k.pe, mm, reads=[t_wt[s], t_hT], writes=[pt])
                            evac_n(ps, pt, zT[ho + tt * 128: ho + (tt + 1) * 128, zc0:zc0 + 512], t_z, None, 512)

        for (tok0, n, m) in cfg.blocks:
            norm_tiles(tok0, n, m, tok0)
        ftiles = [(c, c) for c in range(0, WF, 512)]
        ttiles = [(WF + c, c) for c in range(0, WT, 512)]
        project(cfg.blocks, 0, ftiles, ttiles, scr["zF"], scr["zT"], scr["gLI"], scr["gFP"], scr["t_z"], True)
        if cfg.TO:
            for (tok0, n, m) in cfg.oblocks:
                norm_tiles(tok0, n, m, tok0 - cfg.NOWN)
            ftiles_o = [(1024 + c, c) for c in range(0, 1024, 512)]
            ttiles_o = [(WF + c, c) for c in range(0, 1024, 512)] + [(WF + 2048, 1024)]
            project(cfg.oblocks, cfg.NOWN, ftiles_o, ttiles_o, scr["zFo"], scr["zTo"], scr["gLIo"], scr["gFPo"], scr["t_zo"], False)
    k.barrier()


def make_scratch(k, cfg):
    nc = k.nc
    scr = {}

    def dt(name, shape, dtype):
        scr[name] = nc.dram_tensor(name, list(shape), dtype, kind="Internal").ap()
    dt("modrow", [2, 6 * D], F32)
    dt("zF", [WF, cfg.NOWN], BF16)
    dt("zT", [cfg.NOWN, WT], BF16)
    dt("gLI", [8, cfg.NOWN], F32)
    dt("gFP", [8, cfg.NOWN], F32)
    if cfg.TO:
        dt("zFo", [1024, cfg.TO], BF16)
        dt("zTo", [cfg.TO, 1536], BF16)
        dt("gLIo", [8, cfg.TO], F32)
        dt("gFPo", [8, cfg.TO], F32)
    dt("hF", [cfg.NOWN, 1024], F32)
    dt("ymixT", [2048, cfg.NOWN], BF16)
    dt("x1s", [cfg.NOWN, D], F32)
    dt("xdisp", [NE * cfg.CAP, D], BF16)
    dt("ydisp", [NE * cfg.CAP, D], F32)
    for n in ("t_modrow", "t_z", "t_zo", "t_hF", "t_ymix", "t_out", "t_x1s", "t_xdisp", "t_ydisp"):
        scr[n] = k.track(multi=True)
    NT = cfg.NOWN // 128
    scr["IDX"] = k.es.enter_context(nc.sbuf_tensor("s_IDX", [128, NT, 2], I32))
    scr["WTS"] = k.es.enter_context(nc.sbuf_tensor("s_WTS", [128, NT, 2], F32))
    scr["t_idx"] = k.track()
    return scr


def host_consts():
    import ml_dtypes
    c = {}
    c["ident"] = np.eye(128, dtype=np.float32)
    s = np.arange(128)[:, None]
    t = np.arange(128)[None, :]
    mf = np.where(s <= t, 0.0, NEG).astype(np.float32)
    mb = np.where(s >= t, 0.0, NEG).astype(np.float32)
    c["maskneg"] = np.ascontiguousarray(np.stack([np.broadcast_to(mf[:, None, :], (128, 4, 128)),
                                                  np.broadcast_to(mb[:, None, :], (128, 4, 128))], 0))
    sel = np.zeros((4, 4, 128), np.float32)
    for h in range(4):
        sel[h, h, :] = 1.0
    c["sel"] = sel
    c["eye4"] = np.eye(4, dtype=np.float32)
    for cap in (128, 256, 384, 512):
        c["ebase_%d" % cap] = (np.arange(NE) * cap).astype(np.float32)
    c["ltri"] = (s < t).astype(np.float32)
    tt = np.arange(2048)
    c["mS"] = np.ascontiguousarray(np.broadcast_to(np.where(tt % 128 == 0, 0.0, 1.0).astype(np.float32), (4, 2048)))
    c["mE"] = np.ascontiguousarray(np.broadcast_to(np.where(tt % 128 == 127, 0.0, 1.0).astype(np.float32), (4, 2048)))
    c["nS"] = np.ascontiguousarray(np.broadcast_to(np.where(tt % 128 == 0, -1e30, 0.0).astype(np.float32), (4, 2048)))
    c["nE"] = np.ascontiguousarray(np.broadcast_to(np.where(tt % 128 == 127, -1e30, 0.0).astype(np.float32), (4, 2048)))
    return c


def prep_core(inp, core, cfg, consts):
    b = core // 2
    odd = core % 2
    NP, TP, TS, TO = cfg.NP, cfg.TP, cfg.TS, cfg.TO
    xs = inp["x_sample"][b]
    if not odd:
        own = xs[0:TS]
        oth = xs[TS:TS + TO]
    else:
        LT = TS + TO
        full = xs[0:LT][::-1]
        own = full[0:TS]
        oth = full[TS:LT]
    pr = []
    for j in range(NP):
        p = inp["x_prompt"][core * NP + j]
        pr.append(p[::-1] if odd else p)
    x = np.ascontiguousarray(np.concatenate(pr + [own, oth], axis=0), dtype=np.float32)
    m = {"x": x}
    c2 = np.stack([inp["c_ctx"], inp["c"][b]], axis=0)
    m["c2T"] = np.ascontiguousarray(c2.reshape(2, KD, 128).transpose(2, 1, 0).reshape(128, 2 * KD))
    m["w_ada"] = inp["w_ada"][0]
    m["b_ada2"] = np.ascontiguousarray(np.broadcast_to(inp["b_ada"][0][None, :], (2, 6 * D)))
    m["norm1_w"] = inp["norm1_w"][0]
    w = inp["w_in"][0]
    q, kk, v, o = w[:, 0:512], w[:, 512:1024], w[:, 1024:2048], w[:, 2048:3072]
    g = w[:, 3072:3088]
    xr, xg = w[:, 3088:4112], w[:, 4112:5136]
    m["w_in"] = np.ascontiguousarray(np.concatenate([q, kk, xr, xg, v, o, kk], axis=1))
    d0, d1 = (1, 0) if odd else (0, 1)
    gi = [g[:, 8 * d0:8 * d0 + 4], g[:, 8 * d1:8 * d1 + 4]]
    gf = [g[:, 8 * d0 + 4:8 * d0 + 8], g[:, 8 * d1 + 4:8 * d1 + 8]]
    m["w_gates"] = np.ascontiguousarray(np.concatenate(gi + gf, axis=1))
    bgt = inp["b_gates"][0]
    bi = np.concatenate([bgt[8 * d0:8 * d0 + 4], bgt[8 * d1:8 * d1 + 4]])
    bf = np.concatenate([bgt[8 * d0 + 4:8 * d0 + 8], bgt[8 * d1 + 4:8 * d1 + 8]])
    m["b_gates"] = np.ascontiguousarray(np.stack([bi, bf], axis=1))
    for n in ("ident", "maskneg", "sel", "eye4", "mS", "mE", "nS", "nE"):
        m[n] = consts[n]
    m["mlstm_norm_w"] = inp["mlstm_norm_w"][0]
    dirs = (1, 0) if odd else (0, 1)
    m["st_c"] = np.ascontiguousarray(np.stack([inp["state_mlstm_c"][b, 0, dd] for dd in dirs], 0))
    m["st_n"] = np.ascontiguousarray(np.stack([inp["state_mlstm_n"][b, 0, dd].T for dd in dirs], 0))
    m["st_m"] = np.ascontiguousarray(np.stack([inp["state_mlstm_m"][b, 0, dd] for dd in dirs], 0)[:, :, None])
    def pg(v):
        return v.reshape(NG, 128).T
    wa, wx = inp["rg_wa"][0], inp["rg_wx"][0]
    m["rg_w"] = np.ascontiguousarray(np.stack([np.stack([wa[dd], wx[dd]], 0) for dd in dirs], 0).transpose(3, 0, 1, 2, 4))
    m["rg_b"] = np.ascontiguousarray(np.stack([np.stack([pg(inp["rg_ba"][0, dd]), pg(inp["rg_bx"][0, dd])], 0) for dd in dirs], 0).transpose(2, 0, 1, 3))
    m["rg_lam"] = np.ascontiguousarray(np.stack([pg(inp["rg_lambda"][0, dd]) for dd in dirs], 0).transpose(1, 0, 2))
    m["st_h"] = np.ascontiguousarray(np.stack([pg(inp["state_rglru_h"][b, 0, dd]) for dd in dirs], 0).transpose(1, 0, 2))
    cwt = inp["conv_w"][0]
    z1 = np.zeros_like(cwt[0])
    taps = [z1, cwt[3], cwt[2], cwt[1], cwt[0]] if odd else [cwt[0], cwt[1], cwt[2], cwt[3], z1]
    m["conv_w5"] = np.ascontiguousarray(np.stack([pg(t) for t in taps], 2))
    m["conv_b"] = np.ascontiguousarray(pg(inp["conv_b"][0]))
    m["w_out"] = inp["w_out"][0]
    m["norm2_w"] = inp["norm2_w"][0]
    m["final_norm_w"] = inp["final_norm_w"]
    rew = inp["router_expert_w"][0]
    m["w_router"] = np.ascontiguousarray(np.concatenate([inp["router_group_w"][0]] + [rew[g_] for g_ in range(4)], axis=1))
    m["b_router"] = np.ascontiguousarray(np.concatenate([inp["router_group_b"][0], inp["router_expert_b"][0].reshape(-1)]))
    m["ebase"] = consts["ebase_%d" % cfg.CAP]
    m["ltri"] = consts["ltri"]
    m["ew_gate"] = inp["expert_w_gate"][0]
    m["ew_up"] = inp["expert_w_up"][0]
    m["ew_down"] = inp["expert_w_down"][0]
    return m


def declare_io(nc, cfg):
    io = {}

    def inp(name, shape, dt=F32):
        io[name] = nc.dram_tensor(name, list(shape), dt, kind="ExternalInput").ap()
    inp("x", [cfg.NALL, D])
    inp("c2T", [128, 2 * KD])
    inp("w_ada", [D, 6 * D])
    inp("b_ada2", [2, 6 * D])
    inp("norm1_w", [D])
    inp("w_in", [D, WCOLS])
    inp("w_gates", [D, 16])
    inp("b_gates", [8, 2])
    inp("ident", [128, 128])
    inp("maskneg", [2, 128, 4, 128])
    inp("sel", [4, 4, 128])
    inp("eye4", [4, 4])
    for n in ("mS", "mE", "nS", "nE"):
        inp(n, [4, 2048])
    inp("mlstm_norm_w", [1024])
    inp("st_c", [2, 4, 128, 256])
    inp("st_n", [2, 128, 4])
    inp("st_m", [2, 4, 1])
    inp("rg_w", [128, 2, 2, NG, 128])
    inp("rg_b", [128, 2, 2, NG])
    inp("rg_lam", [128, 2, NG])
    inp("st_h", [128, 2, NG])
    inp("conv_w5", [128, NG, 5])
    inp("conv_b", [128, NG])
    inp("w_out", [D, D])
    inp("norm2_w", [D])
    inp("final_norm_w", [D])
    inp("w_router", [D, 36])
    inp("b_router", [36])
    inp("ebase", [NE])
    inp("ltri", [128, 128])
    inp("ew_gate", [NE, D, FF])
    inp("ew_up", [NE, D, FF])
    inp("ew_down", [NE, FF, D])

    def outp(name, shape, dt=F32):
        io[name] = nc.dram_tensor(name, list(shape), dt, kind="ExternalOutput").ap()
    outp("o_c", [cfg.NP, 2, 4, 128, 256])
    outp("o_n", [cfg.NP, 2, 128, 4])
    outp("o_m", [cfg.NP, 2, 4, 1])
    outp("o_h", [128, cfg.NP, 2, NG])
    outp("y", [cfg.NOWN, D])
    outp("o_cnt", [128, NE])
    return io


def seg_list(cfg):
    segs = []
    for j in range(cfg.NP):
        segs.append(("prompt", j * cfg.TP, cfg.TP, j))
    segs.append(("own", cfg.NPT, cfg.TS, 0))
    return segs


def phase2(k, cfg, io, scr):
    nc = k.nc
    T = k.track
    L = 128
    with contextlib.ExitStack() as es:
        def sb(name, shape, dt):
            return es.enter_context(nc.sbuf_tensor("s2_" + name, list(shape), dt))

        def V(fn, r=(), w=()):
            k.op(k.dve, fn, r, w)

        def A(fn, r=(), w=()):
            k.op(k.act, fn, r, w)

        def P(fn, r=(), w=()):
            k.op(k.pe, fn, r, w)

        TMAX = max(cfg.TP, cfg.TS, cfg.TO)
        NCMAX = TMAX // L
        identB = sb("identB", [128, 128], BF16)
        identF = sb("identF", [128, 128], F32)
        maskneg = [sb(f"maskneg{d}", [128, 4, 128], BF16) for d in range(2)]
        sel = sb("sel", [4, 4, 128], F32)
        eye4 = sb("eye4", [4, 4], F32)
        ones4 = sb("ones4", [4, 128], F32)
        onescol = sb("onescol", [128, 1], BF16)
        mS = sb("mS", [4, TMAX], F32)
        mE = sb("mE", [4, TMAX], F32)
        nS = sb("nS", [4, TMAX], F32)
        nE = sb("nE", [4, TMAX], F32)
        nwbc = sb("nwbc", [128, 1024], F32)
        t_c = T()
        k.load(k.pool, identB[:], io["ident"], t_c)
        k.load(k.sp, identF[:], io["ident"], t_c)
        for d in range(2):
            k.load(k.pool, maskneg[d][:], io["maskneg"][d], t_c)
        k.load(k.sp, sel[:], io["sel"], t_c)
        k.load(k.sp, eye4[:], io["eye4"], t_c)
        k.load(k.sp, mS[:], io["mS"][:, 0:TMAX], t_c)
        k.load(k.sp, mE[:], io["mE"][:, 0:TMAX], t_c)
        k.load(k.sp, nS[:], io["nS"][:, 0:TMAX], t_c)
        k.load(k.sp, nE[:], io["nE"][:, 0:TMAX], t_c)
        k.load(k.sp, nwbc[:], io["mlstm_norm_w"].partition_broadcast(128), t_c)
        V(lambda: nc.vector.memset(ones4[:], 1.0), w=[t_c])
        V(lambda: nc.vector.memset(onescol[:], 1.0), w=[t_c])

        R = [sb(f"R{i}", [4, TMAX], F32) for i in range(8)]
        t_R = [T() for _ in range(8)]
        cr = sb("cr", [4, 8, NCMAX], F32)
        t_cr = T()
        AD = sb("AD", [4, NCMAX, 4], F32)
        t_AD = T()
        m0 = sb("m0", [4, 2], F32)
        t_m0 = T()
        CX = [sb(f"CX{h}", [128, 256], F32) for h in range(H)]
        nst = sb("nst", [128, 4], F32)
        Cb = sb("Cb", [128, 4, 257], BF16)
        t_CX, t_n, t_Cb = T(), T(), T()
        qT = [sb(f"qT{i}", [128, 4, 128], BF16) for i in range(2)]
        kT = [sb(f"kT{i}", [128, 4, 128], BF16) for i in range(2)]
        vx = [sb(f"vx{i}", [128, 4, 256], BF16) for i in range(2)]
        kt = [sb(f"kt{i}", [128, 4, 128], BF16) for i in range(2)]
        ot = [sb(f"ot{i}", [128, 1024], BF16) for i in range(2)]
        hfl = [sb(f"hfl{i}", [128, 1024], F32) for i in range(2)]
        t_qT, t_kT, t_vx, t_kt, t_ot, t_hfl = ([T(), T()] for _ in range(6))
        class Two:
            def __init__(self, name, shape, dt):
                self.t = [sb(f"{name}_{i}", shape, dt) for i in range(2)]
                self.tr = [T(), T()]
        wsel = [0]
        _AH, _DT, _ST = Two("AH", [4, 4, 128], F32), Two("DT", [128, 512], F32), Two("ST", [128, 512], BF16)
        _QS, _KW = Two("QS", [128, 4, 128], BF16), Two("KW", [128, 4, 128], BF16)
        _COL, _RD = Two("COL", [128, 12], F32), Two("RD", [128, 8], F32)
        _HS, _SG, _YA = Two("HS", [128, 1024], F32), Two("SG", [128, 1024], F32), Two("YA", [128, 1024], BF16)
        _JK, _STT = Two("junk", [128, 256], F32), Two("st", [128, 12], F32)
        HF = [sb(f"HF{i}", [128, 1024], F32) for i in range(2)]
        t_HF = [T(), T()]
        YT = [sb(f"YT{i}", [128, 8, 128], BF16) for i in range(2)]
        t_YT = [T(), T()]
        slot = [0]

        def prep(gLI, gFP, g0, Tn, d, t_gz):
            ncn = Tn // L
            LI, FPr, TMP, B, MP, CBr, CLr, WKr = [r[:, 0:Tn] for r in R]
            tLI, tFP, tTMP, tB, tMP, tCB, tCL, tWK = t_R
            k.load(k.sp, LI, gLI[4 * d:4 * d + 4, g0:g0 + Tn], tLI, reads=[t_gz])
            k.load(k.sp, FPr, gFP[4 * d:4 * d + 4, g0:g0 + Tn], tFP, reads=[t_gz])
            rv = (lambda ap: ap[:, ::-1]) if d == 1 else (lambda ap: ap)
            A(lambda: nc.scalar.activation(out=TMP, in_=FPr, func=AF.Abs), [tFP], [tTMP])
            A(lambda: nc.scalar.activation(out=TMP, in_=TMP, func=AF.Exp, scale=-1.0), [tTMP], [tTMP])
            V(lambda: nc.vector.tensor_scalar_add(out=TMP, in0=TMP, scalar1=1.0), [tTMP], [tTMP])
            A(lambda: nc.scalar.activation(out=TMP, in_=TMP, func=AF.Ln), [tTMP], [tTMP])
            V(lambda: nc.vector.scalar_tensor_tensor(out=FPr, in0=FPr, scalar=0.0, in1=TMP, op0=ALU.min, op1=ALU.subtract),
              [tFP, tTMP], [tFP])
            msk = (mE if d == 1 else mS)[:, 0:Tn]
            nmk = (nE if d == 1 else nS)[:, 0:Tn]
            V(lambda: nc.vector.tensor_tensor_scan(out=rv(B), data0=rv(msk), data1=rv(FPr), initial=0.0, op0=ALU.mult, op1=ALU.add),
              [tFP, t_c], [tB])
            V(lambda: nc.vector.tensor_tensor(out=LI, in0=LI, in1=B, op=ALU.subtract), [tLI, tB], [tLI])
            V(lambda: nc.vector.tensor_tensor_scan(out=rv(MP), data0=rv(nmk), data1=rv(LI), initial=-1e30, op0=ALU.add, op1=ALU.max),
              [tLI, t_c], [tMP])
            e0 = 0 if d == 1 else L - 1
            bend = B[:, e0::L]
            mpend = MP[:, e0::L]
            c_mA, c_min, c_Mend, c_al, c_tmp = (cr[:, i, 0:ncn] for i in (2, 3, 4, 5, 6))
            V(lambda: nc.vector.tensor_tensor_scan(out=rv(c_mA), data0=rv(mpend), data1=rv(bend), initial=m0[:, d:d + 1],
                                                   op0=ALU.max, op1=ALU.add), [tMP, tB, t_m0], [t_cr])
            if d == 0:
                V(lambda: nc.vector.tensor_copy(out=c_min[:, 0:1], in_=m0[:, 0:1]), [t_m0, t_cr], [t_cr])
                if ncn > 1:
                    V(lambda: nc.vector.tensor_copy(out=c_min[:, 1:ncn], in_=c_mA[:, 0:ncn - 1]), [t_cr], [t_cr])
            else:
                V(lambda: nc.vector.tensor_copy(out=c_min[:, ncn - 1:ncn], in_=m0[:, 1:2]), [t_m0, t_cr], [t_cr])
                if ncn > 1:
                    V(lambda: nc.vector.tensor_copy(out=c_min[:, 0:ncn - 1], in_=c_mA[:, 1:ncn]), [t_cr], [t_cr])
            V(lambda: nc.vector.tensor_tensor(out=c_Mend, in0=c_min, in1=mpend, op=ALU.max), [t_cr, tMP], [t_cr])
            V(lambda: nc.vector.tensor_tensor(out=c_tmp, in0=c_min, in1=c_Mend, op=ALU.subtract), [t_cr], [t_cr])
            A(lambda: nc.scalar.activation(out=c_al, in_=c_tmp, func=AF.Exp), [t_cr], [t_cr])
            V(lambda: nc.vector.tensor_tensor(out=AD[:, 0:ncn, :], in0=c_al.unsqueeze(2).to_broadcast([4, ncn, 4]),
                                              in1=eye4[:].unsqueeze(1).to_broadcast([4, ncn, 4]), op=ALU.mult), [t_cr, t_c], [t_AD])
            v3 = lambda ap: ap.rearrange("p (c l) -> p c l", l=L)
            bc = lambda ap: ap.unsqueeze(2).to_broadcast([4, ncn, L])
            V(lambda: nc.vector.tensor_tensor(out=v3(MP), in0=v3(MP), in1=bc(c_min), op=ALU.max), [tMP, t_cr], [tMP])
            V(lambda: nc.vector.tensor_tensor(out=v3(CBr), in0=bc(c_min), in1=v3(MP), op=ALU.subtract), [tMP, t_cr], [tCB])
            A(lambda: nc.scalar.activation(out=CBr, in_=CBr, func=AF.Exp), [tCB], [tCB])
            V(lambda: nc.vector.tensor_tensor(out=CLr, in0=B, in1=MP, op=ALU.add), [tB, tMP], [tCL])
            A(lambda: nc.scalar.activation(out=CLr, in_=CLr, func=AF.Exp, scale=-1.0), [tCL], [tCL])
            V(lambda: nc.vector.tensor_tensor(out=v3(WKr), in0=v3(LI), in1=bc(c_Mend), op=ALU.subtract), [tLI, t_cr], [tWK])
            A(lambda: nc.scalar.activation(out=WKr, in_=WKr, func=AF.Exp), [tWK], [tWK])
            V(lambda: nc.vector.tensor_scalar(out=MP, in0=MP, scalar1=-1.0, scalar2=None, op0=ALU.mult), [tMP], [tMP])

        def chunk_cols(t0, c):
            w_ = wsel[0]
            AH, DT, ST, QS, KW, COL, RD, HS, SG, YA, junk, st = (x.t[w_] for x in (_AH, _DT, _ST, _QS, _KW, _COL, _RD, _HS, _SG, _YA, _JK, _STT))
            t_AH, t_DT, t_ST, t_QS, t_KW, t_COL, t_RD, t_HS, t_SG, t_YA, t_junk, t_st = (x.tr[w_] for x in (_AH, _DT, _ST, _QS, _KW, _COL, _RD, _HS, _SG, _YA, _JK, _STT))
            ps, pt = k.bank()
            WKr, CLr = R[7], R[6]

            def f():
                nc.tensor.transpose(ps[:, 0:4], WKr[:, t0:t0 + L], identF[0:4, 0:4])
                nc.tensor.transpose(ps[:, 4:8], CLr[:, t0:t0 + L], identF[0:4, 0:4])
                return nc.tensor.matmul(ps[:, 8:12], lhsT=ones4[:], rhs=AD[:, c, :], start=True, stop=True)
            P(f, [t_R[7], t_R[6], t_AD, t_c], [pt])
            V(lambda: nc.vector.tensor_copy(out=COL[:], in_=ps[:, 0:12]), [pt], [t_COL])

        def state_update(s, c):
            w_ = wsel[0]
            AH, DT, ST, QS, KW, COL, RD, HS, SG, YA, junk, st = (x.t[w_] for x in (_AH, _DT, _ST, _QS, _KW, _COL, _RD, _HS, _SG, _YA, _JK, _STT))
            t_AH, t_DT, t_ST, t_QS, t_KW, t_COL, t_RD, t_HS, t_SG, t_YA, t_junk, t_st = (x.tr[w_] for x in (_AH, _DT, _ST, _QS, _KW, _COL, _RD, _HS, _SG, _YA, _JK, _STT))
            V(lambda: nc.vector.tensor_tensor(out=KW[:], in0=kt[s][:], in1=COL[:, 0:4].unsqueeze(2).to_broadcast([128, 4, 128]), op=ALU.mult),
              [t_kt[s], t_COL], [t_KW])
            banks = [k.bank(), k.bank(), k.bank()]

            def f():
                for h in range(H):
                    ps = banks[h // 2][0]
                    nc.tensor.matmul(ps[:, (h % 2) * 256:(h % 2 + 1) * 256], lhsT=KW[:, h, :], rhs=vx[s][:, h, :], start=True, stop=True)
                for h in range(H):
                    ins = nc.tensor.matmul(banks[2][0][:, h:h + 1], lhsT=KW[:, h, :], rhs=onescol[:], start=True, stop=True)
                return ins
            P(f, [t_KW, t_vx[s], t_c], [b[1] for b in banks])
            for h in range(H):
                ps = banks[h // 2][0]
                V(lambda h=h, ps=ps: nc.vector.scalar_tensor_tensor(out=CX[h][:], in0=CX[h][:], scalar=COL[:, 8 + h:9 + h],
                                                                    in1=ps[:, (h % 2) * 256:(h % 2 + 1) * 256], op0=ALU.mult, op1=ALU.add),
                  [t_CX, t_COL, banks[h // 2][1]], [t_CX])
            V(lambda: nc.vector.tensor_tensor(out=nst[:], in0=nst[:], in1=COL[:, 8:12], op=ALU.mult), [t_n, t_COL], [t_n])
            V(lambda: nc.vector.tensor_tensor(out=nst[:], in0=nst[:], in1=banks[2][0][:, 0:4], op=ALU.add), [t_n, banks[2][1]], [t_n])

        def refresh_Cb():
            for h in range(H):
                A(lambda h=h: nc.scalar.copy(out=Cb[:, h, 0:256], in_=CX[h][:]), [t_CX], [t_Cb])
            V(lambda: nc.vector.tensor_copy(out=Cb[:, :, 256], in_=nst[:]), [t_n], [t_Cb])

        def load_chunk(zT, zF, g0, full, t_gz, with_o, hF_src):
            s = slot[0] % 2
            slot[0] += 1
            k.load(k.sp, vx[s][:], zT[g0:g0 + L, 0:1024].rearrange("t (h v) -> t h v", h=4), t_vx[s], reads=[t_gz])
            kcol = 2048 if full else 1024
            k.load(k.sp, kt[s][:], zT[g0:g0 + L, kcol:kcol + 512].rearrange("t (h v) -> t h v", h=4), t_kt[s], reads=[t_gz])
            if full:
                k.load(k.sp, qT[s][:], zF[0:512, g0:g0 + L].rearrange("(h p) t -> p h t", p=128), t_qT[s], reads=[t_gz])
                k.load(k.sp, kT[s][:], zF[512:1024, g0:g0 + L].rearrange("(h p) t -> p h t", p=128), t_kT[s], reads=[t_gz])
            if with_o:
                k.load(k.sp, ot[s][:], zT[g0:g0 + L, 1024:2048], t_ot[s], reads=[t_gz])
                k.load(k.sp, hfl[s][:], hF_src, t_hfl[s], reads=[scr["t_hF"]])
            return s

        def full_chunk(s, t0, g0, c, d, last_dir):
            w_ = wsel[0]
            AH, DT, ST, QS, KW, COL, RD, HS, SG, YA, junk, st = (x.t[w_] for x in (_AH, _DT, _ST, _QS, _KW, _COL, _RD, _HS, _SG, _YA, _JK, _STT))
            t_AH, t_DT, t_ST, t_QS, t_KW, t_COL, t_RD, t_HS, t_SG, t_YA, t_junk, t_st = (x.tr[w_] for x in (_AH, _DT, _ST, _QS, _KW, _COL, _RD, _HS, _SG, _YA, _JK, _STT))
            MPn, CBr = R[4], R[5]
            V(lambda: nc.vector.tensor_tensor(out=AH[:], in0=R[0][:, t0:t0 + L].unsqueeze(1).to_broadcast([4, 4, L]),
                                              in1=eye4[:].unsqueeze(2).to_broadcast([4, 4, L]), op=ALU.mult), [t_R[0], t_c], [t_AH])
            pE, tE = k.bank()

            def fE():
                nc.tensor.matmul(pE[:, :], lhsT=identB[:], rhs=maskneg[d][:].rearrange("p h t -> p (h t)"), start=True, stop=False)
                for h in range(H):
                    nc.tensor.matmul(pE[:, h * L:(h + 1) * L], lhsT=sel[:, h, :], rhs=MPn[:, t0:t0 + L], start=False, stop=False)
                    ins = nc.tensor.matmul(pE[:, h * L:(h + 1) * L], lhsT=AH[:, h, :], rhs=ones4[:], start=False, stop=(h == H - 1))
                return ins
            P(fE, [t_c, t_R[4], t_AH], [tE])
            A(lambda: nc.scalar.activation(out=DT[:], in_=pE[:, :], func=AF.Exp), [tE], [t_DT])
            pS, tS = k.bank()

            def fS():
                for h in range(H):
                    ins = nc.tensor.matmul(pS[:, h * L:(h + 1) * L], lhsT=kT[s][:, h, :], rhs=qT[s][:, h, :], start=True, stop=True)
                return ins
            P(fS, [t_kT[s], t_qT[s]], [tS])
            V(lambda: nc.vector.tensor_tensor(out=ST[:], in0=pS[:, :], in1=DT[:], op=ALU.mult), [tS, t_DT], [t_ST])
            pB, tB_ = k.bank()

            def fB():
                for h in range(H):
                    ins = nc.tensor.matmul(pB[:, h * L:(h + 1) * L], lhsT=sel[:, h, :], rhs=CBr[:, t0:t0 + L], start=True, stop=True)
                return ins
            P(fB, [t_c, t_R[5]], [tB_])
            V(lambda: nc.vector.tensor_tensor(out=QS[:].rearrange("p h t -> p (h t)"), in0=qT[s][:].rearrange("p h t -> p (h t)"),
                                              in1=pB[:, :], op=ALU.mult), [t_qT[s], tB_], [t_QS])
            nb = [k.bank(), k.bank(), k.bank()]

            def fN():
                for h in range(H):
                    ps = nb[h // 2][0][:, (h % 2) * 256:(h % 2 + 1) * 256]
                    nc.tensor.matmul(ps, lhsT=ST[:, h * L:(h + 1) * L], rhs=vx[s][:, h, :], start=True, stop=False)
                    nc.tensor.matmul(ps, lhsT=QS[:, h, :], rhs=Cb[:, h, 0:256], start=False, stop=True)
                for h in range(H):
                    nc.tensor.matmul(nb[2][0][:, h:h + 1], lhsT=ST[:, h * L:(h + 1) * L], rhs=onescol[:], start=True, stop=False)
                    ins = nc.tensor.matmul(nb[2][0][:, h:h + 1], lhsT=QS[:, h, :], rhs=Cb[:, h, 256:257], start=False, stop=True)
                return ins
            P(fN, [t_ST, t_QS, t_vx[s], t_Cb, t_c], [b[1] for b in nb])
            A(lambda: nc.scalar.activation(out=RD[:, 0:4], in_=nb[2][0][:, 0:4], func=AF.Abs), [nb[2][1]], [t_RD])
            V(lambda: nc.vector.tensor_tensor(out=RD[:, 0:4], in0=RD[:, 0:4], in1=COL[:, 4:8], op=ALU.max), [t_RD, t_COL], [t_RD])
            V(lambda: nc.vector.reciprocal(out=RD[:, 4:8], in_=RD[:, 0:4]), [t_RD], [t_RD])
            if not last_dir:
                hs = s
                for h in range(H):
                    ps = nb[h // 2][0][:, (h % 2) * 256:(h % 2 + 1) * 256]
                    A(lambda h=h, ps=ps: nc.scalar.activation(out=HF[hs][:, h * 256:(h + 1) * 256], in_=ps, func=AF.Copy, scale=RD[:, 4 + h:5 + h]),
                      [nb[h // 2][1], t_RD], [t_HF[hs]])
                k.store(k.sp, scr["hF"][g0:g0 + L, :], HF[hs][:], t_HF[hs], scr["t_hF"])
            else:
                for h in range(H):
                    ps = nb[h // 2][0][:, (h % 2) * 256:(h % 2 + 1) * 256]
                    V(lambda h=h, ps=ps: nc.vector.scalar_tensor_tensor(out=HS[:, h * 256:(h + 1) * 256], in0=ps, scalar=RD[:, 4 + h:5 + h],
                                                                        in1=hfl[s][:, h * 256:(h + 1) * 256], op0=ALU.mult, op1=ALU.add),
                      [nb[h // 2][1], t_RD, t_hfl[s]], [t_HS])
                for h in range(H):
                    A(lambda h=h: nc.scalar.activation(out=junk[:], in_=HS[:, h * 256:(h + 1) * 256], func=AF.Square, accum_out=st[:, h:h + 1]),
                      [t_HS], [t_junk, t_st])
                V(lambda: nc.vector.tensor_scalar(out=st[:, 4:8], in0=st[:, 0:4], scalar1=1.0 / DV, scalar2=EPS, op0=ALU.mult, op1=ALU.add),
                  [t_st], [t_st])
                A(lambda: nc.scalar.activation(out=st[:, 4:8], in_=st[:, 4:8], func=AF.Ln), [t_st], [t_st])
                A(lambda: nc.scalar.activation(out=st[:, 8:12], in_=st[:, 4:8], func=AF.Exp, scale=-0.5), [t_st], [t_st])
                A(lambda: nc.scalar.activation(out=SG[:], in_=ot[s][:], func=AF.Tanh, scale=0.5), [t_ot[s]], [t_SG])
                V(lambda: nc.vector.tensor_scalar(out=SG[:], in0=SG[:], scalar1=0.5, scalar2=0.5, op0=ALU.mult, op1=ALU.add), [t_SG], [t_SG])
                V(lambda: nc.vector.tensor_tensor(out=SG[:], in0=SG[:], in1=nwbc[:], op=ALU.mult), [t_SG, t_c], [t_SG])
                for h in range(H):
                    V(lambda h=h: nc.vector.scalar_tensor_tensor(out=YA[:, h * 256:(h + 1) * 256], in0=HS[:, h * 256:(h + 1) * 256],
                                                                 scalar=st[:, 8 + h:9 + h], in1=SG[:, h * 256:(h + 1) * 256],
                                                                 op0=ALU.mult, op1=ALU.mult), [t_HS, t_st, t_SG], [t_YA])
                pY, tY = k.bank()
                pYb = pY[:].bitcast(BF16)

                def fT():
                    for j in range(8):
                        ins = nc.tensor.transpose(pYb[:, j * 128:(j + 1) * 128], YA[:, j * 128:(j + 1) * 128], identB[:])
                    return ins
                P(fT, [t_YA, t_c], [tY])
                ys = full_chunk.ys % 2
                full_chunk.ys += 1
                A(lambda: nc.scalar.copy(out=YT[ys][:].rearrange("p j t -> p (j t)"), in_=pYb), [tY], [t_YT[ys]])
                k.store(k.sp, scr["ymixT"][0:1024, g0:g0 + L].rearrange("(j p) t -> p j t", p=128), YT[ys][:], t_YT[ys], scr["t_ymix"])
        full_chunk.ys = 0

        def init_state_zero(d):
            for h in range(H):
                V(lambda h=h: nc.vector.memset(CX[h][:], 0.0), w=[t_CX])
            V(lambda: nc.vector.memset(nst[:], 0.0), w=[t_n])
            V(lambda: nc.vector.memset(m0[:, d:d + 1], 0.0), w=[t_m0])

        def init_state_input(d):
            for h in range(H):
                k.load(k.sp, CX[h][:], io["st_c"][d, h], t_CX)
            k.load(k.sp, nst[:], io["st_n"][d], t_n)
            k.load(k.sp, m0[:, d:d + 1], io["st_m"][d], t_m0)

        def carry_m(d, c_last):
            V(lambda: nc.vector.tensor_copy(out=m0[:, d:d + 1], in_=cr[:, 2, c_last:c_last + 1]), [t_cr, t_m0], [t_m0])

        def run_pass(kind, seg_g0, Tn, d, full, last_dir, zT, zF, gLI, gFP, t_gz):
            ncn = Tn // L
            prep(gLI, gFP, seg_g0, Tn, d, t_gz)
            if full:
                refresh_Cb()
            order = range(ncn) if d == 0 else range(ncn - 1, -1, -1)
            order = list(order)
            nxt = None
            for i, c in enumerate(order):
                t0 = c * L
                g0 = seg_g0 + t0
                if nxt is None:
                    nxt = load_chunk(zT, zF, g0, full, t_gz, full and last_dir, scr["hF"][g0:g0 + L, :] if full else None)
                s = nxt
                if i + 1 < len(order):
                    g1 = seg_g0 + order[i + 1] * L
                    nxt = load_chunk(zT, zF, g1, full, t_gz, full and last_dir, scr["hF"][g1:g1 + L, :] if full else None)
                wsel[0] = (wsel[0] + 1) % 2
                chunk_cols(t0, c)
                if full:
                    full_chunk(s, t0, g0, c, d, last_dir)
                state_update(s, c)
                if full and i + 1 < len(order):
                    refresh_Cb()

        if cfg.TO:
            init_state_input(1)
            run_pass("other", 0, cfg.TO, 1, False, False, scr["zTo"], None, scr["gLIo"], scr["gFPo"], scr["t_zo"])
            carry_m(1, 0)
            run_pass("own", cfg.NPT, cfg.TS, 1, True, False, scr["zT"], scr["zF"], scr["gLI"], scr["gFP"], scr["t_z"])
        else:
            init_state_input(1)
            run_pass("own", cfg.NPT, cfg.TS, 1, True, False, scr["zT"], scr["zF"], scr["gLI"], scr["gFP"], scr["t_z"])
        init_state_input(0)
        run_pass("own", cfg.NPT, cfg.TS, 0, True, True, scr["zT"], scr["zF"], scr["gLI"], scr["gFP"], scr["t_z"])
        for j in range(cfg.NP):
            for d in (1, 0):
                init_state_zero(d)
                run_pass("prompt", j * cfg.TP, cfg.TP, d, True, d == 0, scr["zT"], scr["zF"], scr["gLI"], scr["gFP"], scr["t_z"])
                ncn = cfg.TP // L
                c_last = ncn - 1 if d == 0 else 0
                t_o = scr["t_out"]
                for h in range(H):
                    k.store(k.sp, io["o_c"][j, d, h], CX[h][:], t_CX, t_o)
                k.store(k.sp, io["o_n"][j, d], nst[:], t_n, t_o)
                k.store(k.sp, io["o_m"][j, d], cr[:, 2, c_last:c_last + 1], t_cr, t_o)
    k.barrier()


def phase3(k, cfg, io, scr):
    nc = k.nc
    T = k.track
    with contextlib.ExitStack() as es:
        def sb(name, shape, dt):
            return es.enter_context(nc.sbuf_tensor("s3_" + name, list(shape), dt))

        def V(fn, r=(), w=()):
            k.op(k.dve, fn, r, w)

        def A(fn, r=(), w=()):
            k.op(k.act, fn, r, w)

        def P(fn, r=(), w=()):
            k.op(k.pe, fn, r, w)

        TMAX = max(cfg.TP, cfg.TS, cfg.TO)
        wgt = sb("wgt", [128, 2, 2, NG, 128], BF16)
        cw = sb("cw", [128, NG, 5], F32)
        cb = sb("cb", [128, NG], F32)
        bax = sb("bax", [128, 2, 2, NG], F32)
        spn = sb("spn", [128, 2, NG], F32)
        lam = sb("lam", [128, 2, NG], F32)
        h0 = sb("h0", [128, 2, NG], F32)
        onec = sb("onec", [128, 1], F32)
        hfin = sb("hfin", [128, cfg.NP, 2, NG], F32)
        hcar = sb("hcar", [128, NG], F32)
        t_c, t_hfin, t_hcar = T(), T(), T()
        k.load(k.pool, wgt[:], io["rg_w"], t_c)
        k.load(k.sp, cw[:], io["conv_w5"], t_c)
        k.load(k.sp, cb[:], io["conv_b"], t_c)
        k.load(k.sp, bax[:], io["rg_b"], t_c)
        k.load(k.sp, lam[:], io["rg_lam"], t_c)
        k.load(k.sp, h0[:], io["st_h"], t_c)
        V(lambda: nc.vector.memset(onec[:], 1.0), w=[t_c])
        V(lambda: nc.vector.tensor_scalar(out=bax[:], in0=bax[:], scalar1=0.5, scalar2=None, op0=ALU.mult), [t_c], [t_c])
        tmp8 = sb("tmp8", [128, 2, NG], F32)
        t_t8 = T()
        A(lambda: nc.scalar.activation(out=tmp8[:], in_=lam[:], func=AF.Abs), [t_c], [t_t8])
        A(lambda: nc.scalar.activation(out=tmp8[:], in_=tmp8[:], func=AF.Exp, scale=-1.0), [t_t8], [t_t8])
        V(lambda: nc.vector.tensor_scalar_add(out=tmp8[:], in0=tmp8[:], scalar1=1.0), [t_t8], [t_t8])
        A(lambda: nc.scalar.activation(out=tmp8[:], in_=tmp8[:], func=AF.Ln), [t_t8], [t_t8])
        V(lambda: nc.vector.tensor_scalar(out=spn[:], in0=lam[:], scalar1=-1.0, scalar2=0.0, op0=ALU.mult, op1=ALU.max), [t_c], [t_c])
        V(lambda: nc.vector.tensor_tensor(out=spn[:], in0=spn[:], in1=tmp8[:], op=ALU.add), [t_c, t_t8], [t_c])
        V(lambda: nc.vector.tensor_scalar(out=spn[:], in0=spn[:], scalar1=-4.0, scalar2=None, op0=ALU.mult), [t_c], [t_c])
        V(lambda: nc.vector.memset(hfin[:], 0.0), w=[t_hfin])

        xr = [sb(f"xr{i}", [128, TMAX], BF16) for i in range(2)]
        xg = [sb(f"xg{i}", [128, TMAX], BF16) for i in range(2)]
        t_xr, t_xg = [T(), T()], [T(), T()]
        class BSet:
            def __init__(self, i):
                self.xc = sb(f"xc_{i}", [128, TMAX], F32)
                self.xcb = sb(f"xcb_{i}", [128, TMAX], BF16)
                self.av = [sb(f"av{d}_{i}", [128, TMAX], F32) for d in range(2)]
                self.uv = [sb(f"uv{d}_{i}", [128, TMAX], F32) for d in range(2)]
                self.hv = [sb(f"hv{d}_{i}", [128, TMAX], F32) for d in range(2)]
                self.g1 = sb(f"g1_{i}", [128, TMAX], F32)
                self.t_xc, self.t_xcb, self.t_g1 = T(), T(), T()
                self.t_a, self.t_u, self.t_h = [T(), T()], [T(), T()], [T(), T()]
        bsets = [BSet(0), BSet(1)]
        cur = [bsets[0]]
        nseg = [0]

        def next_set():
            nseg[0] += 1
            cur[0] = bsets[nseg[0] % 2]
        th = [sb(f"th{i}", [128, 512], F32) for i in range(2)]
        thx = [sb(f"thx{i}", [128, 512], F32) for i in range(2)]
        sq = [sb(f"sq{i}", [128, 512], F32) for i in range(2)]
        t_th, t_thx, t_sq = [T(), T()], [T(), T()], [T(), T()]
        yb = [sb(f"yb{i}", [128, TMAX], BF16) for i in range(2)]
        t_yb = [T(), T()]
        cnt = [0, 0]

        def conv(s, g, Tn, W):
            B_ = cur[0]
            xc, xcb, av, uv, hv, g1 = B_.xc, B_.xcb, B_.av, B_.uv, B_.hv, B_.g1
            t_xc, t_xcb, t_a, t_u, t_h, t_g1 = B_.t_xc, B_.t_xcb, B_.t_a, B_.t_u, B_.t_h, B_.t_g1
            nr = Tn // W
            x3 = xr[s][:, 0:Tn].rearrange("p (r w) -> p r w", w=W)
            c3 = xc[:, 0:Tn].rearrange("p (r w) -> p r w", w=W)
            V(lambda: nc.vector.tensor_scalar(out=xc[:, 0:Tn], in0=xr[s][:, 0:Tn], scalar1=cw[:, g, 2:3], scalar2=cb[:, g:g + 1],
                                              op0=ALU.mult, op1=ALU.add), [t_xr[s], t_c], [t_xc])
            for o in (-2, -1, 1, 2):
                lo_o, hi_o = max(0, -o), W - max(0, o)
                lo_i, hi_i = max(0, o), W - max(0, -o)
                V(lambda o=o, lo_o=lo_o, hi_o=hi_o, lo_i=lo_i, hi_i=hi_i:
                  nc.vector.scalar_tensor_tensor(out=c3[:, :, lo_o:hi_o], in0=x3[:, :, lo_i:hi_i], scalar=cw[:, g, o + 2:o + 3],
                                                 in1=c3[:, :, lo_o:hi_o], op0=ALU.mult, op1=ALU.add), [t_xr[s], t_c, t_xc], [t_xc])
            A(lambda: nc.scalar.copy(out=xcb[:, 0:Tn], in_=xc[:, 0:Tn]), [t_xc], [t_xcb])

        def gates(g, Tn, d):
            B_ = cur[0]
            xc, xcb, av, uv, hv, g1 = B_.xc, B_.xcb, B_.av, B_.uv, B_.hv, B_.g1
            t_xc, t_xcb, t_a, t_u, t_h, t_g1 = B_.t_xc, B_.t_xcb, B_.t_a, B_.t_u, B_.t_h, B_.t_g1
            for b0 in range(0, Tn, 512):
                n = min(512, Tn - b0)
                i = cnt[0] % 2
                cnt[0] += 1
                pa, ta = k.bank()
                px, tx = k.bank()

                def f(pa=pa, px=px, b0=b0, n=n):
                    nc.tensor.matmul(pa[:, 0:n], lhsT=wgt[:, d, 0, g, :], rhs=xcb[:, b0:b0 + n], start=True, stop=True)
                    return nc.tensor.matmul(px[:, 0:n], lhsT=wgt[:, d, 1, g, :], rhs=xcb[:, b0:b0 + n], start=True, stop=True)
                P(f, [t_c, t_xcb], [ta, tx])
                A(lambda pa=pa, n=n, i=i: nc.scalar.activation(out=th[i][:, 0:n], in_=pa[:, 0:n], func=AF.Tanh, scale=0.5, bias=bax[:, d, 0, g:g + 1]),
                  [ta, t_c], [t_th[i]])
                A(lambda px=px, n=n, i=i: nc.scalar.activation(out=thx[i][:, 0:n], in_=px[:, 0:n], func=AF.Tanh, scale=0.5, bias=bax[:, d, 1, g:g + 1]),
                  [tx, t_c], [t_thx[i]])
                A(lambda b0=b0, n=n, i=i: nc.scalar.activation(out=av[d][:, b0:b0 + n], in_=th[i][:, 0:n], func=AF.Exp,
                                                               scale=spn[:, d, g:g + 1], bias=spn[:, d, g:g + 1]), [t_th[i], t_c], [t_a[d]])
                A(lambda b0=b0, n=n, i=i: nc.scalar.activation(out=sq[i][:, 0:n], in_=av[d][:, b0:b0 + n], func=AF.Square), [t_a[d]], [t_sq[i]])
                A(lambda n=n, i=i: nc.scalar.activation(out=sq[i][:, 0:n], in_=sq[i][:, 0:n], func=AF.Ln, scale=-1.0, bias=onec[:]), [t_sq[i], t_c], [t_sq[i]])
                A(lambda n=n, i=i: nc.scalar.activation(out=sq[i][:, 0:n], in_=sq[i][:, 0:n], func=AF.Exp, scale=0.5), [t_sq[i]], [t_sq[i]])
                V(lambda b0=b0, n=n, i=i: nc.vector.scalar_tensor_tensor(out=uv[d][:, b0:b0 + n], in0=thx[i][:, 0:n], scalar=1.0, in1=xc[:, b0:b0 + n],
                                                                         op0=ALU.add, op1=ALU.mult), [t_thx[i], t_xc], [t_u[d]])
                V(lambda b0=b0, n=n, i=i: nc.vector.scalar_tensor_tensor(out=uv[d][:, b0:b0 + n], in0=uv[d][:, b0:b0 + n], scalar=0.5, in1=sq[i][:, 0:n],
                                                                         op0=ALU.mult, op1=ALU.mult), [t_u[d], t_sq[i]], [t_u[d]])

        def scan(Tn, d, init_ap, t_init):
            B_ = cur[0]
            xc, xcb, av, uv, hv, g1 = B_.xc, B_.xcb, B_.av, B_.uv, B_.hv, B_.g1
            t_xc, t_xcb, t_a, t_u, t_h, t_g1 = B_.t_xc, B_.t_xcb, B_.t_a, B_.t_u, B_.t_h, B_.t_g1
            rv = (lambda ap: ap[:, ::-1]) if d == 1 else (lambda ap: ap)
            V(lambda: nc.vector.tensor_tensor_scan(out=rv(hv[d][:, 0:Tn]), data0=rv(av[d][:, 0:Tn]), data1=rv(uv[d][:, 0:Tn]),
                                                   initial=init_ap, op0=ALU.mult, op1=ALU.add), [t_a[d], t_u[d], t_init], [t_h[d]])

        def load_x(zF, row_xr, row_xg, g0, Tn, t_gz):
            s = cnt[1] % 2
            cnt[1] += 1
            k.load(k.sp, xr[s][:, 0:Tn], zF[row_xr:row_xr + 128, g0:g0 + Tn], t_xr[s], reads=[t_gz])
            if row_xg is not None:
                k.load(k.sp, xg[s][:, 0:Tn], zF[row_xg:row_xg + 128, g0:g0 + Tn], t_xg[s], reads=[t_gz])
            return s

        def out_part(s, g, g0, Tn):
            B_ = cur[0]
            xc, xcb, av, uv, hv, g1 = B_.xc, B_.xcb, B_.av, B_.uv, B_.hv, B_.g1
            t_xc, t_xcb, t_a, t_u, t_h, t_g1 = B_.t_xc, B_.t_xcb, B_.t_a, B_.t_u, B_.t_h, B_.t_g1
            X = xg[s][:, 0:Tn]
            G1, G2 = g1[:, 0:Tn], hv[0][:, 0:Tn]
            t_g2 = t_h[0]
            V(lambda: nc.vector.tensor_tensor(out=G1, in0=X, in1=X, op=ALU.mult), [t_xg[s]], [t_g1])
            V(lambda: nc.vector.tensor_scalar(out=G1, in0=G1, scalar1=0.044715, scalar2=1.0, op0=ALU.mult, op1=ALU.add), [t_g1], [t_g1])
            V(lambda: nc.vector.tensor_tensor(out=G1, in0=G1, in1=X, op=ALU.mult), [t_g1, t_xg[s]], [t_g1])
            A(lambda: nc.scalar.activation(out=G1, in_=G1, func=AF.Tanh, scale=0.7978845608028654), [t_g1], [t_g1])
            V(lambda: nc.vector.scalar_tensor_tensor(out=G1, in0=G1, scalar=1.0, in1=X, op0=ALU.add, op1=ALU.mult), [t_g1, t_xg[s]], [t_g1])
            V(lambda: nc.vector.tensor_tensor(out=G2, in0=hv[0][:, 0:Tn], in1=hv[1][:, 0:Tn], op=ALU.add), [t_h[0], t_h[1]], [t_g2])
            ys = cnt[1] % 2
            V(lambda: nc.vector.scalar_tensor_tensor(out=yb[ys][:, 0:Tn], in0=G2, scalar=0.5, in1=G1, op0=ALU.mult, op1=ALU.mult),
              [t_g1, t_g2], [t_yb[ys]])
            k.store(k.sp, scr["ymixT"][1024 + g * 128:1024 + (g + 1) * 128, g0:g0 + Tn], yb[ys][:, 0:Tn], t_yb[ys], scr["t_ymix"])

        zero_col = sb("zero_col", [128, 1], F32)
        V(lambda: nc.vector.memset(zero_col[:], 0.0), w=[t_c])
        for g in range(NG):
            if cfg.TO:
                next_set()
                s = load_x(scr["zFo"], g * 128, None, 0, cfg.TO, scr["t_zo"])
                conv(s, g, cfg.TO, 64)
                gates(g, cfg.TO, 1)
                scan(cfg.TO, 1, h0[:, 1, g:g + 1], t_c)
                V(lambda g=g, B_=cur[0]: nc.vector.tensor_copy(out=hcar[:, g:g + 1], in_=B_.hv[1][:, 0:1]), [cur[0].t_h[1], t_hcar], [t_hcar])
                init1, t_init1 = hcar[:, g:g + 1], t_hcar
            else:
                init1, t_init1 = h0[:, 1, g:g + 1], t_c
            next_set()
            s = load_x(scr["zF"], 1024 + g * 128, 2048 + g * 128, cfg.NPT, cfg.TS, scr["t_z"])
            conv(s, g, cfg.TS, 64)
            for d in range(2):
                gates(g, cfg.TS, d)
            scan(cfg.TS, 0, h0[:, 0, g:g + 1], t_c)
            scan(cfg.TS, 1, init1, t_init1)
            out_part(s, g, cfg.NPT, cfg.TS)
            for j in range(cfg.NP):
                next_set()
                s = load_x(scr["zF"], 1024 + g * 128, 2048 + g * 128, j * cfg.TP, cfg.TP, scr["t_z"])
                conv(s, g, cfg.TP, cfg.TP)
                for d in range(2):
                    gates(g, cfg.TP, d)
                    scan(cfg.TP, d, zero_col[:], t_c)
                V(lambda j=j, g=g, B_=cur[0]: nc.vector.tensor_copy(out=hfin[:, j, 0, g:g + 1], in_=B_.hv[0][:, cfg.TP - 1:cfg.TP]), [cur[0].t_h[0], t_hfin], [t_hfin])
                V(lambda j=j, g=g, B_=cur[0]: nc.vector.tensor_copy(out=hfin[:, j, 1, g:g + 1], in_=B_.hv[1][:, 0:1]), [cur[0].t_h[1], t_hfin], [t_hfin])
                out_part(s, g, j * cfg.TP, cfg.TP)
        k.store(k.sp, io["o_h"], hfin[:], t_hfin, scr["t_out"])
    k.barrier()


def phase4(k, cfg, io, scr):
    nc = k.nc
    T = k.track
    CAP = cfg.CAP
    NT = cfg.NOWN // 128
    IDX, WTS, t_idx = scr["IDX"], scr["WTS"], scr["t_idx"]
    with contextlib.ExitStack() as es:
        def sb(name, shape, dt):
            return es.enter_context(nc.sbuf_tensor("s4_" + name, list(shape), dt))

        def V(fn, r=(), w=()):
            k.op(k.dve, fn, r, w)

        def A(fn, r=(), w=()):
            k.op(k.act, fn, r, w)

        def P(fn, r=(), w=()):
            k.op(k.pe, fn, r, w)

        wo = sb("wo", [128, KD, D], BF16)
        t_wo = T()
        wov = io["w_out"].rearrange("(k p) c -> p k c", p=128)
        for q in range(4):
            for hh in range(2):
                k.load(k.pool, wo[:, hh * 8:(hh + 1) * 8, q * 512:(q + 1) * 512], wov[:, hh * 8:(hh + 1) * 8, q * 512:(q + 1) * 512], t_wo)
        identF = sb("identF", [128, 128], F32)
        wr = sb("wr", [128, KD, 36], F32)
        brbc = sb("brbc", [128, 36], F32)
        ebase = sb("ebase", [128, NE], F32)
        ltri = sb("ltri", [128, 128], BF16)
        onesm = sb("onesm", [128, 128], BF16)
        n2bc = sb("n2bc", [128, D], F32)
        t_c = T()
        k.load(k.sp, identF[:], io["ident"], t_c)
        k.load(k.sp, wr[:], io["w_router"].rearrange("(k p) c -> p k c", p=128), t_c)
        k.load(k.sp, brbc[:], io["b_router"].partition_broadcast(128), t_c)
        k.load(k.sp, ebase[:], io["ebase"].partition_broadcast(128), t_c)
        k.load(k.pool, ltri[:], io["ltri"], t_c)
        k.load(k.sp, n2bc[:], io["norm2_w"].partition_broadcast(128), t_c)
        V(lambda: nc.vector.memset(onesm[:], 1.0), w=[t_c])
        g1bc = sb("g1bc", [128, D], F32)
        G2bc = sb("G2bc", [128, D], F32)
        S2bc = sb("S2bc", [128, D], F32)
        t_g1, t_G2, t_S2 = T(), T(), T()
        run = sb("run", [128, NE], F32)
        t_run = T()
        V(lambda: nc.vector.memset(run[:], 0.0), w=[t_run])
        ym = [sb(f"ym{i}", [128, KD, 128], BF16) for i in range(2)]
        xs = [sb(f"xs{i}", [128, D], F32) for i in range(2)]
        t_ym, t_xs = [T(), T()], [T(), T()]
        x1 = [sb(f"x1{i}", [128, D], F32) for i in range(2)]
        t_x1 = [T(), T()]
        tmpf = sb("tmpf", [128, D], F32)
        t_tmpf = T()
        h2f = sb("h2f", [128, D], F32)
        t_h2f = T()
        h2b = [sb(f"h2b{i}", [128, D], BF16) for i in range(2)]
        t_h2b = [T(), T()]
        h2T = sb("h2T", [128, KD, 128], F32)
        t_h2T = T()
        junk = sb("junk", [128, D], BF16)
        t_junk = T()
        st = sb("st", [128, 8], F32)
        t_st = T()
        rt = sb("rt", [128, 160], F32)
        t_rt = T()
        mx8 = sb("mx8", [128, 8], F32)
        t_mx = T()
        mab = sb("mab", [128, NE], BF16)
        t_mab = T()
        cur_mod = [-1]

        def load_mod(m):
            if cur_mod[0] == m:
                return
            cur_mod[0] = m
            tm = scr["t_modrow"]
            k.load(k.sp, g1bc[:], scr["modrow"][m:m + 1, 2 * D:3 * D].partition_broadcast(128), t_g1, reads=[tm])
            k.load(k.sp, S2bc[:], scr["modrow"][m:m + 1, 3 * D:4 * D].partition_broadcast(128), t_S2, reads=[tm])
            k.load(k.sp, G2bc[:], scr["modrow"][m:m + 1, 4 * D:5 * D].partition_broadcast(128), t_G2, reads=[tm])
            V(lambda: nc.vector.scalar_tensor_tensor(out=G2bc[:], in0=G2bc[:], scalar=1.0, in1=n2bc[:], op0=ALU.add, op1=ALU.mult),
              [t_G2, t_c], [t_G2])

        def load_tile(i):
            s = i % 2
            g0 = i * 128
            k.load(k.sp, ym[s][:], scr["ymixT"][:, g0:g0 + 128].rearrange("(k p) t -> p k t", p=128), t_ym[s], reads=[scr["t_ymix"]])
            k.load(k.sp, xs[s][:], io["x"][g0:g0 + 128, :], t_xs[s])

        load_tile(0)
        for i in range(NT):
            if i + 1 < NT:
                load_tile(i + 1)
            s = i % 2
            g0 = i * 128
            load_mod(0 if g0 < cfg.NPT else 1)
            for q in range(4):
                ps, pt = k.bank()

                def mm(ps=ps, q=q):
                    for kk in range(KD):
                        ins = nc.tensor.matmul(ps[:, :], lhsT=ym[s][:, kk, :], rhs=wo[:, kk, q * 512:(q + 1) * 512],
                                               start=(kk == 0), stop=(kk == KD - 1))
                    return ins
                P(mm, [t_ym[s], t_wo], [pt])
                V(lambda ps=ps, q=q: nc.vector.tensor_tensor(out=tmpf[:, q * 512:(q + 1) * 512], in0=ps[:, :], in1=g1bc[:, q * 512:(q + 1) * 512],
                                                             op=ALU.mult), [pt, t_g1], [t_tmpf])
            V(lambda: nc.vector.tensor_tensor(out=x1[s][:], in0=tmpf[:], in1=xs[s][:], op=ALU.add), [t_tmpf, t_xs[s]], [t_x1[s]])
            k.store(k.sp, scr["x1s"][g0:g0 + 128, :], x1[s][:], t_x1[s], scr["t_x1s"])
            A(lambda: nc.scalar.activation(out=junk[:], in_=x1[s][:], func=AF.Square, accum_out=st[:, 0:1]), [t_x1[s]], [t_junk, t_st])
            V(lambda: nc.vector.tensor_scalar(out=st[:, 1:2], in0=st[:, 0:1], scalar1=1.0 / D, scalar2=EPS, op0=ALU.mult, op1=ALU.add), [t_st], [t_st])
            A(lambda: nc.scalar.activation(out=st[:, 2:3], in_=st[:, 1:2], func=AF.Ln), [t_st], [t_st])
            A(lambda: nc.scalar.activation(out=st[:, 3:4], in_=st[:, 2:3], func=AF.Exp, scale=-0.5), [t_st], [t_st])
            V(lambda: nc.vector.scalar_tensor_tensor(out=tmpf[:], in0=x1[s][:], scalar=st[:, 3:4], in1=G2bc[:], op0=ALU.mult, op1=ALU.mult),
              [t_x1[s], t_st, t_G2], [t_tmpf])
            V(lambda: nc.vector.tensor_tensor(out=h2f[:], in0=tmpf[:], in1=S2bc[:], op=ALU.add), [t_tmpf, t_S2], [t_h2f])
            A(lambda: nc.scalar.copy(out=h2b[s][:], in_=h2f[:]), [t_h2f], [t_h2b[s]])
            for q in range(4):
                ps, pt = k.bank()

                def tr(ps=ps, q=q):
                    for j in range(4):
                        kk = q * 4 + j
                        ins = nc.tensor.transpose(ps[:, j * 128:(j + 1) * 128], h2f[:, kk * 128:(kk + 1) * 128], identF[:])
                    return ins
                P(tr, [t_h2f, t_c], [pt])
                dst = h2T[:, q * 4:(q + 1) * 4, :].rearrange("p j t -> p (j t)")
                if q % 2 == 0:
                    A(lambda dst=dst, ps=ps: nc.scalar.copy(out=dst, in_=ps[:, :]), [pt], [t_h2T])
                else:
                    V(lambda dst=dst, ps=ps: nc.vector.tensor_copy(out=dst, in_=ps[:, :]), [pt], [t_h2T])
            pl, tl = k.bank()

            def mml():
                for kk in range(KD):
                    ins = nc.tensor.matmul(pl[:, 0:36], lhsT=h2T[:, kk, :], rhs=wr[:, kk, :], start=(kk == 0), stop=(kk == KD - 1))
                return ins
            P(mml, [t_h2T, t_c], [tl])
            LG = rt[:, 0:36]
            V(lambda: nc.vector.tensor_tensor(out=LG, in0=pl[:, 0:36], in1=brbc[:], op=ALU.add), [tl, t_c], [t_rt])
            V(lambda: nc.vector.reduce_max(out=rt[:, 36:37], in_=rt[:, 0:4], axis=AX.X), [t_rt], [t_rt])
            V(lambda: nc.vector.tensor_scalar(out=rt[:, 37:38], in0=rt[:, 36:37], scalar1=-1.0, scalar2=None, op0=ALU.mult), [t_rt], [t_rt])
            V(lambda: nc.vector.tensor_scalar(out=rt[:, 40:44], in0=rt[:, 0:4], scalar1=rt[:, 36:37], scalar2=None, op0=ALU.is_equal), [t_rt], [t_rt])
            A(lambda: nc.scalar.activation(out=rt[:, 44:48], in_=rt[:, 0:4], func=AF.Exp, bias=rt[:, 37:38], accum_out=rt[:, 38:39]), [t_rt], [t_rt])
            V(lambda: nc.vector.reciprocal(out=rt[:, 39:40], in_=rt[:, 38:39]), [t_rt], [t_rt])
            V(lambda: nc.vector.tensor_scalar(out=rt[:, 48:56], in0=rt[:, 4:12], scalar1=rt[:, 40:41], scalar2=None, op0=ALU.mult), [t_rt], [t_rt])
            for g in range(1, 4):
                V(lambda g=g: nc.vector.scalar_tensor_tensor(out=rt[:, 48:56], in0=rt[:, 4 + 8 * g:12 + 8 * g], scalar=rt[:, 40 + g:41 + g],
                                                             in1=rt[:, 48:56], op0=ALU.mult, op1=ALU.add), [t_rt], [t_rt])
            V(lambda: nc.vector.max(out=mx8[:], in_=rt[:, 48:56]), [t_rt], [t_mx])
            V(lambda: nc.vector.tensor_scalar(out=rt[:, 56:64], in0=rt[:, 48:56], scalar1=mx8[:, 0:1], scalar2=None, op0=ALU.is_equal), [t_rt, t_mx], [t_rt])
            V(lambda: nc.vector.tensor_scalar(out=rt[:, 64:72], in0=rt[:, 48:56], scalar1=mx8[:, 1:2], scalar2=None, op0=ALU.is_equal), [t_rt, t_mx], [t_rt])
            V(lambda: nc.vector.tensor_tensor(out=rt[:, 72:73], in0=mx8[:, 1:2], in1=mx8[:, 0:1], op=ALU.subtract), [t_mx, t_rt], [t_rt])
            A(lambda: nc.scalar.activation(out=rt[:, 73:74], in_=rt[:, 72:73], func=AF.Exp), [t_rt], [t_rt])
            V(lambda: nc.vector.tensor_scalar_add(out=rt[:, 74:75], in0=rt[:, 73:74], scalar1=1.0), [t_rt], [t_rt])
            V(lambda: nc.vector.reciprocal(out=rt[:, 74:75], in_=rt[:, 74:75]), [t_rt], [t_rt])
            V(lambda: nc.vector.tensor_tensor(out=WTS[:, i, 0:1], in0=rt[:, 74:75], in1=rt[:, 39:40], op=ALU.mult), [t_rt, t_idx], [t_idx])
            V(lambda: nc.vector.tensor_tensor(out=WTS[:, i, 1:2], in0=rt[:, 39:40], in1=WTS[:, i, 0:1], op=ALU.subtract), [t_rt, t_idx], [t_idx])
            for kk2, (oc, oh) in enumerate(((80, 56), (112, 64))):
                V(lambda oc=oc, oh=oh: nc.vector.tensor_tensor(out=rt[:, oc:oc + 32].rearrange("p (g e) -> p g e", g=4),
                                                               in0=rt[:, 40:44].unsqueeze(2).to_broadcast([128, 4, 8]),
                                                               in1=rt[:, oh:oh + 8].unsqueeze(1).to_broadcast([128, 4, 8]), op=ALU.mult), [t_rt], [t_rt])
            V(lambda: nc.vector.tensor_tensor(out=mab[:], in0=rt[:, 80:112], in1=rt[:, 112:144], op=ALU.add), [t_rt], [t_mab])
            pp, tp = k.bank()

            def mmp():
                nc.tensor.matmul(pp[:, 0:32], lhsT=ltri[:], rhs=mab[:], start=True, stop=True)
                return nc.tensor.matmul(pp[:, 32:64], lhsT=onesm[:], rhs=mab[:], start=True, stop=True)
            P(mmp, [t_c, t_mab], [tp])
            V(lambda: nc.vector.tensor_tensor(out=rt[:, 0:32], in0=pp[:, 0:32], in1=run[:], op=ALU.add), [tp, t_run, t_rt], [t_rt])
            V(lambda: nc.vector.tensor_scalar(out=rt[:, 32:64], in0=rt[:, 0:32], scalar1=float(CAP), scalar2=1.0e6, op0=ALU.is_ge, op1=ALU.mult),
              [t_rt], [t_rt])
            V(lambda: nc.vector.tensor_tensor(out=rt[:, 0:32], in0=rt[:, 0:32], in1=rt[:, 32:64], op=ALU.add), [t_rt], [t_rt])
            V(lambda: nc.vector.tensor_tensor(out=rt[:, 0:32], in0=rt[:, 0:32], in1=ebase[:], op=ALU.add), [t_rt, t_c], [t_rt])
            V(lambda: nc.vector.tensor_tensor(out=run[:], in0=run[:], in1=pp[:, 32:64], op=ALU.add), [tp, t_run], [t_run])
            for kk2, oc in enumerate((80, 112)):
                V(lambda oc=oc: nc.vector.tensor_tensor(out=rt[:, 32:64], in0=rt[:, oc:oc + 32], in1=rt[:, 0:32], op=ALU.mult), [t_rt], [t_rt])
                V(lambda kk2=kk2: nc.vector.reduce_sum(out=rt[:, 144 + kk2:145 + kk2], in_=rt[:, 32:64], axis=AX.X), [t_rt], [t_rt])
            V(lambda: nc.vector.tensor_copy(out=IDX[:, i, :], in_=rt[:, 144:146]), [t_rt, t_idx], [t_idx])
            for kk2 in range(2):
                k.idma(out=scr["xdisp"][:, :], out_off=bass.IndirectOffsetOnAxis(ap=IDX[:, i, kk2:kk2 + 1], axis=0),
                       in_=h2b[s][:, :], in_off=None, sem_track=t_h2b[s], reads=[t_h2b[s], t_idx], writes=[scr["t_xdisp"]],
                       bounds_check=NE * CAP - 1, oob_is_err=False)
        k.store(k.sp, io["o_cnt"], run[:], t_run, scr["t_out"])
    k.barrier()


def phase5(k, cfg, io, scr):
    nc = k.nc
    T = k.track
    CAP = cfg.CAP
    NB = CAP // 128
    with contextlib.ExitStack() as es:
        def sb(name, shape, dt):
            return es.enter_context(nc.sbuf_tensor("s5_" + name, list(shape), dt))

        def V(fn, r=(), w=()):
            k.op(k.dve, fn, r, w)

        def A(fn, r=(), w=()):
            k.op(k.act, fn, r, w)

        def P(fn, r=(), w=()):
            k.op(k.pe, fn, r, w)

        identB = sb("identB", [128, 128], BF16)
        t_c = T()
        k.load(k.pool, identB[:], io["ident"], t_c)
        wgu = [sb(f"wgu{i}", [128, KD, 512], BF16) for i in range(4)]
        t_wgu = [T() for _ in range(4)]
        wd = [sb(f"wd{i}", [128, 8, 1024], BF16) for i in range(2)]
        t_wd = [T(), T()]
        xd = [sb("xd0", [128, NB, D], BF16)] * 2
        t_xd = [T()] * 2
        xT = [sb(f"xT{i}", [128, KD, CAP], BF16) for i in range(2)]
        t_xT = [T(), T()]
        hid = [sb(f"hid{i}", [128, 8, CAP], BF16) for i in range(2)]
        t_hid = [T(), T()]
        sg = [sb(f"sg{i}", [128, CAP], F32) for i in range(2)]
        t_sg = [T(), T()]
        yo = [sb(f"yo{i}", [128, 1024], F32) for i in range(4)]
        t_yo = [T() for _ in range(4)]
        cnt = {"gu": 0, "wd": 0, "sg": 0, "yo": 0, "ev": 0}
        wgv = io["ew_gate"].rearrange("e (k p) f -> e p k f", p=128)
        wuv = io["ew_up"].rearrange("e (k p) f -> e p k f", p=128)
        wdv = io["ew_down"].rearrange("e (k p) c -> e p k c", p=128)
        gu_list = []
        for e in range(NE):
            for j in range(2):
                gu_list.append((e, j))
        wd_list = [(e, dh) for e in range(NE) for dh in range(2)]

        def load_gu(idx):
            e, j = gu_list[idx]
            sg_, su_ = (idx % 2) * 2, (idx % 2) * 2 + 1
            for hh in range(2):
                k.load(k.pool, wgu[sg_][:, hh * 8:(hh + 1) * 8, :], wgv[e, :, hh * 8:(hh + 1) * 8, j * 512:(j + 1) * 512], t_wgu[sg_])
            for hh in range(2):
                k.load(k.pool, wgu[su_][:, hh * 8:(hh + 1) * 8, :], wuv[e, :, hh * 8:(hh + 1) * 8, j * 512:(j + 1) * 512], t_wgu[su_])

        def load_wd(idx):
            e, dh = wd_list[idx]
            s_ = idx % 2
            k.load(k.pool, wd[s_][:], wdv[e, :, :, dh * 1024:(dh + 1) * 1024], t_wd[s_])

        def load_x(e):
            s_ = e % 2
            k.load(k.sp, xd[s_][:], scr["xdisp"][e * CAP:(e + 1) * CAP, :].rearrange("(b p) d -> p b d", p=128), t_xd[s_],
                   reads=[scr["t_xdisp"]])

        load_x(0)
        load_gu(0)
        load_wd(0)
        for e in range(NE):
            s_ = e % 2
            for b in range(NB):
                for half in range(2):
                    ps, pt = k.bank()
                    psb = ps[:].bitcast(BF16)

                    def tr(psb=psb, b=b, half=half):
                        for j in range(8):
                            kk = half * 8 + j
                            ins = nc.tensor.transpose(psb[:, j * 128:(j + 1) * 128], xd[s_][:, b, kk * 128:(kk + 1) * 128], identB[:])
                        return ins
                    P(tr, [t_xd[s_], t_c], [pt])
                    dst = xT[s_][:, half * 8:(half + 1) * 8, b * 128:(b + 1) * 128]
                    src = psb.rearrange("p (j t) -> p j t", j=8)
                    if cnt["ev"] % 2 == 0:
                        A(lambda dst=dst, src=src: nc.scalar.copy(out=dst, in_=src), [pt], [t_xT[s_]])
                    else:
                        V(lambda dst=dst, src=src: nc.vector.tensor_copy(out=dst, in_=src), [pt], [t_xT[s_]])
                    cnt["ev"] += 1
            if e + 1 < NE:
                load_x(e + 1)
            for j in range(2):
                gi = e * 2 + j
                if gi + 1 < len(gu_list):
                    load_gu(gi + 1)
                sgw, suw = (gi % 2) * 2, (gi % 2) * 2 + 1
                for f in range(4):
                    pg, tg = k.bank()
                    pu, tu = k.bank()

                    def mm(pg=pg, pu=pu, f=f, sgw=sgw, suw=suw):
                        for kk in range(KD):
                            nc.tensor.matmul(pg[:, 0:CAP], lhsT=wgu[sgw][:, kk, f * 128:(f + 1) * 128], rhs=xT[s_][:, kk, :],
                                             start=(kk == 0), stop=(kk == KD - 1))
                        for kk in range(KD):
                            ins = nc.tensor.matmul(pu[:, 0:CAP], lhsT=wgu[suw][:, kk, f * 128:(f + 1) * 128], rhs=xT[s_][:, kk, :],
                                                   start=(kk == 0), stop=(kk == KD - 1))
                        return ins
                    P(mm, [t_wgu[sgw], t_wgu[suw], t_xT[s_]], [tg, tu])
                    si = cnt["sg"] % 2
                    cnt["sg"] += 1
                    A(lambda pg=pg, si=si: nc.scalar.activation(out=sg[si][:], in_=pg[:, 0:CAP], func=AF.Silu), [tg], [t_sg[si]])
                    V(lambda pu=pu, si=si, j=j, f=f: nc.vector.tensor_tensor(out=hid[s_][:, j * 4 + f, :], in0=sg[si][:], in1=pu[:, 0:CAP], op=ALU.mult),
                      [t_sg[si], tu], [t_hid[s_]])
            for dh in range(2):
                wi = e * 2 + dh
                if wi + 1 < len(wd_list):
                    load_wd(wi + 1)
                ws = wi % 2
                for b in range(NB):
                    yi = cnt["yo"] % 4
                    cnt["yo"] += 1
                    for c in range(2):
                        ps, pt = k.bank()

                        def mm(ps=ps, b=b, c=c, ws=ws):
                            for f in range(8):
                                ins = nc.tensor.matmul(ps[:, :], lhsT=hid[s_][:, f, b * 128:(b + 1) * 128], rhs=wd[ws][:, f, c * 512:(c + 1) * 512],
                                                       start=(f == 0), stop=(f == 7))
                            return ins
                        P(mm, [t_hid[s_], t_wd[ws]], [pt])
                        if cnt["ev"] % 2 == 0:
                            A(lambda ps=ps, yi=yi, c=c: nc.scalar.copy(out=yo[yi][:, c * 512:(c + 1) * 512], in_=ps[:, :]), [pt], [t_yo[yi]])
                        else:
                            V(lambda ps=ps, yi=yi, c=c: nc.vector.tensor_copy(out=yo[yi][:, c * 512:(c + 1) * 512], in_=ps[:, :]), [pt], [t_yo[yi]])
                        cnt["ev"] += 1
                    k.store(k.sp, scr["ydisp"][e * CAP + b * 128: e * CAP + (b + 1) * 128, dh * 1024:(dh + 1) * 1024], yo[yi][:], t_yo[yi],
                            scr["t_ydisp"])
    k.barrier()


def phase6(k, cfg, io, scr):
    nc = k.nc
    T = k.track
    CAP = cfg.CAP
    NT = cfg.NOWN // 128
    IDX, WTS, t_idx = scr["IDX"], scr["WTS"], scr["t_idx"]
    with contextlib.ExitStack() as es:
        def sb(name, shape, dt):
            return es.enter_context(nc.sbuf_tensor("s6_" + name, list(shape), dt))

        def V(fn, r=(), w=()):
            k.op(k.dve, fn, r, w)

        def A(fn, r=(), w=()):
            k.op(k.act, fn, r, w)

        fnbc = sb("fnbc", [128, D], F32)
        g2bc = sb("g2bc", [128, D], F32)
        t_c, t_g2 = T(), T()
        k.load(k.sp, fnbc[:], io["final_norm_w"].partition_broadcast(128), t_c)
        y1 = [sb(f"y1{i}", [128, D], F32) for i in range(2)]
        y2 = [sb(f"y2{i}", [128, D], F32) for i in range(2)]
        x1 = [sb(f"x1{i}", [128, D], F32) for i in range(2)]
        t_y1, t_y2, t_x1 = [T(), T()], [T(), T()], [T(), T()]
        acc = sb("acc", [128, D], F32)
        t_acc = T()
        yout = [sb(f"yout{i}", [128, D], F32) for i in range(2)]
        t_yout = [T(), T()]
        junk = sb("junk", [128, D], BF16)
        t_junk = T()
        st = sb("st", [128, 8], F32)
        t_st = T()
        cur_mod = [-1]

        def load_mod(m):
            if cur_mod[0] == m:
                return
            cur_mod[0] = m
            k.load(k.sp, g2bc[:], scr["modrow"][m:m + 1, 5 * D:6 * D].partition_broadcast(128), t_g2, reads=[scr["t_modrow"]])

        def load_tile(i):
            s = i % 2
            g0 = i * 128
            k.idma(out=y1[s][:, :], out_off=None, in_=scr["ydisp"][:, :], in_off=bass.IndirectOffsetOnAxis(ap=IDX[:, i, 0:1], axis=0),
                   sem_track=t_y1[s], reads=[scr["t_ydisp"], t_idx], writes=[t_y1[s]], bounds_check=NE * CAP - 1, oob_is_err=False)
            k.idma(out=y2[s][:, :], out_off=None, in_=scr["ydisp"][:, :], in_off=bass.IndirectOffsetOnAxis(ap=IDX[:, i, 1:2], axis=0),
                   sem_track=t_y2[s], reads=[scr["t_ydisp"], t_idx], writes=[t_y2[s]], bounds_check=NE * CAP - 1, oob_is_err=False)
            k.load(k.sp, x1[s][:], scr["x1s"][g0:g0 + 128, :], t_x1[s], reads=[scr["t_x1s"]])

        load_tile(0)
        for i in range(NT):
            if i + 1 < NT:
                load_tile(i + 1)
            s = i % 2
            g0 = i * 128
            load_mod(0 if g0 < cfg.NPT else 1)
            V(lambda: nc.vector.tensor_scalar(out=acc[:], in0=y1[s][:], scalar1=WTS[:, i, 0:1], scalar2=None, op0=ALU.mult), [t_y1[s], t_idx], [t_acc])
            V(lambda: nc.vector.scalar_tensor_tensor(out=acc[:], in0=y2[s][:], scalar=WTS[:, i, 1:2], in1=acc[:], op0=ALU.mult, op1=ALU.add),
              [t_y2[s], t_idx, t_acc], [t_acc])
            V(lambda: nc.vector.tensor_tensor(out=acc[:], in0=acc[:], in1=g2bc[:], op=ALU.mult), [t_acc, t_g2], [t_acc])
            V(lambda: nc.vector.tensor_tensor(out=acc[:], in0=acc[:], in1=x1[s][:], op=ALU.add), [t_acc, t_x1[s]], [t_acc])
            A(lambda: nc.scalar.activation(out=junk[:], in_=acc[:], func=AF.Square, accum_out=st[:, 0:1]), [t_acc], [t_junk, t_st])
            V(lambda: nc.vector.tensor_scalar(out=st[:, 1:2], in0=st[:, 0:1], scalar1=1.0 / D, scalar2=EPS, op0=ALU.mult, op1=ALU.add), [t_st], [t_st])
            A(lambda: nc.scalar.activation(out=st[:, 2:3], in_=st[:, 1:2], func=AF.Ln), [t_st], [t_st])
            A(lambda: nc.scalar.activation(out=st[:, 3:4], in_=st[:, 2:3], func=AF.Exp, scale=-0.5), [t_st], [t_st])
            V(lambda: nc.vector.scalar_tensor_tensor(out=yout[s][:], in0=acc[:], scalar=st[:, 3:4], in1=fnbc[:], op0=ALU.mult, op1=ALU.mult),
              [t_acc, t_st, t_c], [t_yout[s]])
            k.store(k.sp, io["y"][g0:g0 + 128, :], yout[s][:], t_yout[s], scr["t_out"])
    k.barrier()


def build(cfg):
    nc = bass.Bass("TRN2", target_bir_lowering=False)
    io = declare_io(nc, cfg)
    with contextlib.ExitStack() as es:
        k = KB(nc, es)
        scr = make_scratch(k, cfg)
        phase01(k, cfg, io, scr)
        phase2(k, cfg, io, scr)
        phase3(k, cfg, io, scr)
        phase4(k, cfg, io, scr)
        phase5(k, cfg, io, scr)
        phase6(k, cfg, io, scr)
        k.final_wait()
    return nc, io


def kernel(**inputs):
    inp = {k_: np.asarray(v) for k_, v in inputs.items()}
    cfg = Cfg()
    consts = host_consts()
    nc, io = build(cfg)
    in_names = [n for n in io if not (n.startswith("o_") or n == "y")]
    in_maps = []
    for core in range(8):
        m = prep_core(inp, core, cfg, consts)
        in_maps.append({n: np.ascontiguousarray(m[n], dtype=np.float32) for n in in_names})
    res = run_bass_kernel_spmd(nc, in_maps, core_ids=list(range(8)))
    return assemble(res.results, cfg)


def assemble(results, cfg):
    B, S, DB, DS = 16, 256, 4, 4096
    y_prompt = np.zeros((B, S, D), np.float32)
    y_sample = np.zeros((DB, DS, D), np.float32)
    new_c = np.zeros((B, 1, 2, H, DK, DV), np.float32)
    new_n = np.zeros((B, 1, 2, H, DK), np.float32)
    new_m = np.zeros((B, 1, 2, H), np.float32)
    new_h = np.zeros((B, 1, 2, RW), np.float32)
    for core, r in enumerate(results):
        odd = core % 2
        b = core // 2
        y = np.asarray(r["y"], dtype=np.float32)
        dirs = (1, 0) if odd else (0, 1)
        for j in range(cfg.NP):
            seq = core * cfg.NP + j
            yp = y[j * cfg.TP:(j + 1) * cfg.TP]
            y_prompt[seq] = yp[::-1] if odd else yp
            for ld, gd in enumerate(dirs):
                new_c[seq, 0, gd] = np.asarray(r["o_c"])[j, ld]
                new_n[seq, 0, gd] = np.asarray(r["o_n"])[j, ld].T
                new_m[seq, 0, gd] = np.asarray(r["o_m"])[j, ld][:, 0]
                new_h[seq, 0, gd] = np.asarray(r["o_h"])[:, j, ld, :].T.reshape(-1)
        ys = y[cfg.NPT:cfg.NPT + cfg.TS]
        if odd:
            y_sample[b, cfg.TS:2 * cfg.TS] = ys[::-1]
        else:
            y_sample[b, 0:cfg.TS] = ys
    return (y_prompt, y_sample, new_c, new_n, new_m, new_h)
```

```python
import contextlib
import numpy as np
import concourse.bass as bass
import concourse.mybir as mybir
from concourse.bass_utils import run_bass_kernel_spmd

F32 = mybir.dt.float32
BF16 = mybir.dt.bfloat16
I32 = mybir.dt.int32
U32 = mybir.dt.uint32
AF = mybir.ActivationFunctionType
ALU = mybir.AluOpType
AX = mybir.AxisListType

D = 2048
KD = 16
H = 4
DK = 128
DV = 256
RW = 1024
NG = 8
NE = 32
FF = 1024
EPS = 1e-6
NEG = -30000.0


class Track:
    __slots__ = ("w", "r", "multi", "ds")

    def __init__(self, multi=False):
        self.w = {}
        self.r = {}
        self.multi = multi
        self.ds = {}


class Eng:
    def __init__(self, name, obj, sem, same_wait=True):
        self.name = name
        self.obj = obj
        self.sem = sem
        self.cnt = 0
        self.known = {}
        self.same_wait = same_wait


class DSem:
    def __init__(self, sem):
        self.sem = sem
        self.cnt = 0


class KB:
    def __init__(self, nc, es):
        self.nc = nc
        self.es = es
        self.pe = Eng("pe", nc.tensor, es.enter_context(nc.semaphore("sem_pe")), same_wait=False)
        self.act = Eng("act", nc.scalar, es.enter_context(nc.semaphore("sem_act")))
        self.dve = Eng("dve", nc.vector, es.enter_context(nc.semaphore("sem_dve")))
        self.pool = Eng("pool", nc.gpsimd, es.enter_context(nc.semaphore("sem_pool")))
        self.sp = Eng("sp", nc.sync, None)
        self.engs = [self.pe, self.act, self.dve, self.pool, self.sp]
        self.all_ds = []
        self.bc_regs = {}
        self.free_ds = {"sp": [], "pool": []}
        self.tracks = []
        self.psum = []
        self.pst = []
        for i in range(8):
            t = es.enter_context(nc.psum_tensor(f"psb{i}", [128, 512], F32))
            self.psum.append(t)
            self.pst.append(self.track())
        self.pi = 0

    def track(self, multi=False):
        t = Track(multi)
        self.tracks.append(t)
        return t

    def _ds(self, t, qn):
        if qn not in t.ds:
            if self.free_ds[qn]:
                t.ds[qn] = self.free_ds[qn].pop()
            else:
                d = DSem(self.es.enter_context(self.nc.semaphore(f"dsem_{qn}{len(self.all_ds)}")))
                self.all_ds.append(d)
                t.ds[qn] = d
        return t.ds[qn]

    def bank(self):
        i = self.pi
        self.pi = (self.pi + 1) % 8
        return self.psum[i], self.pst[i]

    def _wait(self, eng, reads, writes):
        deps = {}

        def add(s, v):
            if deps.get(s, 0) < v:
                deps[s] = v

        for t in reads:
            for s, v in t.w.items():
                add(s, v)
        for t in writes:
            if t.multi:
                continue
            for s, v in t.w.items():
                add(s, v)
            for s, v in t.r.items():
                add(s, v)
        for s, v in deps.items():
            if (not eng.same_wait) and eng.sem is not None and s == eng.sem:
                continue
            if eng.known.get(s, 0) < v:
                eng.obj.wait_ge(s, v)
                eng.known[s] = v

    def _mark(self, tok, reads, writes):
        s, v = tok
        for t in writes:
            if t.multi:
                if t.w.get(s, 0) < v:
                    t.w[s] = v
            else:
                t.w = {s: v}
                t.r = {}
        for t in reads:
            if t.r.get(s, 0) < v:
                t.r[s] = v

    def op(self, eng, fn, reads=(), writes=()):
        self._wait(eng, reads, writes)
        ins = fn()
        eng.cnt += 1
        ins.then_inc(eng.sem, 1)
        self._mark((eng.sem, eng.cnt), reads, writes)

    def dma(self, q, out, in_, sem_track, reads=(), writes=(), **kw):
        ds = self._ds(sem_track, q.name)
        self._wait(q, reads, writes)
        ins = q.obj.dma_start(out=out, in_=in_, **kw)
        ds.cnt += 16
        ins.then_inc(ds.sem, 16)
        self._mark((ds.sem, ds.cnt), reads, writes)

    def idma(self, out, out_off, in_, in_off, sem_track, reads=(), writes=(), **kw):
        q = self.pool
        ds = self._ds(sem_track, q.name)
        self._wait(q, reads, writes)
        if "bounds_check" in kw and not hasattr(kw["bounds_check"], "regnum"):
            key = int(kw["bounds_check"])
            if key not in self.bc_regs:
                self.bc_regs[key] = self.nc.gpsimd.to_reg(key)
            kw["bounds_check"] = self.bc_regs[key]
        ins = q.obj.indirect_dma_start(out=out, out_offset=out_off, in_=in_, in_offset=in_off, **kw)
        ds.cnt += 16
        ins.then_inc(ds.sem, 16)
        self._mark((ds.sem, ds.cnt), reads, writes)

    def load(self, q, out, in_, t_dst, reads=(), **kw):
        self.dma(q, out, in_, t_dst, reads=reads, writes=[t_dst], **kw)

    def store(self, q, out, in_, t_src, t_dst, **kw):
        self.dma(q, out, in_, t_src, reads=[t_src], writes=[t_dst], **kw)

    def barrier(self):
        for e in self.engs:
            for e2 in self.engs:
                if e2.sem is not None and e2.cnt > 0 and (e2 is not e or e.same_wait):
                    if e.known.get(e2.sem, 0) < e2.cnt:
                        e.obj.wait_ge(e2.sem, e2.cnt)
                        e.known[e2.sem] = e2.cnt
            for d in self.all_ds:
                if d.cnt > 0 and e.known.get(d.sem, 0) < d.cnt:
                    e.obj.wait_ge(d.sem, d.cnt)
                    e.known[d.sem] = d.cnt
        for t in self.tracks:
            t.w = {}
            t.r = {}
            for qn, d in t.ds.items():
                self.free_ds[qn].append(d)
            t.ds = {}

    def final_wait(self):
        for d in self.all_ds:
            if d.cnt > 0 and self.sp.known.get(d.sem, 0) < d.cnt:
                self.sp.obj.wait_ge(d.sem, d.cnt)
                self.sp.known[d.sem] = d.cnt


class Cfg:
    def __init__(self, NP=2, TP=256, TS=2048, TO=2048, CAP=512, debug=False):
        self.NP, self.TP, self.TS, self.TO, self.CAP, self.debug = NP, TP, TS, TO, CAP, debug
        self.NPT = NP * TP
        self.NOWN = self.NPT + TS
        self.NALL = self.NOWN + TO
        assert self.NPT % 512 == 0 or self.NPT in (256,), self.NPT
        self.blocks = []
        t = 0
        while t < self.NPT:
            n = min(512, self.NPT - t)
            self.blocks.append((t, n, 0))
            t += n
        while t < self.NOWN:
            n = min(512, self.NOWN - t)
            self.blocks.append((t, n, 1))
            t += n
        self.oblocks = []
        while t < self.NALL:
            n = min(512, self.NALL - t)
            self.oblocks.append((t, n, 1))
            t += n


WF = 3072
WT = 2560
WCOLS = WF + WT


def phase01(k, cfg, io, scr):
    nc = k.nc
    T = k.track
    with contextlib.ExitStack() as es:
        def sb(name, shape, dt):
            return es.enter_context(nc.sbuf_tensor("s_" + name, list(shape), dt))

        es0 = contextlib.ExitStack()
        _sb_main = sb

        def sb(name, shape, dt):
            return es0.enter_context(nc.sbuf_tensor("s_" + name, list(shape), dt))
        c2T = sb("c2T", [128, 32], F32)
        c2s = sb("c2s", [128, 32], F32)
        c2b = sb("c2b", [128, 32], BF16)
        t_c2T, t_c2s, t_c2b = T(), T(), T()
        k.load(k.sp, c2T[:], io["c2T"], t_c2T)
        k.op(k.act, lambda: nc.scalar.activation(out=c2s[:], in_=c2T[:], func=AF.Tanh, scale=0.5),
             reads=[t_c2T], writes=[t_c2s])
        k.op(k.dve, lambda: nc.vector.tensor_scalar(out=c2s[:], in0=c2s[:], scalar1=0.5, scalar2=0.5,
                                                     op0=ALU.mult, op1=ALU.add), reads=[t_c2s], writes=[t_c2s])
        k.op(k.dve, lambda: nc.vector.tensor_tensor(out=c2b[:], in0=c2s[:], in1=c2T[:], op=ALU.mult),
             reads=[t_c2s, t_c2T], writes=[t_c2b])
        wa = [sb(f"wa{i}", [128, KD, 512], BF16) for i in range(2)]
        t_wa = [T(), T()]
        brow = [sb(f"brow{i}", [2, 512], F32) for i in range(2)]
        t_brow = [T(), T()]
        mrow = [sb(f"mrow{i}", [2, 512], F32) for i in range(2)]
        t_mrow = [T(), T()]
        t_modrow = scr["t_modrow"]
        w_ada_v = io["w_ada"].rearrange("(k p) c -> p k c", p=128)
        NCT = 6 * D // 512

        def load_wa(ct):
            s = ct % 2
            for hh in range(2):
                k.load(k.pool, wa[s][:, hh * 8:(hh + 1) * 8, :], w_ada_v[:, hh * 8:(hh + 1) * 8, ct * 512:(ct + 1) * 512], t_wa[s])
            k.load(k.sp, brow[s][:], io["b_ada2"][:, ct * 512:(ct + 1) * 512], t_brow[s])

        load_wa(0)
        for ct in range(NCT):
            if ct + 1 < NCT:
                load_wa(ct + 1)
            s = ct % 2
            ps, pt = k.bank()

            def mm(ps=ps, s=s):
                for kk in range(KD):
                    ins = nc.tensor.matmul(ps[0:2, :], lhsT=c2b[:, kk * 2:kk * 2 + 2], rhs=wa[s][:, kk, :],
                                           start=(kk == 0), stop=(kk == KD - 1))
                return ins
            k.op(k.pe, mm, reads=[t_c2b, t_wa[s]], writes=[pt])
            k.op(k.dve, lambda ps=ps, s=s: nc.vector.tensor_tensor(out=mrow[s][:], in0=ps[0:2, :], in1=brow[s][:], op=ALU.add),
                 reads=[pt, t_brow[s]], writes=[t_mrow[s]])
            k.store(k.sp, scr["modrow"][:, ct * 512:(ct + 1) * 512], mrow[s][:], t_mrow[s], t_modrow)

        k.barrier()
        es0.close()
        sb = _sb_main

        NTOK = max(cfg.NOWN, cfg.TO)
        hT = sb("hT", [128, KD, NTOK], BF16)
        t_hT = T()
        n1bc = sb("n1bc", [128, D], F32)
        t_n1 = T()
        k.load(k.sp, n1bc[:], io["norm1_w"].partition_broadcast(128), t_n1)
        g1bc = sb("g1bc", [128, D], F32)
        s1bc = sb("s1bc", [128, D], F32)
        t_g1, t_s1 = T(), T()
        ident = sb("ident_b", [128, 128], BF16)
        t_id = T()
        k.load(k.pool, ident[:], io["ident"], t_id)
        xs = [sb(f"xs{i}", [128, D], F32) for i in range(2)]
        t_xs = [T(), T()]
        junk = sb("junk", [128, D], BF16)
        t_junk = T()
        tmpf = sb("tmpf", [128, D], F32)
        t_tmpf = T()
        hb = [sb(f"hb{i}", [128, D], BF16) for i in range(2)]
        t_hb = [T(), T()]
        st = sb("st1", [128, 8], F32)
        t_st = T()
        cur_mod = [-1]

        def load_mod(m):
            if cur_mod[0] == m:
                return
            cur_mod[0] = m
            k.load(k.sp, g1bc[:], scr["modrow"][m:m + 1, D:2 * D].partition_broadcast(128), t_g1, reads=[t_modrow])
            k.load(k.sp, s1bc[:], scr["modrow"][m:m + 1, 0:D].partition_broadcast(128), t_s1, reads=[t_modrow])
            k.op(k.dve, lambda: nc.vector.scalar_tensor_tensor(out=g1bc[:], in0=g1bc[:], scalar=1.0, in1=n1bc[:],
                                                                op0=ALU.add, op1=ALU.mult), reads=[t_g1, t_n1], writes=[t_g1])

        ntile = [0]

        def norm_tiles(tok0, ntok, m, hoff):
            load_mod(m)
            for i in range(ntok // 128):
                s = ntile[0] % 2
                ntile[0] += 1
                k.load(k.sp, xs[s][:], io["x"][tok0 + i * 128: tok0 + (i + 1) * 128, :], t_xs[s])
                k.op(k.act, lambda s=s: nc.scalar.activation(out=junk[:], in_=xs[s][:], func=AF.Square, accum_out=st[:, 0:1]),
                     reads=[t_xs[s]], writes=[t_junk, t_st])
                k.op(k.dve, lambda: nc.vector.tensor_scalar(out=st[:, 1:2], in0=st[:, 0:1], scalar1=1.0 / D, scalar2=EPS,
                                                             op0=ALU.mult, op1=ALU.add), reads=[t_st], writes=[t_st])
                k.op(k.act, lambda: nc.scalar.activation(out=st[:, 2:3], in_=st[:, 1:2], func=AF.Ln), reads=[t_st], writes=[t_st])
                k.op(k.act, lambda: nc.scalar.activation(out=st[:, 3:4], in_=st[:, 2:3], func=AF.Exp, scale=-0.5), reads=[t_st], writes=[t_st])
                k.op(k.dve, lambda s=s: nc.vector.scalar_tensor_tensor(out=tmpf[:], in0=xs[s][:], scalar=st[:, 3:4], in1=g1bc[:],
                                                                        op0=ALU.mult, op1=ALU.mult),
                     reads=[t_xs[s], t_st, t_g1], writes=[t_tmpf])
                k.op(k.dve, lambda s=s: nc.vector.tensor_tensor(out=hb[s][:], in0=tmpf[:], in1=s1bc[:], op=ALU.add),
                     reads=[t_tmpf, t_s1], writes=[t_hb[s]])
                for half in range(2):
                    ps, pt = k.bank()
                    psb = ps[:].bitcast(BF16)

                    def tr(psb=psb, s=s, half=half):
                        for j in range(8):
                            kk = half * 8 + j
                            ins = nc.tensor.transpose(psb[:, j * 128:(j + 1) * 128], hb[s][:, kk * 128:(kk + 1) * 128], ident[:])
                        return ins
                    k.op(k.pe, tr, reads=[t_hb[s], t_id], writes=[pt])
                    dst = hT[:, half * 8:(half + 1) * 8, hoff + i * 128: hoff + (i + 1) * 128]
                    src = psb.rearrange("p (j t) -> p j t", j=8)
                    if half == 0:
                        k.op(k.act, lambda dst=dst, src=src: nc.scalar.copy(out=dst, in_=src), reads=[pt], writes=[t_hT])
                    else:
                        k.op(k.dve, lambda dst=dst, src=src: nc.vector.tensor_copy(out=dst, in_=src), reads=[pt], writes=[t_hT])

        wt = [sb(f"wt{i}", [128, KD, 512], BF16) for i in range(2)]
        t_wt = [T(), T()]
        wg = sb("wgate", [128, KD, 16], BF16)
        t_wg = T()
        bg = sb("bgate", [8, 2], F32)
        t_bg = T()
        k.load(k.pool, wg[:], io["w_gates"].rearrange("(k p) c -> p k c", p=128), t_wg)
        k.load(k.sp, bg[:], io["b_gates"], t_bg)
        ev = [sb(f"ev{i}", [128, 512], BF16) for i in range(4)]
        t_ev = [T() for _ in range(4)]
        evg = [sb(f"evg{i}", [8, 512], F32) for i in range(2)]
        t_evg = [T(), T()]
        w_in_v = io["w_in"].rearrange("(k p) c -> p k c", p=128)
        wcnt = [0]
        evc = [0]

        def load_w(c0):
            s = wcnt[0] % 2
            wcnt[0] += 1
            for hh in range(2):
                k.load(k.pool, wt[s][:, hh * 8:(hh + 1) * 8, :], w_in_v[:, hh * 8:(hh + 1) * 8, c0:c0 + 512], t_wt[s])
            return s

        def evac_n(ps, pt, dram_dst, t_dst, scale, n):
            i = evc[0] % 4
            evc[0] += 1
            if i % 2 == 0:
                if scale is None:
                    k.op(k.act, lambda: nc.scalar.copy(out=ev[i][:, 0:n], in_=ps[:, 0:n]), reads=[pt], writes=[t_ev[i]])
                else:
                    k.op(k.act, lambda: nc.scalar.mul(out=ev[i][:, 0:n], in_=ps[:, 0:n], mul=scale), reads=[pt], writes=[t_ev[i]])
            else:
                if scale is None:
                    k.op(k.dve, lambda: nc.vector.tensor_copy(out=ev[i][:, 0:n], in_=ps[:, 0:n]), reads=[pt], writes=[t_ev[i]])
                else:
                    k.op(k.dve, lambda: nc.vector.tensor_scalar(out=ev[i][:, 0:n], in0=ps[:, 0:n], scalar1=scale, scalar2=None,
                                                                 op0=ALU.mult), reads=[pt], writes=[t_ev[i]])
            k.store(k.sp, dram_dst, ev[i][:, 0:n], t_ev[i], t_dst)

        def project(blocks, hoff0, ftiles, ttiles, zF, zT, gLI, gFP, t_z, qscale):
            for (tok0, n, m) in blocks:
                ho = tok0 - hoff0
                for gi, gdst in enumerate((gLI, gFP)):
                    ps, pt = k.bank()

                    def mm(ps=ps, gi=gi, ho=ho, n=n):
                        for kk in range(KD):
                            ins = nc.tensor.matmul(ps[0:8, 0:n], lhsT=wg[:, kk, gi * 8:(gi + 1) * 8], rhs=hT[:, kk, ho:ho + n],
                                                   start=(kk == 0), stop=(kk == KD - 1))
                        return ins
                    k.op(k.pe, mm, reads=[t_wg, t_hT], writes=[pt])
                    k.op(k.act, lambda ps=ps, gi=gi, n=n: nc.scalar.activation(out=evg[gi][:, 0:n], in_=ps[0:8, 0:n], func=AF.Identity,
                                                                               bias=bg[:, gi:gi + 1]),
                         reads=[pt, t_bg], writes=[t_evg[gi]])
                    k.store(k.sp, gdst[:, ho:ho + n], evg[gi][:, 0:n], t_evg[gi], t_z)
            tiles = [("F", c) for c in ftiles] + [("T", c) for c in ttiles]
            nxt = load_w(tiles[0][1][0])
            for ti, (kind, (c0, zc0)) in enumerate(tiles):
                s = nxt
                if ti + 1 < len(tiles):
                    nxt = load_w(tiles[ti + 1][1][0])
                if kind == "F":
                    for (tok0, n, m) in blocks:
                        ho = tok0 - hoff0
                        for sub in range(4):
                            ps, pt = k.bank()

                            def mm(ps=ps, s=s, sub=sub, ho=ho, n=n):
                                for kk in range(KD):
                                    ins = nc.tensor.matmul(ps[:, 0:n], lhsT=wt[s][:, kk, sub * 128:(sub + 1) * 128], rhs=hT[:, kk, ho:ho + n],
                                                           start=(kk == 0), stop=(kk == KD - 1))
                                return ins
                            k.op(k.pe, mm, reads=[t_wt[s], t_hT], writes=[pt])
                            row0 = zc0 + sub * 128
                            scale = DK ** -0.5 if (qscale and row0 < 512) else None
                            evac_n(ps, pt, zF[row0:row0 + 128, ho:ho + n], t_z, scale, n)
                else:
                    for (tok0, n, m) in blocks:
                        ho = tok0 - hoff0
                        for tt in range(n // 128):
                            ps, pt = k.bank()

                            def mm(ps=ps, s=s, tt=tt, ho=ho):
                                for kk in range(KD):
                                    ins = nc.tensor.matmul(ps[:, :], lhsT=hT[:, kk, ho + tt * 128: ho + (tt + 1) * 128], rhs=wt[s][:, kk, :],
                                                           start=(kk == 0), stop=(kk == KD - 1))
                                return ins
                            k.op(k.pe, mm, reads=[t_wt[s], t_hT], writes=[pt])
                            evac_n(ps, pt, zT[ho + tt * 128: ho + (tt + 1) * 128, zc0:zc0 + 512], t_z, None, 512)

        for (tok0, n, m) in cfg.blocks:
            norm_tiles(tok0, n, m, tok0)
        ftiles = [(c, c) for c in range(0, WF, 512)]
        ttiles = [(WF + c, c) for c in range(0, WT, 512)]
        project(cfg.blocks, 0, ftiles, ttiles, scr["zF"], scr["zT"], scr["gLI"], scr["gFP"], scr["t_z"], True)
        if cfg.TO:
            for (tok0, n, m) in cfg.oblocks:
                norm_tiles(tok0, n, m, tok0 - cfg.NOWN)
            ftiles_o = [(1024 + c, c) for c in range(0, 1024, 512)]
            ttiles_o = [(WF + c, c) for c in range(0, 1024, 512)] + [(WF + 2048, 1024)]
            project(cfg.oblocks, cfg.NOWN, ftiles_o, ttiles_o, scr["zFo"], scr["zTo"], scr["gLIo"], scr["gFPo"], scr["t_zo"], False)
    k.barrier()


def make_scratch(k, cfg):
    nc = k.nc
    scr = {}

    def dt(name, shape, dtype):
        scr[name] = nc.dram_tensor(name, list(shape), dtype, kind="Internal").ap()
    dt("modrow", [2, 6 * D], F32)
    dt("zF", [WF, cfg.NOWN], BF16)
    dt("zT", [cfg.NOWN, WT], BF16)
    dt("gLI", [8, cfg.NOWN], F32)
    dt("gFP", [8, cfg.NOWN], F32)
    if cfg.TO:
        dt("zFo", [1024, cfg.TO], BF16)
        dt("zTo", [cfg.TO, 1536], BF16)
        dt("gLIo", [8, cfg.TO], F32)
        dt("gFPo", [8, cfg.TO], F32)
    dt("hF", [cfg.NOWN, 1024], F32)
    dt("ymixT", [2048, cfg.NOWN], BF16)
    dt("x1s", [cfg.NOWN, D], F32)
    dt("xdisp", [NE * cfg.CAP, D], BF16)
    dt("ydisp", [NE * cfg.CAP, D], F32)
    for n in ("t_modrow", "t_z", "t_zo", "t_hF", "t_ymix", "t_out", "t_x1s", "t_xdisp", "t_ydisp"):
        scr[n] = k.track(multi=True)
    NT = cfg.NOWN // 128
    scr["IDX"] = k.es.enter_context(nc.sbuf_tensor("s_IDX", [128, NT, 2], I32))
    scr["WTS"] = k.es.enter_context(nc.sbuf_tensor("s_WTS", [128, NT, 2], F32))
    scr["t_idx"] = k.track()
    return scr


def host_consts():
    import ml_dtypes
    c = {}
    c["ident"] = np.eye(128, dtype=np.float32)
    s = np.arange(128)[:, None]
    t = np.arange(128)[None, :]
    mf = np.where(s <= t, 0.0, NEG).astype(np.float32)
    mb = np.where(s >= t, 0.0, NEG).astype(np.float32)
    c["maskneg"] = np.ascontiguousarray(np.stack([np.broadcast_to(mf[:, None, :], (128, 4, 128)),
                                                  np.broadcast_to(mb[:, None, :], (128, 4, 128))], 0))
    sel = np.zeros((4, 4, 128), np.float32)
    for h in range(4):
        sel[h, h, :] = 1.0
    c["sel"] = sel
    c["eye4"] = np.eye(4, dtype=np.float32)
    for cap in (128, 256, 384, 512):
        c["ebase_%d" % cap] = (np.arange(NE) * cap).astype(np.float32)
    c["ltri"] = (s < t).astype(np.float32)
    tt = np.arange(2048)
    c["mS"] = np.ascontiguousarray(np.broadcast_to(np.where(tt % 128 == 0, 0.0, 1.0).astype(np.float32), (4, 2048)))
    c["mE"] = np.ascontiguousarray(np.broadcast_to(np.where(tt % 128 == 127, 0.0, 1.0).astype(np.float32), (4, 2048)))
    c["nS"] = np.ascontiguousarray(np.broadcast_to(np.where(tt % 128 == 0, -1e30, 0.0).astype(np.float32), (4, 2048)))
    c["nE"] = np.ascontiguousarray(np.broadcast_to(np.where(tt % 128 == 127, -1e30, 0.0).astype(np.float32), (4, 2048)))
    return c


def prep_core(inp, core, cfg, consts):
    b = core // 2
    odd = core % 2
    NP, TP, TS, TO = cfg.NP, cfg.TP, cfg.TS, cfg.TO
    xs = inp["x_sample"][b]
    if not odd:
        own = xs[0:TS]
        oth = xs[TS:TS + TO]
    else:
        LT = TS + TO
        full = xs[0:LT][::-1]
        own = full[0:TS]
        oth = full[TS:LT]
    pr = []
    for j in range(NP):
        p = inp["x_prompt"][core * NP + j]
        pr.append(p[::-1] if odd else p)
    x = np.ascontiguousarray(np.concatenate(pr + [own, oth], axis=0), dtype=np.float32)
    m = {"x": x}
    c2 = np.stack([inp["c_ctx"], inp["c"][b]], axis=0)
    m["c2T"] = np.ascontiguousarray(c2.reshape(2, KD, 128).transpose(2, 1, 0).reshape(128, 2 * KD))
    m["w_ada"] = inp["w_ada"][0]
    m["b_ada2"] = np.ascontiguousarray(np.broadcast_to(inp["b_ada"][0][None, :], (2, 6 * D)))
    m["norm1_w"] = inp["norm1_w"][0]
    w = inp["w_in"][0]
    q, kk, v, o = w[:, 0:512], w[:, 512:1024], w[:, 1024:2048], w[:, 2048:3072]
    g = w[:, 3072:3088]
    xr, xg = w[:, 3088:4112], w[:, 4112:5136]
    m["w_in"] = np.ascontiguousarray(np.concatenate([q, kk, xr, xg, v, o, kk], axis=1))
    d0, d1 = (1, 0) if odd else (0, 1)
    gi = [g[:, 8 * d0:8 * d0 + 4], g[:, 8 * d1:8 * d1 + 4]]
    gf = [g[:, 8 * d0 + 4:8 * d0 + 8], g[:, 8 * d1 + 4:8 * d1 + 8]]
    m["w_gates"] = np.ascontiguousarray(np.concatenate(gi + gf, axis=1))
    bgt = inp["b_gates"][0]
    bi = np.concatenate([bgt[8 * d0:8 * d0 + 4], bgt[8 * d1:8 * d1 + 4]])
    bf = np.concatenate([bgt[8 * d0 + 4:8 * d0 + 8], bgt[8 * d1 + 4:8 * d1 + 8]])
    m["b_gates"] = np.ascontiguousarray(np.stack([bi, bf], axis=1))
    for n in ("ident", "maskneg", "sel", "eye4", "mS", "mE", "nS", "nE"):
        m[n] = consts[n]
    m["mlstm_norm_w"] = inp["mlstm_norm_w"][0]
    dirs = (1, 0) if odd else (0, 1)
    m["st_c"] = np.ascontiguousarray(np.stack([inp["state_mlstm_c"][b, 0, dd] for dd in dirs], 0))
    m["st_n"] = np.ascontiguousarray(np.stack([inp["state_mlstm_n"][b, 0, dd].T for dd in dirs], 0))
    m["st_m"] = np.ascontiguousarray(np.stack([inp["state_mlstm_m"][b, 0, dd] for dd in dirs], 0)[:, :, None])
    def pg(v):
        return v.reshape(NG, 128).T
    wa, wx = inp["rg_wa"][0], inp["rg_wx"][0]
    m["rg_w"] = np.ascontiguousarray(np.stack([np.stack([wa[dd], wx[dd]], 0) for dd in dirs], 0).transpose(3, 0, 1, 2, 4))
    m["rg_b"] = np.ascontiguousarray(np.stack([np.stack([pg(inp["rg_ba"][0, dd]), pg(inp["rg_bx"][0, dd])], 0) for dd in dirs], 0).transpose(2, 0, 1, 3))
    m["rg_lam"] = np.ascontiguousarray(np.stack([pg(inp["rg_lambda"][0, dd]) for dd in dirs], 0).transpose(1, 0, 2))
    m["st_h"] = np.ascontiguousarray(np.stack([pg(inp["state_rglru_h"][b, 0, dd]) for dd in dirs], 0).transpose(1, 0, 2))
    cwt = inp["conv_w"][0]
    z1 = np.zeros_like(cwt[0])
    taps = [z1, cwt[3], cwt[2], cwt[1], cwt[0]] if odd else [cwt[0], cwt[1], cwt[2], cwt[3], z1]
    m["conv_w5"] = np.ascontiguousarray(np.stack([pg(t) for t in taps], 2))
    m["conv_b"] = np.ascontiguousarray(pg(inp["conv_b"][0]))
    m["w_out"] = inp["w_out"][0]
    m["norm2_w"] = inp["norm2_w"][0]
    m["final_norm_w"] = inp["final_norm_w"]
    rew = inp["router_expert_w"][0]
    m["w_router"] = np.ascontiguousarray(np.concatenate([inp["router_group_w"][0]] + [rew[g_] for g_ in range(4)], axis=1))
    m["b_router"] = np.ascontiguousarray(np.concatenate([inp["router_group_b"][0], inp["router_expert_b"][0].reshape(-1)]))
    m["ebase"] = consts["ebase_%d" % cfg.CAP]
    m["ltri"] = consts["ltri"]
    m["ew_gate"] = inp["expert_w_gate"][0]
    m["ew_up"] = inp["expert_w_up"][0]
    m["ew_down"] = inp["expert_w_down"][0]
    return m


def declare_io(nc, cfg):
    io = {}

    def inp(name, shape, dt=F32):
        io[name] = nc.dram_tensor(name, list(shape), dt, kind="ExternalInput").ap()
    inp("x", [cfg.NALL, D])
    inp("c2T", [128, 2 * KD])
    inp("w_ada", [D, 6 * D])
    inp("b_ada2", [2, 6 * D])
    inp("norm1_w", [D])
    inp("w_in", [D, WCOLS])
    inp("w_gates", [D, 16])
    inp("b_gates", [8, 2])
    inp("ident", [128, 128])
    inp("maskneg", [2, 128, 4, 128])
    inp("sel", [4, 4, 128])
    inp("eye4", [4, 4])
    for n in ("mS", "mE", "nS", "nE"):
        inp(n, [4, 2048])
    inp("mlstm_norm_w", [1024])
    inp("st_c", [2, 4, 128, 256])
    inp("st_n", [2, 128, 4])
    inp("st_m", [2, 4, 1])
    inp("rg_w", [128, 2, 2, NG, 128])
    inp("rg_b", [128, 2, 2, NG])
    inp("rg_lam", [128, 2, NG])
    inp("st_h", [128, 2, NG])
    inp("conv_w5", [128, NG, 5])
    inp("conv_b", [128, NG])
    inp("w_out", [D, D])
    inp("norm2_w", [D])
    inp("final_norm_w", [D])
    inp("w_router", [D, 36])
    inp("b_router", [36])
    inp("ebase", [NE])
    inp("ltri", [128, 128])
    inp("ew_gate", [NE, D, FF])
    inp("ew_up", [NE, D, FF])
    inp("ew_down", [NE, FF, D])

    def outp(name, shape, dt=F32):
        io[name] = nc.dram_tensor(name, list(shape), dt, kind="ExternalOutput").ap()
    outp("o_c", [cfg.NP, 2, 4, 128, 256])
    outp("o_n", [cfg.NP, 2, 128, 4])
    outp("o_m", [cfg.NP, 2, 4, 1])
    outp("o_h", [128, cfg.NP, 2, NG])
    outp("y", [cfg.NOWN, D])
    outp("o_cnt", [128, NE])
    return io


def seg_list(cfg):
    segs = []
    for j in range(cfg.NP):
        segs.append(("prompt", j * cfg.TP, cfg.TP, j))
    segs.append(("own", cfg.NPT, cfg.TS, 0))
    return segs


def phase2(k, cfg, io, scr):
    nc = k.nc
    T = k.track
    L = 128
    with contextlib.ExitStack() as es:
        def sb(name, shape, dt):
            return es.enter_context(nc.sbuf_tensor("s2_" + name, list(shape), dt))

        def V(fn, r=(), w=()):
            k.op(k.dve, fn, r, w)

        def A(fn, r=(), w=()):
            k.op(k.act, fn, r, w)

        def P(fn, r=(), w=()):
            k.op(k.pe, fn, r, w)

        TMAX = max(cfg.TP, cfg.TS, cfg.TO)
        NCMAX = TMAX // L
        identB = sb("identB", [128, 128], BF16)
        identF = sb("identF", [128, 128], F32)
        maskneg = [sb(f"maskneg{d}", [128, 4, 128], BF16) for d in range(2)]
        sel = sb("sel", [4, 4, 128], F32)
        eye4 = sb("eye4", [4, 4], F32)
        ones4 = sb("ones4", [4, 128], F32)
        onescol = sb("onescol", [128, 1], BF16)
        mS = sb("mS", [4, TMAX], F32)
        mE = sb("mE", [4, TMAX], F32)
        nS = sb("nS", [4, TMAX], F32)
        nE = sb("nE", [4, TMAX], F32)
        nwbc = sb("nwbc", [128, 1024], F32)
        t_c = T()
        k.load(k.pool, identB[:], io["ident"], t_c)
        k.load(k.sp, identF[:], io["ident"], t_c)
        for d in range(2):
            k.load(k.pool, maskneg[d][:], io["maskneg"][d], t_c)
        k.load(k.sp, sel[:], io["sel"], t_c)
        k.load(k.sp, eye4[:], io["eye4"], t_c)
        k.load(k.sp, mS[:], io["mS"][:, 0:TMAX], t_c)
        k.load(k.sp, mE[:], io["mE"][:, 0:TMAX], t_c)
        k.load(k.sp, nS[:], io["nS"][:, 0:TMAX], t_c)
        k.load(k.sp, nE[:], io["nE"][:, 0:TMAX], t_c)
        k.load(k.sp, nwbc[:], io["mlstm_norm_w"].partition_broadcast(128), t_c)
        V(lambda: nc.vector.memset(ones4[:], 1.0), w=[t_c])
        V(lambda: nc.vector.memset(onescol[:], 1.0), w=[t_c])

        R = [sb(f"R{i}", [4, TMAX], F32) for i in range(8)]
        t_R = [T() for _ in range(8)]
        cr = sb("cr", [4, 8, NCMAX], F32)
        t_cr = T()
        AD = sb("AD", [4, NCMAX, 4], F32)
        t_AD = T()
        m0 = sb("m0", [4, 2], F32)
        t_m0 = T()
        CX = [sb(f"CX{h}", [128, 256], F32) for h in range(H)]
        nst = sb("nst", [128, 4], F32)
        Cb = sb("Cb", [128, 4, 257], BF16)
        t_CX, t_n, t_Cb = T(), T(), T()
        qT = [sb(f"qT{i}", [128, 4, 128], BF16) for i in range(2)]
        kT = [sb(f"kT{i}", [128, 4, 128], BF16) for i in range(2)]
        vx = [sb(f"vx{i}", [128, 4, 256], BF16) for i in range(2)]
        kt = [sb(f"kt{i}", [128, 4, 128], BF16) for i in range(2)]
        ot = [sb(f"ot{i}", [128, 1024], BF16) for i in range(2)]
        hfl = [sb(f"hfl{i}", [128, 1024], F32) for i in range(2)]
        t_qT, t_kT, t_vx, t_kt, t_ot, t_hfl = ([T(), T()] for _ in range(6))
        class Two:
            def __init__(self, name, shape, dt):
                self.t = [sb(f"{name}_{i}", shape, dt) for i in range(2)]
                self.tr = [T(), T()]
        wsel = [0]
        _AH, _DT, _ST = Two("AH", [4, 4, 128], F32), Two("DT", [128, 512], F32), Two("ST", [128, 512], BF16)
        _QS, _KW = Two("QS", [128, 4, 128], BF16), Two("KW", [128, 4, 128], BF16)
        _COL, _RD = Two("COL", [128, 12], F32), Two("RD", [128, 8], F32)
        _HS, _SG, _YA = Two("HS", [128, 1024], F32), Two("SG", [128, 1024], F32), Two("YA", [128, 1024], BF16)
        _JK, _STT = Two("junk", [128, 256], F32), Two("st", [128, 12], F32)
        HF = [sb(f"HF{i}", [128, 1024], F32) for i in range(2)]
        t_HF = [T(), T()]
        YT = [sb(f"YT{i}", [128, 8, 128], BF16) for i in range(2)]
        t_YT = [T(), T()]
        slot = [0]

        def prep(gLI, gFP, g0, Tn, d, t_gz):
            ncn = Tn // L
            LI, FPr, TMP, B, MP, CBr, CLr, WKr = [r[:, 0:Tn] for r in R]
            tLI, tFP, tTMP, tB, tMP, tCB, tCL, tWK = t_R
            k.load(k.sp, LI, gLI[4 * d:4 * d + 4, g0:g0 + Tn], tLI, reads=[t_gz])
            k.load(k.sp, FPr, gFP[4 * d:4 * d + 4, g0:g0 + Tn], tFP, reads=[t_gz])
            rv = (lambda ap: ap[:, ::-1]) if d == 1 else (lambda ap: ap)
            A(lambda: nc.scalar.activation(out=TMP, in_=FPr, func=AF.Abs), [tFP], [tTMP])
            A(lambda: nc.scalar.activation(out=TMP, in_=TMP, func=AF.Exp, scale=-1.0), [tTMP], [tTMP])
            V(lambda: nc.vector.tensor_scalar_add(out=TMP, in0=TMP, scalar1=1.0), [tTMP], [tTMP])
            A(lambda: nc.scalar.activation(out=TMP, in_=TMP, func=AF.Ln), [tTMP], [tTMP])
            V(lambda: nc.vector.scalar_tensor_tensor(out=FPr, in0=FPr, scalar=0.0, in1=TMP, op0=ALU.min, op1=ALU.subtract),
              [tFP, tTMP], [tFP])
            msk = (mE if d == 1 else mS)[:, 0:Tn]
            nmk = (nE if d == 1 else nS)[:, 0:Tn]
            V(lambda: nc.vector.tensor_tensor_scan(out=rv(B), data0=rv(msk), data1=rv(FPr), initial=0.0, op0=ALU.mult, op1=ALU.add),
              [tFP, t_c], [tB])
            V(lambda: nc.vector.tensor_tensor(out=LI, in0=LI, in1=B, op=ALU.subtract), [tLI, tB], [tLI])
            V(lambda: nc.vector.tensor_tensor_scan(out=rv(MP), data0=rv(nmk), data1=rv(LI), initial=-1e30, op0=ALU.add, op1=ALU.max),
              [tLI, t_c], [tMP])
            e0 = 0 if d == 1 else L - 1
            bend = B[:, e0::L]
            mpend = MP[:, e0::L]
            c_mA, c_min, c_Mend, c_al, c_tmp = (cr[:, i, 0:ncn] for i in (2, 3, 4, 5, 6))
            V(lambda: nc.vector.tensor_tensor_scan(out=rv(c_mA), data0=rv(mpend), data1=rv(bend), initial=m0[:, d:d + 1],
                                                   op0=ALU.max, op1=ALU.add), [tMP, tB, t_m0], [t_cr])
            if d == 0:
                V(lambda: nc.vector.tensor_copy(out=c_min[:, 0:1], in_=m0[:, 0:1]), [t_m0, t_cr], [t_cr])
                if ncn > 1:
                    V(lambda: nc.vector.tensor_copy(out=c_min[:, 1:ncn], in_=c_mA[:, 0:ncn - 1]), [t_cr], [t_cr])
            else:
                V(lambda: nc.vector.tensor_copy(out=c_min[:, ncn - 1:ncn], in_=m0[:, 1:2]), [t_m0, t_cr], [t_cr])
                if ncn > 1:
                    V(lambda: nc.vector.tensor_copy(out=c_min[:, 0:ncn - 1], in_=c_mA[:, 1:ncn]), [t_cr], [t_cr])
            V(lambda: nc.vector.tensor_tensor(out=c_Mend, in0=c_min, in1=mpend, op=ALU.max), [t_cr, tMP], [t_cr])
            V(lambda: nc.vector.tensor_tensor(out=c_tmp, in0=c_min, in1=c_Mend, op=ALU.subtract), [t_cr], [t_cr])
            A(lambda: nc.scalar.activation(out=c_al, in_=c_tmp, func=AF.Exp), [t_cr], [t_cr])
            V(lambda: nc.vector.tensor_tensor(out=AD[:, 0:ncn, :], in0=c_al.unsqueeze(2).to_broadcast([4, ncn, 4]),
                                              in1=eye4[:].unsqueeze(1).to_broadcast([4, ncn, 4]), op=ALU.mult), [t_cr, t_c], [t_AD])
            v3 = lambda ap: ap.rearrange("p (c l) -> p c l", l=L)
            bc = lambda ap: ap.unsqueeze(2).to_broadcast([4, ncn, L])
            V(lambda: nc.vector.tensor_tensor(out=v3(MP), in0=v3(MP), in1=bc(c_min), op=ALU.max), [tMP, t_cr], [tMP])
            V(lambda: nc.vector.tensor_tensor(out=v3(CBr), in0=bc(c_min), in1=v3(MP), op=ALU.subtract), [tMP, t_cr], [tCB])
            A(lambda: nc.scalar.activation(out=CBr, in_=CBr, func=AF.Exp), [tCB], [tCB])
            V(lambda: nc.vector.tensor_tensor(out=CLr, in0=B, in1=MP, op=ALU.add), [tB, tMP], [tCL])
            A(lambda: nc.scalar.activation(out=CLr, in_=CLr, func=AF.Exp, scale=-1.0), [tCL], [tCL])
            V(lambda: nc.vector.tensor_tensor(out=v3(WKr), in0=v3(LI), in1=bc(c_Mend), op=ALU.subtract), [tLI, t_cr], [tWK])
            A(lambda: nc.scalar.activation(out=WKr, in_=WKr, func=AF.Exp), [tWK], [tWK])
            V(lambda: nc.vector.tensor_scalar(out=MP, in0=MP, scalar1=-1.0, scalar2=None, op0=ALU.mult), [tMP], [tMP])

        def chunk_cols(t0, c):
            w_ = wsel[0]
            AH, DT, ST, QS, KW, COL, RD, HS, SG, YA, junk, st = (x.t[w_] for x in (_AH, _DT, _ST, _QS, _KW, _COL, _RD, _HS, _SG, _YA, _JK, _STT))
            t_AH, t_DT, t_ST, t_QS, t_KW, t_COL, t_RD, t_HS, t_SG, t_YA, t_junk, t_st = (x.tr[w_] for x in (_AH, _DT, _ST, _QS, _KW, _COL, _RD, _HS, _SG, _YA, _JK, _STT))
            ps, pt = k.bank()
            WKr, CLr = R[7], R[6]

            def f():
                nc.tensor.transpose(ps[:, 0:4], WKr[:, t0:t0 + L], identF[0:4, 0:4])
                nc.tensor.transpose(ps[:, 4:8], CLr[:, t0:t0 + L], identF[0:4, 0:4])
                return nc.tensor.matmul(ps[:, 8:12], lhsT=ones4[:], rhs=AD[:, c, :], start=True, stop=True)
            P(f, [t_R[7], t_R[6], t_AD, t_c], [pt])
            V(lambda: nc.vector.tensor_copy(out=COL[:], in_=ps[:, 0:12]), [pt], [t_COL])

        def state_pre(s, c):
            w_ = wsel[0]
            AH, DT, ST, QS, KW, COL, RD, HS, SG, YA, junk, st = (x.t[w_] for x in (_AH, _DT, _ST, _QS, _KW, _COL, _RD, _HS, _SG, _YA, _JK, _STT))
            t_AH, t_DT, t_ST, t_QS, t_KW, t_COL, t_RD, t_HS, t_SG, t_YA, t_junk, t_st = (x.tr[w_] for x in (_AH, _DT, _ST, _QS, _KW, _COL, _RD, _HS, _SG, _YA, _JK, _STT))
            V(lambda: nc.vector.tensor_tensor(out=KW[:], in0=kt[s][:], in1=COL[:, 0:4].unsqueeze(2).to_broadcast([128, 4, 128]), op=ALU.mult),
              [t_kt[s], t_COL], [t_KW])

        def state_post(s, c):
            w_ = wsel[0]
            AH, DT, ST, QS, KW, COL, RD, HS, SG, YA, junk, st = (x.t[w_] for x in (_AH, _DT, _ST, _QS, _KW, _COL, _RD, _HS, _SG, _YA, _JK, _STT))
            t_AH, t_DT, t_ST, t_QS, t_KW, t_COL, t_RD, t_HS, t_SG, t_YA, t_junk, t_st = (x.tr[w_] for x in (_AH, _DT, _ST, _QS, _KW, _COL, _RD, _HS, _SG, _YA, _JK, _STT))
            banks = [k.bank(), k.bank(), k.bank()]

            def f():
                for h in range(H):
                    ps = banks[h // 2][0]
                    nc.tensor.matmul(ps[:, (h % 2) * 256:(h % 2 + 1) * 256], lhsT=KW[:, h, :], rhs=vx[s][:, h, :], start=True, stop=True)
                for h in range(H):
                    ins = nc.tensor.matmul(banks[2][0][:, h:h + 1], lhsT=KW[:, h, :], rhs=onescol[:], start=True, stop=True)
                return ins
            P(f, [t_KW, t_vx[s], t_c], [b[1] for b in banks])
            for h in range(H):
                ps = banks[h // 2][0]
                V(lambda h=h, ps=ps: nc.vector.scalar_tensor_tensor(out=CX[h][:], in0=CX[h][:], scalar=COL[:, 8 + h:9 + h],
                                                                    in1=ps[:, (h % 2) * 256:(h % 2 + 1) * 256], op0=ALU.mult, op1=ALU.add),
                  [t_CX, t_COL, banks[h // 2][1]], [t_CX])
            V(lambda: nc.vector.tensor_tensor(out=nst[:], in0=nst[:], in1=COL[:, 8:12], op=ALU.mult), [t_n, t_COL], [t_n])
            V(lambda: nc.vector.tensor_tensor(out=nst[:], in0=nst[:], in1=banks[2][0][:, 0:4], op=ALU.add), [t_n, banks[2][1]], [t_n])

        def refresh_Cb():
            for h in range(H):
                A(lambda h=h: nc.scalar.copy(out=Cb[:, h, 0:256], in_=CX[h][:]), [t_CX], [t_Cb])
            V(lambda: nc.vector.tensor_copy(out=Cb[:, :, 256], in_=nst[:]), [t_n], [t_Cb])

        def load_chunk(zT, zF, g0, full, t_gz, with_o, hF_src):
            s = slot[0] % 2
            slot[0] += 1
            k.load(k.sp, vx[s][:], zT[g0:g0 + L, 0:1024].rearrange("t (h v) -> t h v", h=4), t_vx[s], reads=[t_gz])
            kcol = 2048 if full else 1024
            k.load(k.sp, kt[s][:], zT[g0:g0 + L, kcol:kcol + 512].rearrange("t (h v) -> t h v", h=4), t_kt[s], reads=[t_gz])
            if full:
                k.load(k.sp, qT[s][:], zF[0:512, g0:g0 + L].rearrange("(h p) t -> p h t", p=128), t_qT[s], reads=[t_gz])
                k.load(k.sp, kT[s][:], zF[512:1024, g0:g0 + L].rearrange("(h p) t -> p h t", p=128), t_kT[s], reads=[t_gz])
            if with_o:
                k.load(k.sp, ot[s][:], zT[g0:g0 + L, 1024:2048], t_ot[s], reads=[t_gz])
                k.load(k.sp, hfl[s][:], hF_src, t_hfl[s], reads=[scr["t_hF"]])
            return s

        def full_pre(s, t0, g0, c, d, last_dir):
            w_ = wsel[0]
            AH, DT, ST, QS, KW, COL, RD, HS, SG, YA, junk, st = (x.t[w_] for x in (_AH, _DT, _ST, _QS, _KW, _COL, _RD, _HS, _SG, _YA, _JK, _STT))
            t_AH, t_DT, t_ST, t_QS, t_KW, t_COL, t_RD, t_HS, t_SG, t_YA, t_junk, t_st = (x.tr[w_] for x in (_AH, _DT, _ST, _QS, _KW, _COL, _RD, _HS, _SG, _YA, _JK, _STT))
            MPn, CBr = R[4], R[5]
            V(lambda: nc.vector.tensor_tensor(out=AH[:], in0=R[0][:, t0:t0 + L].unsqueeze(1).to_broadcast([4, 4, L]),
                                              in1=eye4[:].unsqueeze(2).to_broadcast([4, 4, L]), op=ALU.mult), [t_R[0], t_c], [t_AH])
            pE, tE = k.bank()

            def fE():
                nc.tensor.matmul(pE[:, :], lhsT=identB[:], rhs=maskneg[d][:].rearrange("p h t -> p (h t)"), start=True, stop=False)
                for h in range(H):
                    nc.tensor.matmul(pE[:, h * L:(h + 1) * L], lhsT=sel[:, h, :], rhs=MPn[:, t0:t0 + L], start=False, stop=False)
                    ins = nc.tensor.matmul(pE[:, h * L:(h + 1) * L], lhsT=AH[:, h, :], rhs=ones4[:], start=False, stop=(h == H - 1))
                return ins
            P(fE, [t_c, t_R[4], t_AH], [tE])
            A(lambda: nc.scalar.activation(out=DT[:], in_=pE[:, :], func=AF.Exp), [tE], [t_DT])
            pS, tS = k.bank()

            def fS():
                for h in range(H):
                    ins = nc.tensor.matmul(pS[:, h * L:(h + 1) * L], lhsT=kT[s][:, h, :], rhs=qT[s][:, h, :], start=True, stop=True)
                return ins
            P(fS, [t_kT[s], t_qT[s]], [tS])
            V(lambda: nc.vector.tensor_tensor(out=ST[:], in0=pS[:, :], in1=DT[:], op=ALU.mult), [tS, t_DT], [t_ST])
            pB, tB_ = k.bank()

            def fB():
                for h in range(H):
                    ins = nc.tensor.matmul(pB[:, h * L:(h + 1) * L], lhsT=sel[:, h, :], rhs=CBr[:, t0:t0 + L], start=True, stop=True)
                return ins
            P(fB, [t_c, t_R[5]], [tB_])
            V(lambda: nc.vector.tensor_tensor(out=QS[:].rearrange("p h t -> p (h t)"), in0=qT[s][:].rearrange("p h t -> p (h t)"),
                                              in1=pB[:, :], op=ALU.mult), [t_qT[s], tB_], [t_QS])

        def full_post(s, t0, g0, c, d, last_dir):
            w_ = wsel[0]
            AH, DT, ST, QS, KW, COL, RD, HS, SG, YA, junk, st = (x.t[w_] for x in (_AH, _DT, _ST, _QS, _KW, _COL, _RD, _HS, _SG, _YA, _JK, _STT))
            t_AH, t_DT, t_ST, t_QS, t_KW, t_COL, t_RD, t_HS, t_SG, t_YA, t_junk, t_st = (x.tr[w_] for x in (_AH, _DT, _ST, _QS, _KW, _COL, _RD, _HS, _SG, _YA, _JK, _STT))
            nb = [k.bank(), k.bank(), k.bank()]

            def fN():
                for h in range(H):
                    ps = nb[h // 2][0][:, (h % 2) * 256:(h % 2 + 1) * 256]
                    nc.tensor.matmul(ps, lhsT=ST[:, h * L:(h + 1) * L], rhs=vx[s][:, h, :], start=True, stop=False)
                    nc.tensor.matmul(ps, lhsT=QS[:, h, :], rhs=Cb[:, h, 0:256], start=False, stop=True)
                for h in range(H):
                    nc.tensor.matmul(nb[2][0][:, h:h + 1], lhsT=ST[:, h * L:(h + 1) * L], rhs=onescol[:], start=True, stop=False)
                    ins = nc.tensor.matmul(nb[2][0][:, h:h + 1], lhsT=QS[:, h, :], rhs=Cb[:, h, 256:257], start=False, stop=True)
                return ins
            P(fN, [t_ST, t_QS, t_vx[s], t_Cb, t_c], [b[1] for b in nb])
            A(lambda: nc.scalar.activation(out=RD[:, 0:4], in_=nb[2][0][:, 0:4], func=AF.Abs), [nb[2][1]], [t_RD])
            V(lambda: nc.vector.tensor_tensor(out=RD[:, 0:4], in0=RD[:, 0:4], in1=COL[:, 4:8], op=ALU.max), [t_RD, t_COL], [t_RD])
            V(lambda: nc.vector.reciprocal(out=RD[:, 4:8], in_=RD[:, 0:4]), [t_RD], [t_RD])
            if not last_dir:
                hs = s
                for h in range(H):
                    ps = nb[h // 2][0][:, (h % 2) * 256:(h % 2 + 1) * 256]
                    A(lambda h=h, ps=ps: nc.scalar.activation(out=HF[hs][:, h * 256:(h + 1) * 256], in_=ps, func=AF.Copy, scale=RD[:, 4 + h:5 + h]),
                      [nb[h // 2][1], t_RD], [t_HF[hs]])
                k.store(k.sp, scr["hF"][g0:g0 + L, :], HF[hs][:], t_HF[hs], scr["t_hF"])
            else:
                for h in range(H):
                    ps = nb[h // 2][0][:, (h % 2) * 256:(h % 2 + 1) * 256]
                    V(lambda h=h, ps=ps: nc.vector.scalar_tensor_tensor(out=HS[:, h * 256:(h + 1) * 256], in0=ps, scalar=RD[:, 4 + h:5 + h],
                                                                        in1=hfl[s][:, h * 256:(h + 1) * 256], op0=ALU.mult, op1=ALU.add),
                      [nb[h // 2][1], t_RD, t_hfl[s]], [t_HS])
                for h in range(H):
                    A(lambda h=h: nc.scalar.activation(out=junk[:], in_=HS[:, h * 256:(h + 1) * 256], func=AF.Square, accum_out=st[:, h:h + 1]),
                      [t_HS], [t_junk, t_st])
                V(lambda: nc.vector.tensor_scalar(out=st[:, 4:8], in0=st[:, 0:4], scalar1=1.0 / DV, scalar2=EPS, op0=ALU.mult, op1=ALU.add),
                  [t_st], [t_st])
                A(lambda: nc.scalar.activation(out=st[:, 4:8], in_=st[:, 4:8], func=AF.Ln), [t_st], [t_st])
                A(lambda: nc.scalar.activation(out=st[:, 8:12], in_=st[:, 4:8], func=AF.Exp, scale=-0.5), [t_st], [t_st])
                A(lambda: nc.scalar.activation(out=SG[:], in_=ot[s][:], func=AF.Tanh, scale=0.5), [t_ot[s]], [t_SG])
                V(lambda: nc.vector.tensor_scalar(out=SG[:], in0=SG[:], scalar1=0.5, scalar2=0.5, op0=ALU.mult, op1=ALU.add), [t_SG], [t_SG])
                V(lambda: nc.vector.tensor_tensor(out=SG[:], in0=SG[:], in1=nwbc[:], op=ALU.mult), [t_SG, t_c], [t_SG])
                for h in range(H):
                    V(lambda h=h: nc.vector.scalar_tensor_tensor(out=YA[:, h * 256:(h + 1) * 256], in0=HS[:, h * 256:(h + 1) * 256],
                                                                 scalar=st[:, 8 + h:9 + h], in1=SG[:, h * 256:(h + 1) * 256],
                                                                 op0=ALU.mult, op1=ALU.mult), [t_HS, t_st, t_SG], [t_YA])
                pY, tY = k.bank()
                pYb = pY[:].bitcast(BF16)

                def fT():
                    for j in range(8):
                        ins = nc.tensor.transpose(pYb[:, j * 128:(j + 1) * 128], YA[:, j * 128:(j + 1) * 128], identB[:])
                    return ins
                P(fT, [t_YA, t_c], [tY])
                ys = full_post.ys % 2
                full_post.ys += 1
                A(lambda: nc.scalar.copy(out=YT[ys][:].rearrange("p j t -> p (j t)"), in_=pYb), [tY], [t_YT[ys]])
                k.store(k.sp, scr["ymixT"][0:1024, g0:g0 + L].rearrange("(j p) t -> p j t", p=128), YT[ys][:], t_YT[ys], scr["t_ymix"])
        full_post.ys = 0

        def init_state_zero(d):
            for h in range(H):
                V(lambda h=h: nc.vector.memset(CX[h][:], 0.0), w=[t_CX])
            V(lambda: nc.vector.memset(nst[:], 0.0), w=[t_n])
            V(lambda: nc.vector.memset(m0[:, d:d + 1], 0.0), w=[t_m0])

        def init_state_input(d):
            for h in range(H):
                k.load(k.sp, CX[h][:], io["st_c"][d, h], t_CX)
            k.load(k.sp, nst[:], io["st_n"][d], t_n)
            k.load(k.sp, m0[:, d:d + 1], io["st_m"][d], t_m0)

        def carry_m(d, c_last):
            V(lambda: nc.vector.tensor_copy(out=m0[:, d:d + 1], in_=cr[:, 2, c_last:c_last + 1]), [t_cr, t_m0], [t_m0])

        def run_pass(kind, seg_g0, Tn, d, full, last_dir, zT, zF, gLI, gFP, t_gz):
            ncn = Tn // L
            prep(gLI, gFP, seg_g0, Tn, d, t_gz)
            if full:
                refresh_Cb()
            order = list(range(ncn) if d == 0 else range(ncn - 1, -1, -1))
            hfsrc = lambda g0: scr["hF"][g0:g0 + L, :] if full else None
            info = []
            for i, c in enumerate(order):
                info.append(dict(c=c, t0=c * L, g0=seg_g0 + c * L, w=None, s=None))

            def ld(ci):
                ci["s"] = load_chunk(zT, zF, ci["g0"], full, t_gz, full and last_dir, hfsrc(ci["g0"]))

            def pre(ci):
                wsel[0] = (wsel[0] + 1) % 2
                ci["w"] = wsel[0]
                chunk_cols(ci["t0"], ci["c"])
                if full:
                    full_pre(ci["s"], ci["t0"], ci["g0"], ci["c"], d, last_dir)
                state_pre(ci["s"], ci["c"])

            def post(ci, is_last):
                wsel[0] = ci["w"]
                if full:
                    full_post(ci["s"], ci["t0"], ci["g0"], ci["c"], d, last_dir)
                state_post(ci["s"], ci["c"])
                if full and not is_last:
                    refresh_Cb()

            ld(info[0])
            if len(info) > 1:
                ld(info[1])
            pre(info[0])
            for i in range(len(info)):
                if i + 1 < len(info):
                    pre(info[i + 1])
                wnext = wsel[0]
                post(info[i], i + 1 == len(info))
                wsel[0] = wnext
                if i + 2 < len(info):
                    ld(info[i + 2])

        if cfg.TO:
            init_state_input(1)
            run_pass("other", 0, cfg.TO, 1, False, False, scr["zTo"], None, scr["gLIo"], scr["gFPo"], scr["t_zo"])
            carry_m(1, 0)
            run_pass("own", cfg.NPT, cfg.TS, 1, True, False, scr["zT"], scr["zF"], scr["gLI"], scr["gFP"], scr["t_z"])
        else:
            init_state_input(1)
            run_pass("own", cfg.NPT, cfg.TS, 1, True, False, scr["zT"], scr["zF"], scr["gLI"], scr["gFP"], scr["t_z"])
        init_state_input(0)
        run_pass("own", cfg.NPT, cfg.TS, 0, True, True, scr["zT"], scr["zF"], scr["gLI"], scr["gFP"], scr["t_z"])
        for j in range(cfg.NP):
            for d in (1, 0):
                init_state_zero(d)
                run_pass("prompt", j * cfg.TP, cfg.TP, d, True, d == 0, scr["zT"], scr["zF"], scr["gLI"], scr["gFP"], scr["t_z"])
                ncn = cfg.TP // L
                c_last = ncn - 1 if d == 0 else 0
                t_o = scr["t_out"]
                for h in range(H):
                    k.store(k.sp, io["o_c"][j, d, h], CX[h][:], t_CX, t_o)
                k.store(k.sp, io["o_n"][j, d], nst[:], t_n, t_o)
                k.store(k.sp, io["o_m"][j, d], cr[:, 2, c_last:c_last + 1], t_cr, t_o)
    k.barrier()


def phase3(k, cfg, io, scr):
    nc = k.nc
    T = k.track
    with contextlib.ExitStack() as es:
        def sb(name, shape, dt):
            return es.enter_context(nc.sbuf_tensor("s3_" + name, list(shape), dt))

        def V(fn, r=(), w=()):
            k.op(k.dve, fn, r, w)

        def A(fn, r=(), w=()):
            k.op(k.act, fn, r, w)

        def P(fn, r=(), w=()):
            k.op(k.pe, fn, r, w)

        TMAX = max(cfg.TP, cfg.TS, cfg.TO)
        wgt = sb("wgt", [128, 2, 2, NG, 128], BF16)
        cw = sb("cw", [128, NG, 5], F32)
        cb = sb("cb", [128, NG], F32)
        bax = sb("bax", [128, 2, 2, NG], F32)
        spn = sb("spn", [128, 2, NG], F32)
        lam = sb("lam", [128, 2, NG], F32)
        h0 = sb("h0", [128, 2, NG], F32)
        onec = sb("onec", [128, 1], F32)
        hfin = sb("hfin", [128, cfg.NP, 2, NG], F32)
        hcar = sb("hcar", [128, NG], F32)
        t_c, t_hfin, t_hcar = T(), T(), T()
        k.load(k.pool, wgt[:], io["rg_w"], t_c)
        k.load(k.sp, cw[:], io["conv_w5"], t_c)
        k.load(k.sp, cb[:], io["conv_b"], t_c)
        k.load(k.sp, bax[:], io["rg_b"], t_c)
        k.load(k.sp, lam[:], io["rg_lam"], t_c)
        k.load(k.sp, h0[:], io["st_h"], t_c)
        V(lambda: nc.vector.memset(onec[:], 1.0), w=[t_c])
        V(lambda: nc.vector.tensor_scalar(out=bax[:], in0=bax[:], scalar1=0.5, scalar2=None, op0=ALU.mult), [t_c], [t_c])
        tmp8 = sb("tmp8", [128, 2, NG], F32)
        t_t8 = T()
        A(lambda: nc.scalar.activation(out=tmp8[:], in_=lam[:], func=AF.Abs), [t_c], [t_t8])
        A(lambda: nc.scalar.activation(out=tmp8[:], in_=tmp8[:], func=AF.Exp, scale=-1.0), [t_t8], [t_t8])
        V(lambda: nc.vector.tensor_scalar_add(out=tmp8[:], in0=tmp8[:], scalar1=1.0), [t_t8], [t_t8])
        A(lambda: nc.scalar.activation(out=tmp8[:], in_=tmp8[:], func=AF.Ln), [t_t8], [t_t8])
        V(lambda: nc.vector.tensor_scalar(out=spn[:], in0=lam[:], scalar1=-1.0, scalar2=0.0, op0=ALU.mult, op1=ALU.max), [t_c], [t_c])
        V(lambda: nc.vector.tensor_tensor(out=spn[:], in0=spn[:], in1=tmp8[:], op=ALU.add), [t_c, t_t8], [t_c])
        V(lambda: nc.vector.tensor_scalar(out=spn[:], in0=spn[:], scalar1=-4.0, scalar2=None, op0=ALU.mult), [t_c], [t_c])
        V(lambda: nc.vector.memset(hfin[:], 0.0), w=[t_hfin])

        xr = [sb(f"xr{i}", [128, TMAX], BF16) for i in range(2)]
        xg = [sb(f"xg{i}", [128, TMAX], BF16) for i in range(2)]
        t_xr, t_xg = [T(), T()], [T(), T()]
        class BSet:
            def __init__(self, i):
                self.xc = sb(f"xc_{i}", [128, TMAX], F32)
                self.xcb = sb(f"xcb_{i}", [128, TMAX], BF16)
                self.av = [sb(f"av{d}_{i}", [128, TMAX], F32) for d in range(2)]
                self.uv = [sb(f"uv{d}_{i}", [128, TMAX], F32) for d in range(2)]
                self.hv = [sb(f"hv{d}_{i}", [128, TMAX], F32) for d in range(2)]
                self.g1 = sb(f"g1_{i}", [128, TMAX], F32)
                self.t_xc, self.t_xcb, self.t_g1 = T(), T(), T()
                self.t_a, self.t_u, self.t_h = [T(), T()], [T(), T()], [T(), T()]
        bsets = [BSet(0), BSet(1)]
        cur = [bsets[0]]
        nseg = [0]

        def next_set():
            nseg[0] += 1
            cur[0] = bsets[nseg[0] % 2]
        th = [sb(f"th{i}", [128, 512], F32) for i in range(2)]
        thx = [sb(f"thx{i}", [128, 512], F32) for i in range(2)]
        sq = [sb(f"sq{i}", [128, 512], F32) for i in range(2)]
        t_th, t_thx, t_sq = [T(), T()], [T(), T()], [T(), T()]
        yb = [sb(f"yb{i}", [128, TMAX], BF16) for i in range(2)]
        t_yb = [T(), T()]
        cnt = [0, 0]

        def conv(s, g, Tn, W):
            B_ = cur[0]
            xc, xcb, av, uv, hv, g1 = B_.xc, B_.xcb, B_.av, B_.uv, B_.hv, B_.g1
            t_xc, t_xcb, t_a, t_u, t_h, t_g1 = B_.t_xc, B_.t_xcb, B_.t_a, B_.t_u, B_.t_h, B_.t_g1
            nr = Tn // W
            x3 = xr[s][:, 0:Tn].rearrange("p (r w) -> p r w", w=W)
            c3 = xc[:, 0:Tn].rearrange("p (r w) -> p r w", w=W)
            V(lambda: nc.vector.tensor_scalar(out=xc[:, 0:Tn], in0=xr[s][:, 0:Tn], scalar1=cw[:, g, 2:3], scalar2=cb[:, g:g + 1],
                                              op0=ALU.mult, op1=ALU.add), [t_xr[s], t_c], [t_xc])
            for o in (-2, -1, 1, 2):
                lo_o, hi_o = max(0, -o), W - max(0, o)
                lo_i, hi_i = max(0, o), W - max(0, -o)
                V(lambda o=o, lo_o=lo_o, hi_o=hi_o, lo_i=lo_i, hi_i=hi_i:
                  nc.vector.scalar_tensor_tensor(out=c3[:, :, lo_o:hi_o], in0=x3[:, :, lo_i:hi_i], scalar=cw[:, g, o + 2:o + 3],
                                                 in1=c3[:, :, lo_o:hi_o], op0=ALU.mult, op1=ALU.add), [t_xr[s], t_c, t_xc], [t_xc])
            A(lambda: nc.scalar.copy(out=xcb[:, 0:Tn], in_=xc[:, 0:Tn]), [t_xc], [t_xcb])

        def gates(g, Tn, d):
            B_ = cur[0]
            xc, xcb, av, uv, hv, g1 = B_.xc, B_.xcb, B_.av, B_.uv, B_.hv, B_.g1
            t_xc, t_xcb, t_a, t_u, t_h, t_g1 = B_.t_xc, B_.t_xcb, B_.t_a, B_.t_u, B_.t_h, B_.t_g1
            for b0 in range(0, Tn, 512):
                n = min(512, Tn - b0)
                i = cnt[0] % 2
                cnt[0] += 1
                pa, ta = k.bank()
                px, tx = k.bank()

                def f(pa=pa, px=px, b0=b0, n=n):
                    nc.tensor.matmul(pa[:, 0:n], lhsT=wgt[:, d, 0, g, :], rhs=xcb[:, b0:b0 + n], start=True, stop=True)
                    return nc.tensor.matmul(px[:, 0:n], lhsT=wgt[:, d, 1, g, :], rhs=xcb[:, b0:b0 + n], start=True, stop=True)
                P(f, [t_c, t_xcb], [ta, tx])
                A(lambda pa=pa, n=n, i=i: nc.scalar.activation(out=th[i][:, 0:n], in_=pa[:, 0:n], func=AF.Tanh, scale=0.5, bias=bax[:, d, 0, g:g + 1]),
                  [ta, t_c], [t_th[i]])
                A(lambda px=px, n=n, i=i: nc.scalar.activation(out=thx[i][:, 0:n], in_=px[:, 0:n], func=AF.Tanh, scale=0.5, bias=bax[:, d, 1, g:g + 1]),
                  [tx, t_c], [t_thx[i]])
                A(lambda b0=b0, n=n, i=i: nc.scalar.activation(out=av[d][:, b0:b0 + n], in_=th[i][:, 0:n], func=AF.Exp,
                                                               scale=spn[:, d, g:g + 1], bias=spn[:, d, g:g + 1]), [t_th[i], t_c], [t_a[d]])
                A(lambda b0=b0, n=n, i=i: nc.scalar.activation(out=sq[i][:, 0:n], in_=av[d][:, b0:b0 + n], func=AF.Square), [t_a[d]], [t_sq[i]])
                A(lambda n=n, i=i: nc.scalar.activation(out=sq[i][:, 0:n], in_=sq[i][:, 0:n], func=AF.Ln, scale=-1.0, bias=onec[:]), [t_sq[i], t_c], [t_sq[i]])
                A(lambda n=n, i=i: nc.scalar.activation(out=sq[i][:, 0:n], in_=sq[i][:, 0:n], func=AF.Exp, scale=0.5), [t_sq[i]], [t_sq[i]])
                V(lambda b0=b0, n=n, i=i: nc.vector.scalar_tensor_tensor(out=uv[d][:, b0:b0 + n], in0=thx[i][:, 0:n], scalar=1.0, in1=xc[:, b0:b0 + n],
                                                                         op0=ALU.add, op1=ALU.mult), [t_thx[i], t_xc], [t_u[d]])
                V(lambda b0=b0, n=n, i=i: nc.vector.scalar_tensor_tensor(out=uv[d][:, b0:b0 + n], in0=uv[d][:, b0:b0 + n], scalar=0.5, in1=sq[i][:, 0:n],
                                                                         op0=ALU.mult, op1=ALU.mult), [t_u[d], t_sq[i]], [t_u[d]])

        def scan(Tn, d, init_ap, t_init):
            B_ = cur[0]
            xc, xcb, av, uv, hv, g1 = B_.xc, B_.xcb, B_.av, B_.uv, B_.hv, B_.g1
            t_xc, t_xcb, t_a, t_u, t_h, t_g1 = B_.t_xc, B_.t_xcb, B_.t_a, B_.t_u, B_.t_h, B_.t_g1
            rv = (lambda ap: ap[:, ::-1]) if d == 1 else (lambda ap: ap)
            V(lambda: nc.vector.tensor_tensor_scan(out=rv(hv[d][:, 0:Tn]), data0=rv(av[d][:, 0:Tn]), data1=rv(uv[d][:, 0:Tn]),
                                                   initial=init_ap, op0=ALU.mult, op1=ALU.add), [t_a[d], t_u[d], t_init], [t_h[d]])

        def load_x(zF, row_xr, row_xg, g0, Tn, t_gz):
            s = cnt[1] % 2
            cnt[1] += 1
            k.load(k.sp, xr[s][:, 0:Tn], zF[row_xr:row_xr + 128, g0:g0 + Tn], t_xr[s], reads=[t_gz])
            if row_xg is not None:
                k.load(k.sp, xg[s][:, 0:Tn], zF[row_xg:row_xg + 128, g0:g0 + Tn], t_xg[s], reads=[t_gz])
            return s

        def out_part(s, g, g0, Tn):
            B_ = cur[0]
            xc, xcb, av, uv, hv, g1 = B_.xc, B_.xcb, B_.av, B_.uv, B_.hv, B_.g1
            t_xc, t_xcb, t_a, t_u, t_h, t_g1 = B_.t_xc, B_.t_xcb, B_.t_a, B_.t_u, B_.t_h, B_.t_g1
            X = xg[s][:, 0:Tn]
            G1, G2 = g1[:, 0:Tn], hv[0][:, 0:Tn]
            t_g2 = t_h[0]
            V(lambda: nc.vector.tensor_tensor(out=G1, in0=X, in1=X, op=ALU.mult), [t_xg[s]], [t_g1])
            V(lambda: nc.vector.tensor_scalar(out=G1, in0=G1, scalar1=0.044715, scalar2=1.0, op0=ALU.mult, op1=ALU.add), [t_g1], [t_g1])
            V(lambda: nc.vector.tensor_tensor(out=G1, in0=G1, in1=X, op=ALU.mult), [t_g1, t_xg[s]], [t_g1])
            A(lambda: nc.scalar.activation(out=G1, in_=G1, func=AF.Tanh, scale=0.7978845608028654), [t_g1], [t_g1])
            V(lambda: nc.vector.scalar_tensor_tensor(out=G1, in0=G1, scalar=1.0, in1=X, op0=ALU.add, op1=ALU.mult), [t_g1, t_xg[s]], [t_g1])
            V(lambda: nc.vector.tensor_tensor(out=G2, in0=hv[0][:, 0:Tn], in1=hv[1][:, 0:Tn], op=ALU.add), [t_h[0], t_h[1]], [t_g2])
            ys = cnt[1] % 2
            V(lambda: nc.vector.scalar_tensor_tensor(out=yb[ys][:, 0:Tn], in0=G2, scalar=0.5, in1=G1, op0=ALU.mult, op1=ALU.mult),
              [t_g1, t_g2], [t_yb[ys]])
            k.store(k.sp, scr["ymixT"][1024 + g * 128:1024 + (g + 1) * 128, g0:g0 + Tn], yb[ys][:, 0:Tn], t_yb[ys], scr["t_ymix"])

        zero_col = sb("zero_col", [128, 1], F32)
        V(lambda: nc.vector.memset(zero_col[:], 0.0), w=[t_c])
        segs = []
        for g in range(NG):
            if cfg.TO:
                segs.append(dict(g=g, kind="other", T=cfg.TO, W=64, g0=0))
            segs.append(dict(g=g, kind="own", T=cfg.TS, W=64, g0=cfg.NPT))
            for j in range(cfg.NP):
                segs.append(dict(g=g, kind="prompt", T=cfg.TP, W=cfg.TP, g0=j * cfg.TP, j=j))
        for i, sg_ in enumerate(segs):
            sg_["set"] = bsets[i % 2]

        def stA(sg_):
            cur[0] = sg_["set"]
            g = sg_["g"]
            if sg_["kind"] == "other":
                sg_["s"] = load_x(scr["zFo"], g * 128, None, 0, sg_["T"], scr["t_zo"])
            else:
                sg_["s"] = load_x(scr["zF"], 1024 + g * 128, 2048 + g * 128, sg_["g0"], sg_["T"], scr["t_z"])
            conv(sg_["s"], g, sg_["T"], sg_["W"])

        def stB(sg_):
            cur[0] = sg_["set"]
            g = sg_["g"]
            if sg_["kind"] == "other":
                gates(g, sg_["T"], 1)
            else:
                for d in range(2):
                    gates(g, sg_["T"], d)

        def stC(sg_):
            cur[0] = sg_["set"]
            B_ = sg_["set"]
            g = sg_["g"]
            Tn = sg_["T"]
            if sg_["kind"] == "other":
                scan(Tn, 1, h0[:, 1, g:g + 1], t_c)
                V(lambda: nc.vector.tensor_copy(out=hcar[:, g:g + 1], in_=B_.hv[1][:, 0:1]), [B_.t_h[1], t_hcar], [t_hcar])
            elif sg_["kind"] == "own":
                scan(Tn, 0, h0[:, 0, g:g + 1], t_c)
                if cfg.TO:
                    scan(Tn, 1, hcar[:, g:g + 1], t_hcar)
                else:
                    scan(Tn, 1, h0[:, 1, g:g + 1], t_c)
                out_part(sg_["s"], g, sg_["g0"], Tn)
            else:
                j = sg_["j"]
                for d in range(2):
                    scan(Tn, d, zero_col[:], t_c)
                V(lambda: nc.vector.tensor_copy(out=hfin[:, j, 0, g:g + 1], in_=B_.hv[0][:, Tn - 1:Tn]), [B_.t_h[0], t_hfin], [t_hfin])
                V(lambda: nc.vector.tensor_copy(out=hfin[:, j, 1, g:g + 1], in_=B_.hv[1][:, 0:1]), [B_.t_h[1], t_hfin], [t_hfin])
                out_part(sg_["s"], g, sg_["g0"], Tn)

        stA(segs[0])
        stB(segs[0])
        for i in range(len(segs)):
            if i + 1 < len(segs):
                stA(segs[i + 1])
            stC(segs[i])
            if i + 1 < len(segs):
                stB(segs[i + 1])
        k.store(k.sp, io["o_h"], hfin[:], t_hfin, scr["t_out"])
    k.barrier()


def phase4(k, cfg, io, scr):
    nc = k.nc
    T = k.track
    CAP = cfg.CAP
    NT = cfg.NOWN // 128
    IDX, WTS, t_idx = scr["IDX"], scr["WTS"], scr["t_idx"]
    with contextlib.ExitStack() as es:
        def sb(name, shape, dt):
            return es.enter_context(nc.sbuf_tensor("s4_" + name, list(shape), dt))

        def V(fn, r=(), w=()):
            k.op(k.dve, fn, r, w)

        def A(fn, r=(), w=()):
            k.op(k.act, fn, r, w)

        def P(fn, r=(), w=()):
            k.op(k.pe, fn, r, w)

        wo = sb("wo", [128, KD, D], BF16)
        t_wo = T()
        wov = io["w_out"].rearrange("(k p) c -> p k c", p=128)
        for q in range(4):
            for hh in range(2):
                k.load(k.pool, wo[:, hh * 8:(hh + 1) * 8, q * 512:(q + 1) * 512], wov[:, hh * 8:(hh + 1) * 8, q * 512:(q + 1) * 512], t_wo)
        identF = sb("identF", [128, 128], F32)
        wr = sb("wr", [128, KD, 36], F32)
        brbc = sb("brbc", [128, 36], F32)
        ebase = sb("ebase", [128, NE], F32)
        ltri = sb("ltri", [128, 128], BF16)
        onesm = sb("onesm", [128, 128], BF16)
        n2bc = sb("n2bc", [128, D], F32)
        t_c = T()
        k.load(k.sp, identF[:], io["ident"], t_c)
        k.load(k.sp, wr[:], io["w_router"].rearrange("(k p) c -> p k c", p=128), t_c)
        k.load(k.sp, brbc[:], io["b_router"].partition_broadcast(128), t_c)
        k.load(k.sp, ebase[:], io["ebase"].partition_broadcast(128), t_c)
        k.load(k.pool, ltri[:], io["ltri"], t_c)
        k.load(k.sp, n2bc[:], io["norm2_w"].partition_broadcast(128), t_c)
        V(lambda: nc.vector.memset(onesm[:], 1.0), w=[t_c])
        g1bc = sb("g1bc", [128, D], F32)
        G2bc = sb("G2bc", [128, D], F32)
        S2bc = sb("S2bc", [128, D], F32)
        t_g1, t_G2, t_S2 = T(), T(), T()
        run = sb("run", [128, NE], F32)
        t_run = T()
        V(lambda: nc.vector.memset(run[:], 0.0), w=[t_run])
        ym = [sb(f"ym{i}", [128, KD, 128], BF16) for i in range(2)]
        xs = [sb(f"xs{i}", [128, D], F32) for i in range(2)]
        t_ym, t_xs = [T(), T()], [T(), T()]
        x1 = [sb(f"x1{i}", [128, D], F32) for i in range(2)]
        t_x1 = [T(), T()]
        tmpf = sb("tmpf", [128, D], F32)
        t_tmpf = T()
        h2f = sb("h2f", [128, D], F32)
        t_h2f = T()
        h2b = [sb(f"h2b{i}", [128, D], BF16) for i in range(2)]
        t_h2b = [T(), T()]
        h2T = sb("h2T", [128, KD, 128], F32)
        t_h2T = T()
        junk = sb("junk", [128, D], BF16)
        t_junk = T()
        st = sb("st", [128, 8], F32)
        t_st = T()
        rt = sb("rt", [128, 160], F32)
        t_rt = T()
        mx8 = sb("mx8", [128, 8], F32)
        t_mx = T()
        mab = sb("mab", [128, NE], BF16)
        t_mab = T()
        cur_mod = [-1]

        def load_mod(m):
            if cur_mod[0] == m:
                return
            cur_mod[0] = m
            tm = scr["t_modrow"]
            k.load(k.sp, g1bc[:], scr["modrow"][m:m + 1, 2 * D:3 * D].partition_broadcast(128), t_g1, reads=[tm])
            k.load(k.sp, S2bc[:], scr["modrow"][m:m + 1, 3 * D:4 * D].partition_broadcast(128), t_S2, reads=[tm])
            k.load(k.sp, G2bc[:], scr["modrow"][m:m + 1, 4 * D:5 * D].partition_broadcast(128), t_G2, reads=[tm])
            V(lambda: nc.vector.scalar_tensor_tensor(out=G2bc[:], in0=G2bc[:], scalar=1.0, in1=n2bc[:], op0=ALU.add, op1=ALU.mult),
              [t_G2, t_c], [t_G2])

        def load_tile(i):
            s = i % 2
            g0 = i * 128
            k.load(k.sp, ym[s][:], scr["ymixT"][:, g0:g0 + 128].rearrange("(k p) t -> p k t", p=128), t_ym[s], reads=[scr["t_ymix"]])
            k.load(k.sp, xs[s][:], io["x"][g0:g0 + 128, :], t_xs[s])

        load_tile(0)
        for i in range(NT):
            if i + 1 < NT:
                load_tile(i + 1)
            s = i % 2
            g0 = i * 128
            load_mod(0 if g0 < cfg.NPT else 1)
            for q in range(4):
                ps, pt = k.bank()

                def mm(ps=ps, q=q):
                    for kk in range(KD):
                        ins = nc.tensor.matmul(ps[:, :], lhsT=ym[s][:, kk, :], rhs=wo[:, kk, q * 512:(q + 1) * 512],
                                               start=(kk == 0), stop=(kk == KD - 1))
                    return ins
                P(mm, [t_ym[s], t_wo], [pt])
                V(lambda ps=ps, q=q: nc.vector.tensor_tensor(out=tmpf[:, q * 512:(q + 1) * 512], in0=ps[:, :], in1=g1bc[:, q * 512:(q + 1) * 512],
                                                             op=ALU.mult), [pt, t_g1], [t_tmpf])
            V(lambda: nc.vector.tensor_tensor(out=x1[s][:], in0=tmpf[:], in1=xs[s][:], op=ALU.add), [t_tmpf, t_xs[s]], [t_x1[s]])
            k.store(k.sp, scr["x1s"][g0:g0 + 128, :], x1[s][:], t_x1[s], scr["t_x1s"])
            A(lambda: nc.scalar.activation(out=junk[:], in_=x1[s][:], func=AF.Square, accum_out=st[:, 0:1]), [t_x1[s]], [t_junk, t_st])
            V(lambda: nc.vector.tensor_scalar(out=st[:, 1:2], in0=st[:, 0:1], scalar1=1.0 / D, scalar2=EPS, op0=ALU.mult, op1=ALU.add), [t_st], [t_st])
            A(lambda: nc.scalar.activation(out=st[:, 2:3], in_=st[:, 1:2], func=AF.Ln), [t_st], [t_st])
            A(lambda: nc.scalar.activation(out=st[:, 3:4], in_=st[:, 2:3], func=AF.Exp, scale=-0.5), [t_st], [t_st])
            V(lambda: nc.vector.scalar_tensor_tensor(out=tmpf[:], in0=x1[s][:], scalar=st[:, 3:4], in1=G2bc[:], op0=ALU.mult, op1=ALU.mult),
              [t_x1[s], t_st, t_G2], [t_tmpf])
            V(lambda: nc.vector.tensor_tensor(out=h2f[:], in0=tmpf[:], in1=S2bc[:], op=ALU.add), [t_tmpf, t_S2], [t_h2f])
            A(lambda: nc.scalar.copy(out=h2b[s][:], in_=h2f[:]), [t_h2f], [t_h2b[s]])
            for q in range(4):
                ps, pt = k.bank()

                def tr(ps=ps, q=q):
                    for j in range(4):
                        kk = q * 4 + j
                        ins = nc.tensor.transpose(ps[:, j * 128:(j + 1) * 128], h2f[:, kk * 128:(kk + 1) * 128], identF[:])
                    return ins
                P(tr, [t_h2f, t_c], [pt])
                dst = h2T[:, q * 4:(q + 1) * 4, :].rearrange("p j t -> p (j t)")
                if q % 2 == 0:
                    A(lambda dst=dst, ps=ps: nc.scalar.copy(out=dst, in_=ps[:, :]), [pt], [t_h2T])
                else:
                    V(lambda dst=dst, ps=ps: nc.vector.tensor_copy(out=dst, in_=ps[:, :]), [pt], [t_h2T])
            pl, tl = k.bank()

            def mml():
                for kk in range(KD):
                    ins = nc.tensor.matmul(pl[:, 0:36], lhsT=h2T[:, kk, :], rhs=wr[:, kk, :], start=(kk == 0), stop=(kk == KD - 1))
                return ins
            P(mml, [t_h2T, t_c], [tl])
            LG = rt[:, 0:36]
            V(lambda: nc.vector.tensor_tensor(out=LG, in0=pl[:, 0:36], in1=brbc[:], op=ALU.add), [tl, t_c], [t_rt])
            V(lambda: nc.vector.reduce_max(out=rt[:, 36:37], in_=rt[:, 0:4], axis=AX.X), [t_rt], [t_rt])
            V(lambda: nc.vector.tensor_scalar(out=rt[:, 37:38], in0=rt[:, 36:37], scalar1=-1.0, scalar2=None, op0=ALU.mult), [t_rt], [t_rt])
            V(lambda: nc.vector.tensor_scalar(out=rt[:, 40:44], in0=rt[:, 0:4], scalar1=rt[:, 36:37], scalar2=None, op0=ALU.is_equal), [t_rt], [t_rt])
            A(lambda: nc.scalar.activation(out=rt[:, 44:48], in_=rt[:, 0:4], func=AF.Exp, bias=rt[:, 37:38], accum_out=rt[:, 38:39]), [t_rt], [t_rt])
            V(lambda: nc.vector.reciprocal(out=rt[:, 39:40], in_=rt[:, 38:39]), [t_rt], [t_rt])
            V(lambda: nc.vector.tensor_scalar(out=rt[:, 48:56], in0=rt[:, 4:12], scalar1=rt[:, 40:41], scalar2=None, op0=ALU.mult), [t_rt], [t_rt])
            for g in range(1, 4):
                V(lambda g=g: nc.vector.scalar_tensor_tensor(out=rt[:, 48:56], in0=rt[:, 4 + 8 * g:12 + 8 * g], scalar=rt[:, 40 + g:41 + g],
                                                             in1=rt[:, 48:56], op0=ALU.mult, op1=ALU.add), [t_rt], [t_rt])
            V(lambda: nc.vector.max(out=mx8[:], in_=rt[:, 48:56]), [t_rt], [t_mx])
            V(lambda: nc.vector.tensor_scalar(out=rt[:, 56:64], in0=rt[:, 48:56], scalar1=mx8[:, 0:1], scalar2=None, op0=ALU.is_equal), [t_rt, t_mx], [t_rt])
            V(lambda: nc.vector.tensor_scalar(out=rt[:, 64:72], in0=rt[:, 48:56], scalar1=mx8[:, 1:2], scalar2=None, op0=ALU.is_equal), [t_rt, t_mx], [t_rt])
            V(lambda: nc.vector.tensor_tensor(out=rt[:, 72:73], in0=mx8[:, 1:2], in1=mx8[:, 0:1], op=ALU.subtract), [t_mx, t_rt], [t_rt])
            A(lambda: nc.scalar.activation(out=rt[:, 73:74], in_=rt[:, 72:73], func=AF.Exp), [t_rt], [t_rt])
            V(lambda: nc.vector.tensor_scalar_add(out=rt[:, 74:75], in0=rt[:, 73:74], scalar1=1.0), [t_rt], [t_rt])
            V(lambda: nc.vector.reciprocal(out=rt[:, 74:75], in_=rt[:, 74:75]), [t_rt], [t_rt])
            V(lambda: nc.vector.tensor_tensor(out=WTS[:, i, 0:1], in0=rt[:, 74:75], in1=rt[:, 39:40], op=ALU.mult), [t_rt, t_idx], [t_idx])
            V(lambda: nc.vector.tensor_tensor(out=WTS[:, i, 1:2], in0=rt[:, 39:40], in1=WTS[:, i, 0:1], op=ALU.subtract), [t_rt, t_idx], [t_idx])
            for kk2, (oc, oh) in enumerate(((80, 56), (112, 64))):
                V(lambda oc=oc, oh=oh: nc.vector.tensor_tensor(out=rt[:, oc:oc + 32].rearrange("p (g e) -> p g e", g=4),
                                                               in0=rt[:, 40:44].unsqueeze(2).to_broadcast([128, 4, 8]),
                                                               in1=rt[:, oh:oh + 8].unsqueeze(1).to_broadcast([128, 4, 8]), op=ALU.mult), [t_rt], [t_rt])
            V(lambda: nc.vector.tensor_tensor(out=mab[:], in0=rt[:, 80:112], in1=rt[:, 112:144], op=ALU.add), [t_rt], [t_mab])
            pp, tp = k.bank()

            def mmp():
                nc.tensor.matmul(pp[:, 0:32], lhsT=ltri[:], rhs=mab[:], start=True, stop=True)
                return nc.tensor.matmul(pp[:, 32:64], lhsT=onesm[:], rhs=mab[:], start=True, stop=True)
            P(mmp, [t_c, t_mab], [tp])
            V(lambda: nc.vector.tensor_tensor(out=rt[:, 0:32], in0=pp[:, 0:32], in1=run[:], op=ALU.add), [tp, t_run, t_rt], [t_rt])
            V(lambda: nc.vector.tensor_scalar(out=rt[:, 32:64], in0=rt[:, 0:32], scalar1=float(CAP), scalar2=1.0e6, op0=ALU.is_ge, op1=ALU.mult),
              [t_rt], [t_rt])
            V(lambda: nc.vector.tensor_tensor(out=rt[:, 0:32], in0=rt[:, 0:32], in1=rt[:, 32:64], op=ALU.add), [t_rt], [t_rt])
            V(lambda: nc.vector.tensor_tensor(out=rt[:, 0:32], in0=rt[:, 0:32], in1=ebase[:], op=ALU.add), [t_rt, t_c], [t_rt])
            V(lambda: nc.vector.tensor_tensor(out=run[:], in0=run[:], in1=pp[:, 32:64], op=ALU.add), [tp, t_run], [t_run])
            for kk2, oc in enumerate((80, 112)):
                V(lambda oc=oc: nc.vector.tensor_tensor(out=rt[:, 32:64], in0=rt[:, oc:oc + 32], in1=rt[:, 0:32], op=ALU.mult), [t_rt], [t_rt])
                V(lambda kk2=kk2: nc.vector.reduce_sum(out=rt[:, 144 + kk2:145 + kk2], in_=rt[:, 32:64], axis=AX.X), [t_rt], [t_rt])
            V(lambda: nc.vector.tensor_copy(out=IDX[:, i, :], in_=rt[:, 144:146]), [t_rt, t_idx], [t_idx])
            for kk2 in range(2):
                k.idma(out=scr["xdisp"][:, :], out_off=bass.IndirectOffsetOnAxis(ap=IDX[:, i, kk2:kk2 + 1], axis=0),
                       in_=h2b[s][:, :], in_off=None, sem_track=t_h2b[s], reads=[t_h2b[s], t_idx], writes=[scr["t_xdisp"]],
                       bounds_check=NE * CAP - 1, oob_is_err=False)
        k.store(k.sp, io["o_cnt"], run[:], t_run, scr["t_out"])
    k.barrier()


def phase5(k, cfg, io, scr):
    nc = k.nc
    T = k.track
    CAP = cfg.CAP
    NB = CAP // 128
    with contextlib.ExitStack() as es:
        def sb(name, shape, dt):
            return es.enter_context(nc.sbuf_tensor("s5_" + name, list(shape), dt))

        def V(fn, r=(), w=()):
            k.op(k.dve, fn, r, w)

        def A(fn, r=(), w=()):
            k.op(k.act, fn, r, w)

        def P(fn, r=(), w=()):
            k.op(k.pe, fn, r, w)

        identB = sb("identB", [128, 128], BF16)
        t_c = T()
        k.load(k.pool, identB[:], io["ident"], t_c)
        wgu = [sb(f"wgu{i}", [128, KD, 512], BF16) for i in range(4)]
        t_wgu = [T() for _ in range(4)]
        wd = [sb(f"wd{i}", [128, 8, 1024], BF16) for i in range(2)]
        t_wd = [T(), T()]
        xd = [sb("xd0", [128, NB, D], BF16)] * 2
        t_xd = [T()] * 2
        xT = [sb(f"xT{i}", [128, KD, CAP], BF16) for i in range(2)]
        t_xT = [T(), T()]
        hid = [sb(f"hid{i}", [128, 8, CAP], BF16) for i in range(2)]
        t_hid = [T(), T()]
        sg = [sb(f"sg{i}", [128, CAP], F32) for i in range(2)]
        t_sg = [T(), T()]
        yo = [sb(f"yo{i}", [128, 1024], F32) for i in range(4)]
        t_yo = [T() for _ in range(4)]
        cnt = {"gu": 0, "wd": 0, "sg": 0, "yo": 0, "ev": 0}
        wgv = io["ew_gate"].rearrange("e (k p) f -> e p k f", p=128)
        wuv = io["ew_up"].rearrange("e (k p) f -> e p k f", p=128)
        wdv = io["ew_down"].rearrange("e (k p) c -> e p k c", p=128)
        gu_list = []
        for e in range(NE):
            for j in range(2):
                gu_list.append((e, j))
        wd_list = [(e, dh) for e in range(NE) for dh in range(2)]

        def load_gu(idx):
            e, j = gu_list[idx]
            sg_, su_ = (idx % 2) * 2, (idx % 2) * 2 + 1
            for hh in range(2):
                k.load(k.pool, wgu[sg_][:, hh * 8:(hh + 1) * 8, :], wgv[e, :, hh * 8:(hh + 1) * 8, j * 512:(j + 1) * 512], t_wgu[sg_])
            for hh in range(2):
                k.load(k.pool, wgu[su_][:, hh * 8:(hh + 1) * 8, :], wuv[e, :, hh * 8:(hh + 1) * 8, j * 512:(j + 1) * 512], t_wgu[su_])

        def load_wd(idx):
            e, dh = wd_list[idx]
            s_ = idx % 2
            k.load(k.pool, wd[s_][:], wdv[e, :, :, dh * 1024:(dh + 1) * 1024], t_wd[s_])

        def load_x(e):
            s_ = e % 2
            k.load(k.sp, xd[s_][:], scr["xdisp"][e * CAP:(e + 1) * CAP, :].rearrange("(b p) d -> p b d", p=128), t_xd[s_],
                   reads=[scr["t_xdisp"]])

        load_x(0)
        load_gu(0)
        load_wd(0)
        for e in range(NE):
            s_ = e % 2
            for b in range(NB):
                for half in range(2):
                    ps, pt = k.bank()
                    psb = ps[:].bitcast(BF16)

                    def tr(psb=psb, b=b, half=half):
                        for j in range(8):
                            kk = half * 8 + j
                            ins = nc.tensor.transpose(psb[:, j * 128:(j + 1) * 128], xd[s_][:, b, kk * 128:(kk + 1) * 128], identB[:])
                        return ins
                    P(tr, [t_xd[s_], t_c], [pt])
                    dst = xT[s_][:, half * 8:(half + 1) * 8, b * 128:(b + 1) * 128]
                    src = psb.rearrange("p (j t) -> p j t", j=8)
                    if cnt["ev"] % 2 == 0:
                        A(lambda dst=dst, src=src: nc.scalar.copy(out=dst, in_=src), [pt], [t_xT[s_]])
                    else:
                        V(lambda dst=dst, src=src: nc.vector.tensor_copy(out=dst, in_=src), [pt], [t_xT[s_]])
                    cnt["ev"] += 1
            if e + 1 < NE:
                load_x(e + 1)
            for j in range(2):
                gi = e * 2 + j
                if gi + 1 < len(gu_list):
                    load_gu(gi + 1)
                sgw, suw = (gi % 2) * 2, (gi % 2) * 2 + 1
                for f in range(4):
                    pg, tg = k.bank()
                    pu, tu = k.bank()

                    def mm(pg=pg, pu=pu, f=f, sgw=sgw, suw=suw):
                        for kk in range(KD):
                            nc.tensor.matmul(pg[:, 0:CAP], lhsT=wgu[sgw][:, kk, f * 128:(f + 1) * 128], rhs=xT[s_][:, kk, :],
                                             start=(kk == 0), stop=(kk == KD - 1))
                        for kk in range(KD):
                            ins = nc.tensor.matmul(pu[:, 0:CAP], lhsT=wgu[suw][:, kk, f * 128:(f + 1) * 128], rhs=xT[s_][:, kk, :],
                                                   start=(kk == 0), stop=(kk == KD - 1))
                        return ins
                    P(mm, [t_wgu[sgw], t_wgu[suw], t_xT[s_]], [tg, tu])
                    si = cnt["sg"] % 2
                    cnt["sg"] += 1
                    A(lambda pg=pg, si=si: nc.scalar.activation(out=sg[si][:], in_=pg[:, 0:CAP], func=AF.Silu), [tg], [t_sg[si]])
                    V(lambda pu=pu, si=si, j=j, f=f: nc.vector.tensor_tensor(out=hid[s_][:, j * 4 + f, :], in0=sg[si][:], in1=pu[:, 0:CAP], op=ALU.mult),
                      [t_sg[si], tu], [t_hid[s_]])
            for dh in range(2):
                wi = e * 2 + dh
                if wi + 1 < len(wd_list):
                    load_wd(wi + 1)
                ws = wi % 2
                for b in range(NB):
                    yi = cnt["yo"] % 4
                    cnt["yo"] += 1
                    for c in range(2):
                        ps, pt = k.bank()

                        def mm(ps=ps, b=b, c=c, ws=ws):
                            for f in range(8):
                                ins = nc.tensor.matmul(ps[:, :], lhsT=hid[s_][:, f, b * 128:(b + 1) * 128], rhs=wd[ws][:, f, c * 512:(c + 1) * 512],
                                                       start=(f == 0), stop=(f == 7))
                            return ins
                        P(mm, [t_hid[s_], t_wd[ws]], [pt])
                        if cnt["ev"] % 2 == 0:
                            A(lambda ps=ps, yi=yi, c=c: nc.scalar.copy(out=yo[yi][:, c * 512:(c + 1) * 512], in_=ps[:, :]), [pt], [t_yo[yi]])
                        else:
                            V(lambda ps=ps, yi=yi, c=c: nc.vector.tensor_copy(out=yo[yi][:, c * 512:(c + 1) * 512], in_=ps[:, :]), [pt], [t_yo[yi]])
                        cnt["ev"] += 1
                    k.store(k.sp, scr["ydisp"][e * CAP + b * 128: e * CAP + (b + 1) * 128, dh * 1024:(dh + 1) * 1024], yo[yi][:], t_yo[yi],
                            scr["t_ydisp"])
    k.barrier()


def phase6(k, cfg, io, scr):
    nc = k.nc
    T = k.track
    CAP = cfg.CAP
    NT = cfg.NOWN // 128
    IDX, WTS, t_idx = scr["IDX"], scr["WTS"], scr["t_idx"]
    with contextlib.ExitStack() as es:
        def sb(name, shape, dt):
            return es.enter_context(nc.sbuf_tensor("s6_" + name, list(shape), dt))

        def V(fn, r=(), w=()):
            k.op(k.dve, fn, r, w)

        def A(fn, r=(), w=()):
            k.op(k.act, fn, r, w)

        fnbc = sb("fnbc", [128, D], F32)
        g2bc = sb("g2bc", [128, D], F32)
        t_c, t_g2 = T(), T()
        k.load(k.sp, fnbc[:], io["final_norm_w"].partition_broadcast(128), t_c)
        y1 = [sb(f"y1{i}", [128, D], F32) for i in range(2)]
        y2 = [sb(f"y2{i}", [128, D], F32) for i in range(2)]
        x1 = [sb(f"x1{i}", [128, D], F32) for i in range(2)]
        t_y1, t_y2, t_x1 = [T(), T()], [T(), T()], [T(), T()]
        acc = sb("acc", [128, D], F32)
        t_acc = T()
        yout = [sb(f"yout{i}", [128, D], F32) for i in range(2)]
        t_yout = [T(), T()]
        junk = sb("junk", [128, D], BF16)
        t_junk = T()
        st = sb("st", [128, 8], F32)
        t_st = T()
        cur_mod = [-1]

        def load_mod(m):
            if cur_mod[0] == m:
                return
            cur_mod[0] = m
            k.load(k.sp, g2bc[:], scr["modrow"][m:m + 1, 5 * D:6 * D].partition_broadcast(128), t_g2, reads=[scr["t_modrow"]])

        def load_tile(i):
            s = i % 2
            g0 = i * 128
            k.idma(out=y1[s][:, :], out_off=None, in_=scr["ydisp"][:, :], in_off=bass.IndirectOffsetOnAxis(ap=IDX[:, i, 0:1], axis=0),
                   sem_track=t_y1[s], reads=[scr["t_ydisp"], t_idx], writes=[t_y1[s]], bounds_check=NE * CAP - 1, oob_is_err=False)
            k.idma(out=y2[s][:, :], out_off=None, in_=scr["ydisp"][:, :], in_off=bass.IndirectOffsetOnAxis(ap=IDX[:, i, 1:2], axis=0),
                   sem_track=t_y2[s], reads=[scr["t_ydisp"], t_idx], writes=[t_y2[s]], bounds_check=NE * CAP - 1, oob_is_err=False)
            k.load(k.sp, x1[s][:], scr["x1s"][g0:g0 + 128, :], t_x1[s], reads=[scr["t_x1s"]])

        load_tile(0)
        for i in range(NT):
            if i + 1 < NT:
                load_tile(i + 1)
            s = i % 2
            g0 = i * 128
            load_mod(0 if g0 < cfg.NPT else 1)
            V(lambda: nc.vector.tensor_scalar(out=acc[:], in0=y1[s][:], scalar1=WTS[:, i, 0:1], scalar2=None, op0=ALU.mult), [t_y1[s], t_idx], [t_acc])
            V(lambda: nc.vector.scalar_tensor_tensor(out=acc[:], in0=y2[s][:], scalar=WTS[:, i, 1:2], in1=acc[:], op0=ALU.mult, op1=ALU.add),
              [t_y2[s], t_idx, t_acc], [t_acc])
            V(lambda: nc.vector.tensor_tensor(out=acc[:], in0=acc[:], in1=g2bc[:], op=ALU.mult), [t_acc, t_g2], [t_acc])
            V(lambda: nc.vector.tensor_tensor(out=acc[:], in0=acc[:], in1=x1[s][:], op=ALU.add), [t_acc, t_x1[s]], [t_acc])
            A(lambda: nc.scalar.activation(out=junk[:], in_=acc[:], func=AF.Square, accum_out=st[:, 0:1]), [t_acc], [t_junk, t_st])
            V(lambda: nc.vector.tensor_scalar(out=st[:, 1:2], in0=st[:, 0:1], scalar1=1.0 / D, scalar2=EPS, op0=ALU.mult, op1=ALU.add), [t_st], [t_st])
            A(lambda: nc.scalar.activation(out=st[:, 2:3], in_=st[:, 1:2], func=AF.Ln), [t_st], [t_st])
            A(lambda: nc.scalar.activation(out=st[:, 3:4], in_=st[:, 2:3], func=AF.Exp, scale=-0.5), [t_st], [t_st])
            V(lambda: nc.vector.scalar_tensor_tensor(out=yout[s][:], in0=acc[:], scalar=st[:, 3:4], in1=fnbc[:], op0=ALU.mult, op1=ALU.mult),
              [t_acc, t_st, t_c], [t_yout[s]])
            k.store(k.sp, io["y"][g0:g0 + 128, :], yout[s][:], t_yout[s], scr["t_out"])
    k.barrier()


def build(cfg):
    nc = bass.Bass("TRN2", target_bir_lowering=False)
    io = declare_io(nc, cfg)
    with contextlib.ExitStack() as es:
        k = KB(nc, es)
        scr = make_scratch(k, cfg)
        phase01(k, cfg, io, scr)
        phase2(k, cfg, io, scr)
        phase3(k, cfg, io, scr)
        phase4(k, cfg, io, scr)
        phase5(k, cfg, io, scr)
        phase6(k, cfg, io, scr)
        k.final_wait()
    return nc, io


def kernel(**inputs):
    inp = {k_: np.asarray(v) for k_, v in inputs.items()}
    cfg = Cfg()
    consts = host_consts()
    nc, io = build(cfg)
    in_names = [n for n in io if not (n.startswith("o_") or n == "y")]
    in_maps = []
    for core in range(8):
        m = prep_core(inp, core, cfg, consts)
        in_maps.append({n: np.ascontiguousarray(m[n], dtype=np.float32) for n in in_names})
    res = run_bass_kernel_spmd(nc, in_maps, core_ids=list(range(8)))
    return assemble(res.results, cfg)


def assemble(results, cfg):
    B, S, DB, DS = 16, 256, 4, 4096
    y_prompt = np.zeros((B, S, D), np.float32)
    y_sample = np.zeros((DB, DS, D), np.float32)
    new_c = np.zeros((B, 1, 2, H, DK, DV), np.float32)
    new_n = np.zeros((B, 1, 2, H, DK), np.float32)
    new_m = np.zeros((B, 1, 2, H), np.float32)
    new_h = np.zeros((B, 1, 2, RW), np.float32)
    for core, r in enumerate(results):
        odd = core % 2
        b = core // 2
        y = np.asarray(r["y"], dtype=np.float32)
        dirs = (1, 0) if odd else (0, 1)
        for j in range(cfg.NP):
            seq = core * cfg.NP + j
            yp = y[j * cfg.TP:(j + 1) * cfg.TP]
            y_prompt[seq] = yp[::-1] if odd else yp
            for ld, gd in enumerate(dirs):
                new_c[seq, 0, gd] = np.asarray(r["o_c"])[j, ld]
                new_n[seq, 0, gd] = np.asarray(r["o_n"])[j, ld].T
                new_m[seq, 0, gd] = np.asarray(r["o_m"])[j, ld][:, 0]
                new_h[seq, 0, gd] = np.asarray(r["o_h"])[:, j, ld, :].T.reshape(-1)
        ys = y[cfg.NPT:cfg.NPT + cfg.TS]
        if odd:
            y_sample[b, cfg.TS:2 * cfg.TS] = ys[::-1]
        else:
            y_sample[b, 0:cfg.TS] = ys
    return (y_prompt, y_sample, new_c, new_n, new_m, new_h)
```
